# Optimizing a Trainium2 kernel written in Bass

```python
import math
import jax, jax.numpy as jnp
from jax import lax
import numpy as np

D_MODEL = 1024
BATCH = 16
SEQ = 2048
DEPTH = 1

NSA_HEADS = 8
NSA_KV_GROUPS = 2
NSA_HEAD_DIM = 64
NSA_HPG = NSA_HEADS // NSA_KV_GROUPS
CMP_BLOCK = 32
CMP_STRIDE = 16
CMP_HIDDEN = 2 * NSA_HEAD_DIM
SEL_BLOCK = 64
SEL_TOP_N = 8
SEL_QBLK = 64
WINDOW = 512
WIN_QBLK = 128
ROPE_THETA = 500000.0
ROPE_DIM = NSA_HEAD_DIM // 4
GDN_HEADS = 4
GDN_HEAD_DIM = 128
GDN_CONV = 4
GDN_CHUNK = 64
NSA_WIDTH = NSA_HEADS * NSA_HEAD_DIM
NSA_KV_WIDTH = NSA_KV_GROUPS * NSA_HEAD_DIM
GDN_WIDTH = GDN_HEADS * GDN_HEAD_DIM
MIX_WIDTH = NSA_WIDTH + GDN_WIDTH
D_FF = ((8 * D_MODEL + 3 * 256 - 1) // (3 * 256)) * 256
OFF_NSA_KV = NSA_WIDTH
OFF_NSA_GATE = OFF_NSA_KV + 6 * NSA_KV_WIDTH
OFF_GDN_QKV = OFF_NSA_GATE + 3 * NSA_HEADS
OFF_GDN_Z = OFF_GDN_QKV + 3 * GDN_WIDTH
OFF_GDN_B = OFF_GDN_Z + GDN_WIDTH
OFF_GDN_A = OFF_GDN_B + GDN_HEADS
IN_WIDTH = OFF_GDN_A + GDN_HEADS
SPLIT_POINTS = (OFF_NSA_KV, OFF_NSA_GATE, OFF_GDN_QKV, OFF_GDN_Z, OFF_GDN_B, OFF_GDN_A)
NEG_INF = -1e30
FORCE_SCORE = 1e9
RMS_EPS = 1e-6

kernel_name = "nsa_gdn_parallel_hybrid_block"


def rmsnorm(x, g):
    xf = x.astype(jnp.float32)
    y = xf * lax.rsqrt(jnp.mean(xf * xf, axis=-1, keepdims=True) + RMS_EPS)
    return (y * g.astype(jnp.float32)).astype(x.dtype)


def l2norm(x):
    return x * lax.rsqrt(jnp.sum(x * x, axis=-1, keepdims=True) + RMS_EPS)


def partial_rope(x, positions):
    half = ROPE_DIM // 2
    inv = jnp.power(ROPE_THETA, -jnp.arange(half, dtype=jnp.float32) * (2.0 / ROPE_DIM))
    ang = positions.astype(jnp.float32)[..., None] * inv
    cos = jnp.cos(ang)[:, :, None, :]
    sin = jnp.sin(ang)[:, :, None, :]
    xr = x[..., :ROPE_DIM].astype(jnp.float32)
    x1, x2 = xr[..., :half], xr[..., half:]
    rot = jnp.concatenate([x1 * cos - x2 * sin, x2 * cos + x1 * sin], axis=-1)
    return jnp.concatenate([rot.astype(x.dtype), x[..., ROPE_DIM:]], axis=-1)


def masked_softmax(s, mask):
    return jax.nn.softmax(jnp.where(mask, s, NEG_INF), axis=-1)


def nsa_compressed(q, k, v, cmp_pos, cmp_w1, cmp_w2):
    B, T = q.shape[0], q.shape[1]
    n_cmp = (T - CMP_BLOCK) // CMP_STRIDE + 1
    starts = jnp.arange(n_cmp) * CMP_STRIDE
    gidx = starts[:, None] + jnp.arange(CMP_BLOCK)[None, :]

    def compress(x, i):
        blk = x[:, gidx] + cmp_pos[i][None, None, :, None, :]
        blk = jnp.swapaxes(blk, 2, 3).reshape(B, n_cmp, NSA_KV_GROUPS, CMP_BLOCK * NSA_HEAD_DIM)
        return jax.nn.silu(blk @ cmp_w1[i]) @ cmp_w2[i]

    kc = compress(k, 0)
    vc = compress(v, 1)
    s = jnp.einsum('btghd,bngd->bghtn', q, kc, preferred_element_type=jnp.float32) * (NSA_HEAD_DIM ** -0.5)
    t = jnp.arange(T)
    valid = (starts + CMP_BLOCK - 1)[None, :] <= t[:, None]
    has_any = jnp.any(valid, axis=-1)[:, None].astype(jnp.float32)
    p = masked_softmax(s, valid) * has_any
    o = jnp.einsum('bghtn,bngd->btghd', p.astype(vc.dtype), vc)
    return o, p


def nsa_select_indices(p_cmp, T):
    n_cmp = p_cmp.shape[-1]
    n_sel = T // SEL_BLOCK
    cs = jnp.arange(n_cmp) * CMP_STRIDE
    ss = jnp.arange(n_sel) * SEL_BLOCK
    overlap = ((cs[:, None] < ss[None, :] + SEL_BLOCK) & (cs[:, None] + CMP_BLOCK > ss[None, :])).astype(jnp.float32)
    imp = jnp.einsum('bghtn,ns->bgts', p_cmp, overlap)
    cur = jnp.arange(T) // SEL_BLOCK
    j = jnp.arange(n_sel)[None, :]
    causal = j <= cur[:, None]
    forced = (j == 0) | (j == cur[:, None]) | (j == cur[:, None] - 1)
    score = jnp.where(forced, FORCE_SCORE, jnp.where(causal, imp, NEG_INF))
    n_top = min(SEL_TOP_N, n_sel)
    _, idx = lax.top_k(score, n_top)
    return idx


def nsa_selected(q, k, v, idx):
    B, T = q.shape[0], q.shape[1]
    n_sel = T // SEL_BLOCK
    n_top = idx.shape[-1]
    nq = T // SEL_QBLK
    kb = k.reshape(B, n_sel, SEL_BLOCK, NSA_KV_GROUPS, NSA_HEAD_DIM).transpose(0, 3, 1, 2, 4)
    vb = v.reshape(B, n_sel, SEL_BLOCK, NSA_KV_GROUPS, NSA_HEAD_DIM).transpose(0, 3, 1, 2, 4)
    qc = q.reshape(B, nq, SEL_QBLK, NSA_KV_GROUPS, NSA_HPG, NSA_HEAD_DIM).swapaxes(0, 1)
    ic = idx.reshape(B, NSA_KV_GROUPS, nq, SEL_QBLK, n_top).transpose(2, 0, 1, 3, 4)
    gather = jax.vmap(jax.vmap(lambda blocks, ix: blocks[ix]))
    n_keys = n_top * SEL_BLOCK

    def step(args):
        qb, ib, c = args
        kg = gather(kb, ib).reshape(B, NSA_KV_GROUPS, SEL_QBLK, n_keys, NSA_HEAD_DIM)
        vg = gather(vb, ib).reshape(B, NSA_KV_GROUPS, SEL_QBLK, n_keys, NSA_HEAD_DIM)
        s = jnp.einsum('bcghd,bgckd->bghck', qb, kg, preferred_element_type=jnp.float32) * (NSA_HEAD_DIM ** -0.5)
        t = c * SEL_QBLK + jnp.arange(SEL_QBLK)
        kpos = (ib[..., None] * SEL_BLOCK + jnp.arange(SEL_BLOCK)).reshape(B, NSA_KV_GROUPS, SEL_QBLK, n_keys)
        mask = (kpos <= t[None, None, :, None])[:, :, None]
        p = masked_softmax(s, mask)
        return jnp.einsum('bghck,bgckd->bcghd', p.astype(vg.dtype), vg)

    o = lax.map(step, (qc, ic, jnp.arange(nq)))
    return o.swapaxes(0, 1).reshape(B, T, NSA_KV_GROUPS, NSA_HPG, NSA_HEAD_DIM)


def nsa_window(q, k, v):
    B, T = q.shape[0], q.shape[1]
    nb = T // WIN_QBLK
    span = WIN_QBLK + WINDOW
    kp = jnp.pad(k, ((0, 0), (WINDOW, 0), (0, 0), (0, 0)))
    vp = jnp.pad(v, ((0, 0), (WINDOW, 0), (0, 0), (0, 0)))
    qc = q.reshape(B, nb, WIN_QBLK, NSA_KV_GROUPS, NSA_HPG, NSA_HEAD_DIM).swapaxes(0, 1)

    def step(args):
        qb, c = args
        start = c * WIN_QBLK
        kb = lax.dynamic_slice_in_dim(kp, start, span, axis=1)
        vb = lax.dynamic_slice_in_dim(vp, start, span, axis=1)
        s = jnp.einsum('bcghd,bkgd->bghck', qb, kb, preferred_element_type=jnp.float32) * (NSA_HEAD_DIM ** -0.5)
        t = start + jnp.arange(WIN_QBLK)
        kpos = start - WINDOW + jnp.arange(span)
        diff = t[:, None] - kpos[None, :]
        mask = (kpos[None, :] >= 0) & (diff >= 0) & (diff < WINDOW)
        p = masked_softmax(s, mask)
        return jnp.einsum('bghck,bkgd->bcghd', p.astype(vb.dtype), vb)

    o = lax.map(step, (qc, jnp.arange(nb)))
    return o.swapaxes(0, 1).reshape(B, T, NSA_KV_GROUPS, NSA_HPG, NSA_HEAD_DIM)


def causal_depthwise_conv(x, w):
    return lax.conv_general_dilated(x, w[:, None, :], window_strides=(1,), padding=[(GDN_CONV - 1, 0)],
                                    dimension_numbers=('NWC', 'WIO', 'NWC'), feature_group_count=x.shape[-1])


def gated_delta_rule(q, k, v, g, beta):
    B, T, H, Dk = k.shape
    Dv = v.shape[-1]
    C = GDN_CHUNK
    N = T // C

    def chunks(x):
        return x.reshape(B, N, C, H, x.shape[-1]).transpose(1, 0, 3, 2, 4)

    q = chunks(q) * (Dk ** -0.5)
    k = chunks(k)
    v = chunks(v)
    g = g.reshape(B, N, C, H).transpose(1, 0, 3, 2)
    beta = beta.reshape(B, N, C, H).transpose(1, 0, 3, 2)
    gc = jnp.cumsum(g, axis=-1)
    incl = jnp.tril(jnp.ones((C, C), dtype=bool))
    strict = jnp.tril(jnp.ones((C, C), dtype=bool), -1)
    decay = jnp.exp(jnp.where(incl, gc[..., :, None] - gc[..., None, :], -jnp.inf))
    kb = k * beta[..., None]
    a = jnp.where(strict, jnp.einsum('nbhcd,nbhed->nbhce', kb, k) * decay, 0.0)
    eye = jnp.eye(C, dtype=a.dtype)
    tmat = lax.linalg.triangular_solve(a + eye, jnp.broadcast_to(eye, a.shape), left_side=True,
                                       lower=True, unit_diagonal=True)
    u = tmat @ (v * beta[..., None])
    w = tmat @ (kb * jnp.exp(gc)[..., None])
    qk = jnp.where(incl, jnp.einsum('nbhcd,nbhed->nbhce', q, k) * decay, 0.0)

    def step(S, xs):
        q_i, k_i, u_i, w_i, gc_i, qk_i = xs
        v_new = u_i - w_i @ S
        o = (q_i * jnp.exp(gc_i)[..., None]) @ S + qk_i @ v_new
        g_last = gc_i[..., -1:]
        S = S * jnp.exp(g_last)[..., None] + jnp.einsum('bhck,bhcv->bhkv', k_i * jnp.exp(g_last - gc_i)[..., None], v_new)
        return S, o

    S0 = jnp.zeros((B, H, Dk, Dv), jnp.float32)
    _, o = lax.scan(step, S0, (q, k, u, w, gc, qk))
    return o.transpose(1, 0, 3, 2, 4).reshape(B, T, H, Dv)


def gdn_mixer(qkv, z, b, a, conv_w, a_log, dt_bias, norm_g):
    B, T = qkv.shape[0], qkv.shape[1]
    qkv = jax.nn.silu(causal_depthwise_conv(qkv, conv_w)).astype(jnp.float32)
    q, k, v = jnp.split(qkv, 3, axis=-1)
    q = l2norm(q.reshape(B, T, GDN_HEADS, GDN_HEAD_DIM))
    k = l2norm(k.reshape(B, T, GDN_HEADS, GDN_HEAD_DIM))
    v = v.reshape(B, T, GDN_HEADS, GDN_HEAD_DIM)
    beta = jax.nn.sigmoid(b.astype(jnp.float32))
    g = -jnp.exp(a_log.astype(jnp.float32)) * jax.nn.softplus(a.astype(jnp.float32) + dt_bias.astype(jnp.float32))
    o = gated_delta_rule(q, k, v, g, beta)
    o = rmsnorm(o, norm_g) * jax.nn.silu(z.reshape(B, T, GDN_HEADS, GDN_HEAD_DIM).astype(jnp.float32))
    return o.reshape(B, T, GDN_WIDTH).astype(z.dtype)


def hybrid_layer(x, positions, norm1_g, w_in, cmp_pos, cmp_w1, cmp_w2, nsa_norm_g, gdn_conv_w, gdn_a_log,
                 gdn_dt_bias, gdn_norm_g, w_out, norm2_g, w_gate, w_up, w_down):
    B, T = x.shape[0], x.shape[1]
    h = rmsnorm(x, norm1_g)
    proj = h @ w_in
    nsa_q, nsa_kv, nsa_gate, gdn_qkv, gdn_z, gdn_b, gdn_a = jnp.split(proj, SPLIT_POINTS, axis=-1)
    q = partial_rope(nsa_q.reshape(B, T, NSA_HEADS, NSA_HEAD_DIM), positions)
    q = q.reshape(B, T, NSA_KV_GROUPS, NSA_HPG, NSA_HEAD_DIM)
    kv = nsa_kv.reshape(B, T, 6, NSA_KV_GROUPS, NSA_HEAD_DIM)
    k_cmp = partial_rope(kv[:, :, 0], positions)
    k_slc = partial_rope(kv[:, :, 2], positions)
    k_win = partial_rope(kv[:, :, 4], positions)
    o_cmp, p_cmp = nsa_compressed(q, k_cmp, kv[:, :, 1], cmp_pos, cmp_w1, cmp_w2)
    idx = nsa_select_indices(p_cmp, T)
    o_slc = nsa_selected(q, k_slc, kv[:, :, 3], idx)
    o_win = nsa_window(q, k_win, kv[:, :, 5])
    gates = jax.nn.sigmoid(nsa_gate.astype(jnp.float32)).reshape(B, T, NSA_KV_GROUPS, NSA_HPG, 3)
    o_nsa = gates[..., 0:1] * o_cmp + gates[..., 1:2] * o_slc + gates[..., 2:3] * o_win
    o_nsa = rmsnorm(o_nsa.reshape(B, T, NSA_WIDTH).astype(x.dtype), nsa_norm_g)
    o_gdn = gdn_mixer(gdn_qkv, gdn_z, gdn_b, gdn_a, gdn_conv_w, gdn_a_log, gdn_dt_bias, gdn_norm_g)
    x = x + jnp.concatenate([o_nsa, o_gdn], axis=-1) @ w_out
    h = rmsnorm(x, norm2_g)
    x = x + (jax.nn.silu(h @ w_gate) * (h @ w_up)) @ w_down
    return x


def setup_inputs(seed: int = 0) -> dict:
    key = jax.random.key(seed)
    ks = jax.random.split(key, 20)
    f32 = jnp.float32
    L = DEPTH

    def nrm(k, shape, scale):
        return jax.random.normal(k, shape, f32) * scale

    x = nrm(ks[0], (BATCH, SEQ, D_MODEL), 1.0)
    positions = jnp.tile(jnp.arange(SEQ, dtype=jnp.int32)[None, :], (BATCH, 1))
    norm1_g = 1.0 + nrm(ks[1], (L, D_MODEL), 0.02)
    w_in = nrm(ks[2], (L, D_MODEL, IN_WIDTH), D_MODEL ** -0.5)
    cmp_pos = nrm(ks[3], (L, 2, CMP_BLOCK, NSA_HEAD_DIM), 0.1)
    cmp_w1 = nrm(ks[4], (L, 2, CMP_BLOCK * NSA_HEAD_DIM, CMP_HIDDEN), (CMP_BLOCK * NSA_HEAD_DIM) ** -0.5)
    cmp_w2 = nrm(ks[5], (L, 2, CMP_HIDDEN, NSA_HEAD_DIM), CMP_HIDDEN ** -0.5)
    nsa_norm_g = 1.0 + nrm(ks[6], (L, NSA_WIDTH), 0.02)
    gdn_conv_w = nrm(ks[7], (L, GDN_CONV, 3 * GDN_WIDTH), GDN_CONV ** -0.5)
    gdn_a_log = jnp.log(jax.random.uniform(ks[8], (L, GDN_HEADS), f32, 1.0, 16.0))
    dt = jnp.exp(jax.random.uniform(ks[9], (L, GDN_HEADS), f32, math.log(1e-3), math.log(1e-1)))
    gdn_dt_bias = dt + jnp.log(-jnp.expm1(-dt))
    gdn_norm_g = 1.0 + nrm(ks[10], (L, GDN_HEAD_DIM), 0.02)
    w_out = nrm(ks[11], (L, MIX_WIDTH, D_MODEL), MIX_WIDTH ** -0.5)
    norm2_g = 1.0 + nrm(ks[12], (L, D_MODEL), 0.02)
    w_gate = nrm(ks[13], (L, D_MODEL, D_FF), D_MODEL ** -0.5)
    w_up = nrm(ks[14], (L, D_MODEL, D_FF), D_MODEL ** -0.5)
    w_down = nrm(ks[15], (L, D_FF, D_MODEL), D_FF ** -0.5)
    final_g = 1.0 + nrm(ks[16], (D_MODEL,), 0.02)
    return {"x": x, "positions": positions, "norm1_g": norm1_g, "w_in": w_in, "cmp_pos": cmp_pos,
            "cmp_w1": cmp_w1, "cmp_w2": cmp_w2, "nsa_norm_g": nsa_norm_g, "gdn_conv_w": gdn_conv_w,
            "gdn_a_log": gdn_a_log, "gdn_dt_bias": gdn_dt_bias, "gdn_norm_g": gdn_norm_g, "w_out": w_out,
            "norm2_g": norm2_g, "w_gate": w_gate, "w_up": w_up, "w_down": w_down, "final_g": final_g}


def reference(x, positions, norm1_g, w_in, cmp_pos, cmp_w1, cmp_w2, nsa_norm_g, gdn_conv_w, gdn_a_log,
              gdn_dt_bias, gdn_norm_g, w_out, norm2_g, w_gate, w_up, w_down, final_g):
    for l in range(DEPTH):
        x = hybrid_layer(x, positions, norm1_g[l], w_in[l], cmp_pos[l], cmp_w1[l], cmp_w2[l], nsa_norm_g[l],
                         gdn_conv_w[l], gdn_a_log[l], gdn_dt_bias[l], gdn_norm_g[l], w_out[l], norm2_g[l],
                         w_gate[l], w_up[l], w_down[l])
    return rmsnorm(x, final_g)
```

```python
import contextlib
import numpy as np
import ml_dtypes
import concourse.bass as bass
import concourse.mybir as mybir
from concourse.bass_utils import run_bass_kernel_spmd

F32 = mybir.dt.float32
BF16 = mybir.dt.bfloat16
I32 = mybir.dt.int32
AF = mybir.ActivationFunctionType
ALU = mybir.AluOpType
AX = mybir.AxisListType

NCORES = 8
NSEQ = 2
T = 2048
D = 1024
NT = T // 128
DFF = 2816
NFC = DFF // 128
NEG = -30000.0
EPS = 1e-6
OFF_KV = 512
OFF_GATE = OFF_KV + 768
OFF_GQKV = OFF_GATE + 24
OFF_GZ = OFF_GQKV + 1536
OFF_GB = OFF_GZ + 512
OFF_GA = OFF_GB + 4
FM_Q, FM_QSW, FM_K, FM_KSW, FM_VC, FM_GQKV, FM_Z = 0, 4, 8, 14, 20, 21, 33
NFM = 37
NTM = 288
NCBF = 1568 + 2048 + 2048 + 512 + 256


class Res:
    __slots__ = ("name", "w", "r")

    def __init__(self, name=""):
        self.name = name
        self.w = {}
        self.r = {}


class Sched:
    ENGS = ("pe", "act", "dve", "pool", "sp")

    def __init__(self, nc, ring=6):
        self.nc = nc
        self.prog = {k: [] for k in self.ENGS}
        self.esem = {k: nc.alloc_semaphore(name=f"es_{k}") for k in self.ENGS}
        self.ecnt = {k: 0 for k in self.ENGS}
        self.known = {k: {} for k in self.ENGS}
        self.R = ring
        self.rsem = {q: [nc.alloc_semaphore(name=f"rs_{q}{i}") for i in range(ring)] for q in ("sp", "pool", "act")}
        self.rcnt = {q: [0] * ring for q in self.rsem}
        self.rpos = {q: 0 for q in self.rsem}
        self.n_wait = 0
        self.dead = False

    def _collect(self, reads, writes):
        deps = {}

        def add(d):
            for k, (s, v) in d.items():
                if k not in deps or deps[k][1] < v:
                    deps[k] = (s, v)

        for r in reads:
            add(r.w)
        for w in writes:
            add(w.w)
            add(w.r)
        return deps

    def _emit_waits(self, eng, deps):
        kn = self.known[eng]
        for k, (s, v) in deps.items():
            if kn.get(k, 0) < v:
                kn[k] = v
                self.n_wait += 1
                self.prog[eng].append(lambda e, s=s, v=v: e.wait_ge(s, v))

    def op(self, eng, fn, reads=(), writes=()):
        if self.dead:
            return
        deps = self._collect(reads, writes)
        if eng in deps and eng == "pe":
            del deps[eng]
        self._emit_waits(eng, deps)
        self.ecnt[eng] += 1
        cnt = self.ecnt[eng]
        sem = self.esem[eng]
        self.prog[eng].append(lambda e, fn=fn, sem=sem: fn(e).then_inc(sem, 1))
        tok = (sem, cnt)
        for r in reads:
            r.r[eng] = tok
        for w in writes:
            w.w = {eng: tok}
            w.r = {}

    def dma(self, q, out, in_, reads=(), writes=()):
        if self.dead:
            return
        j = self.rpos[q] % self.R
        self.rpos[q] += 1
        sem = self.rsem[q][j]
        key = ("ring", q, j)
        deps = self._collect(reads, writes)
        if self.rcnt[q][j] > 0:
            deps[key] = (sem, 16 * self.rcnt[q][j])
        self._emit_waits(q, deps)
        self.rcnt[q][j] += 1
        val = 16 * self.rcnt[q][j]
        self.prog[q].append(lambda e, out=out, in_=in_, sem=sem: e.dma_start(out=out, in_=in_).then_inc(sem, 16))
        tok = (sem, val)
        for r in reads:
            r.r[key] = tok
        for w in writes:
            w.w = {key: tok}
            w.r = {}

    def barrier(self):
        deps = {}
        for k in self.ENGS:
            if self.ecnt[k]:
                deps[k] = (self.esem[k], self.ecnt[k])
        for q in self.rsem:
            for j in range(self.R):
                if self.rcnt[q][j]:
                    deps[("ring", q, j)] = (self.rsem[q][j], 16 * self.rcnt[q][j])
        for e in self.ENGS:
            d = dict(deps)
            d.pop(e, None)
            self._emit_waits(e, d)

    def run(self):
        nc = self.nc
        with nc.Block() as block:
            @block.tensor
            def _(e):
                for f in self.prog["pe"]:
                    f(e)

            @block.scalar
            def _(e):
                for f in self.prog["act"]:
                    f(e)

            @block.vector
            def _(e):
                for f in self.prog["dve"]:
                    f(e)

            @block.gpsimd
            def _(e):
                for f in self.prog["pool"]:
                    f(e)

            @block.sync
            def _(e):
                for f in self.prog["sp"]:
                    f(e)


def interleave(gens):
    gens = list(gens)
    while gens:
        for g_ in list(gens):
            try:
                next(g_)
            except StopIteration:
                gens.remove(g_)


def build_program(nseq=NSEQ, stage=99, dbg=(), nsa_attn=True, gdn_cut=0, mixers=True):
    nc = bass.Bass("TRN2", target_bir_lowering=False)
    S = Sched(nc)
    es = contextlib.ExitStack()

    def dram(name, shape, dt, kind="ExternalInput"):
        return nc.dram_tensor(name, list(shape), dt, kind=kind).ap()

    def sb(name, shape, dt):
        return es.enter_context(nc.sbuf_tensor("s_" + name, list(shape), dt))

    x_d = dram("x", [nseq, T, D], F32)
    pos_d = dram("pos", [nseq, T], I32)
    wfm_d = dram("wfm", [NFM, 128, 1024], F32)
    wtm_d = dram("wtm", [128, 8 * NTM], F32)
    g1_d = dram("g1", [128, 8], F32)
    ropec_d = dram("ropec", [128, 4], F32)
    cbf_d = dram("cbf", [128, NCBF], BF16)
    selc_d = dram("selc", [128, NT * 32], F32)
    w1_d = dram("w1", [128, 2 * 32 * 128], F32)
    w2_d = dram("w2", [128, 128 + 64], F32)
    posT_d = dram("posT", [64, 2 * 32], F32)
    nsag_d = dram("nsag", [128, 4], F32)
    gdnc_d = dram("gdnc", [1, 8], F32)
    cf32_d = dram("cf32", [128, 386], F32)
    convw_d = dram("convw", [128, 48], F32)
    gng_d = dram("gng", [128, 1], F32)
    wout_d = dram("wout", [128, 8 * D], F32)
    g2_d = dram("g2", [128, 8], F32)
    fg_d = dram("fg", [1, D], F32)
    wg_d = dram("wg", [NFC, 128, 1024], F32)
    wu_d = dram("wu", [NFC, 128, 1024], F32)
    wd_d = dram("wd", [128, NFC * D], F32)
    x2_d = dram("x2s", [nseq, T, D], F32, kind="Internal")
    R_x2s = [Res() for _ in range(NT)]
    out_d = dram("out", [nseq, T, D], F32, kind="ExternalOutput")
    dbg_d = {}
    for name, shape in dbg:
        dbg_d[name] = dram("dbg_" + name, shape, F32, kind="ExternalOutput")

    cbf = sb("cbf", [128, NCBF], BF16)
    ident = cbf[:, 0:128]
    ones_bf = cbf[:, 128:256]
    causal = cbf[:, 256:384]
    anti = cbf[:, 384:512]
    maskA4 = cbf[:, 512:1024]
    maskQ4 = cbf[:, 1024:1536]
    ovl = cbf[:, 1536:1568]
    esel = cbf[:, 1568:1568 + 2048]
    cmpmask = cbf[:, 3616:3616 + 2048]
    ident4 = cbf[:, 5664:5664 + 512]
    causal01 = cbf[:, 6176:6304]
    anti01 = cbf[:, 6304:6432]
    cf32 = sb("cf32", [128, 3 * 128 + 2], F32)
    ltriT = cf32[:, 0:128]
    bones = cf32[:, 128:256]
    ones_f = cf32[:, 256:384]
    cind = cf32[:, 384:386]
    convw = sb("convw", [128, 12, 4], F32)
    gng = sb("gng", [128, 1], F32)
    g2T = sb("g2T", [128, 8], F32)
    fgb = sb("fgb", [128, D], F32)
    selc = sb("selc", [128, NT, 32], F32)
    g1T = sb("g1T", [128, 8], F32)
    ropec = sb("ropec", [128, 4], F32)
    cst = sb("cst", [128, 8], F32)
    nsag = sb("nsag", [128, 4], F32)
    hT = sb("hT", [128, 8, T], BF16)
    mixT = sb("mixT", [128, 8, T], BF16)
    beta_all = sb("beta_all", [128, NT, 4], F32)
    g_all = sb("g_all", [128, NT, 4], F32)
    R_bg = [Res() for _ in range(NT)]
    dtb = sb("dtb", [128, 4], F32)
    negA = sb("negA", [128, 4], F32)
    junk2 = sb("junk2", [128, 512], BF16)
    R_const = Res("const")
    R_hT = [Res(f"hT{i}") for i in range(NT)]
    R_mix = [[Res(f"mix{c}_{i}") for i in range(NT)] for c in range(2)]

    ps = [es.enter_context(nc.psum_tensor(f"ps{i}", [128, 512], F32)) for i in range(8)]
    R_ps = [Res(f"ps{i}") for i in range(8)]

    S.dma("sp", cbf[:], cbf_d[:], writes=[R_const])
    S.dma("sp", selc[:].rearrange("p a b -> p (a b)"), selc_d[:], writes=[R_const])
    S.dma("sp", g1T[:], g1_d[:], writes=[R_const])
    S.dma("sp", ropec[:], ropec_d[:], writes=[R_const])
    S.dma("sp", nsag[:], nsag_d[:], writes=[R_const])
    S.dma("sp", cf32[:], cf32_d[:], writes=[R_const])
    S.dma("sp", convw[:].rearrange("p a b -> p (a b)"), convw_d[:], writes=[R_const])
    S.dma("sp", gng[:], gng_d[:], writes=[R_const])
    S.dma("sp", g2T[:], g2_d[:], writes=[R_const])
    S.dma("sp", fgb[:], fg_d[0:1, :].to_broadcast([128, D]), writes=[R_const])
    S.dma("sp", dtb[:], gdnc_d[0:1, 4:8].to_broadcast([128, 4]), writes=[R_const])
    S.dma("sp", negA[:], gdnc_d[0:1, 0:4].to_broadcast([128, 4]), writes=[R_const])
    S.op("dve", lambda e: e.memset(cst[:, 0:1], EPS), writes=[R_const])
    S.op("dve", lambda e: e.memset(cst[:, 1:2], 1.0), writes=[R_const])
    S.op("dve", lambda e: e.memset(cst[:, 2:3], 0.0), writes=[R_const])
    S.op("dve", lambda e: e.memset(cst[:, 3:4], 1e-30), writes=[R_const])
    S.op("dve", lambda e: e.memset(cst[:, 4:5], EPS * 128), writes=[R_const])
    eps_ap, one_ap, zero_ap, tiny_ap, eps128_ap = cst[:, 0:1], cst[:, 1:2], cst[:, 2:3], cst[:, 3:4], cst[:, 4:5]

    def act(out, in_, func, bias=None, scale=1.0, accum_out=None, reads=(), writes=(), eng="act"):
        kw = {}
        if bias is not None:
            kw["bias"] = bias
        if accum_out is not None:
            kw["accum_out"] = accum_out
        S.op("act", lambda e: e.activation(out=out, in_=in_, func=func, scale=scale, **kw), reads=list(reads) + [R_const], writes=writes)

    def mm(out, lhsT, rhs, start, stop, reads=(), writes=(), skip=False):
        S.op("pe", lambda e: e.matmul(out, lhsT, rhs, start=start, stop=stop, skip_group_check=skip), reads=reads, writes=writes)

    def transpose(out, in_, idn, reads=(), writes=()):
        S.op("pe", lambda e: e.transpose(out, in_, idn), reads=list(reads) + [R_const], writes=writes)

    def rstd_from_ss(rstd, ss, n, reads_writes):
        act(rstd, ss, AF.Ln, bias=eps_ap, scale=1.0 / n, reads=reads_writes, writes=reads_writes)
        act(rstd, rstd, AF.Exp, scale=-0.5, reads=reads_writes, writes=reads_writes)

    act(negA[:], negA[:], AF.Exp, reads=[R_const], writes=[R_const])
    S.op("dve", lambda e: e.tensor_scalar(out=negA[:], in0=negA[:], scalar1=-1.0, scalar2=None, op0=ALU.mult), reads=[R_const], writes=[R_const])

    def tt(out, in0, in1, op, reads=(), writes=(), eng="dve"):
        S.op(eng, lambda e: e.tensor_tensor(out=out, in0=in0, in1=in1, op=op), reads=reads, writes=writes)

    def ts(out, in0, s1, s2, op0, op1=None, reads=(), writes=(), eng="dve"):
        if op1 is None:
            S.op(eng, lambda e: e.tensor_scalar(out=out, in0=in0, scalar1=s1, scalar2=None, op0=op0), reads=reads, writes=writes)
        else:
            S.op(eng, lambda e: e.tensor_scalar(out=out, in0=in0, scalar1=s1, scalar2=s2, op0=op0, op1=op1), reads=reads, writes=writes)

    def stt(out, in0, scalar, in1, op0, op1, reads=(), writes=()):
        S.op("dve", lambda e: e.scalar_tensor_tensor(out=out, in0=in0, scalar=scalar, in1=in1, op0=op0, op1=op1), reads=reads, writes=writes)

    def cp(out, in_, reads=(), writes=(), eng="dve"):
        S.op(eng, lambda e: e.tensor_copy(out=out, in_=in_), reads=reads, writes=writes)

    def memset(ap, val, writes=(), eng="pool"):
        S.op(eng, lambda e: e.memset(ap, val), writes=writes)

    def max8(out, in_, reads=(), writes=()):
        S.op("dve", lambda e: e.max(out=out, in_=in_), reads=reads, writes=writes)

    def recip(out, in_, reads=(), writes=()):
        S.op("dve", lambda e: e.reciprocal(out=out, in_=in_), reads=reads, writes=writes)

    if not mixers:
        nsa_attn = False
        S.op("pool", lambda e: e.memset(mixT[:], 0.0), writes=[x for y in R_mix for x in y])
    for seq in range(nseq):
        with contextlib.ExitStack() as ph:
            def psb(name, shape, dt):
                return ph.enter_context(nc.sbuf_tensor(f"s_{name}_{seq}", list(shape), dt))

            tab = psb("tab", [128, 2, T], F32)
            R_tab = Res()
            with contextlib.ExitStack() as ph0:
                def psb0(name, shape, dt):
                    return ph0.enter_context(nc.sbuf_tensor(f"s_{name}_{seq}", list(shape), dt))
                xt = [psb0(f"xt{i}", [128, D], F32) for i in range(2)]
                R_xt = [Res(), Res()]
                junk = psb0("junk", [128, D], BF16)
                hn = psb0("hn", [128, D], BF16)
                R_hn = Res()
                stat = psb0("stat", [128, 8], F32)
                R_stat = Res()
                posi = psb0("posi", [128, T], I32)
                tmpF = psb0("tmpF", [128, 2, T], F32)
                tmpI = psb0("tmpI", [128, 2, T], I32)
                S.dma("sp", posi[:], pos_d[seq:seq + 1, :].to_broadcast([128, T]), writes=[R_tab])
                ts(tmpF[:, 0, :], posi[:], ropec[:, 0:1], None, ALU.mult, reads=[R_tab, R_const], writes=[R_tab])
                ts(tmpF[:, 1, :], posi[:], ropec[:, 1:2], ropec[:, 2:3], ALU.mult, ALU.add, reads=[R_tab, R_const], writes=[R_tab])
                cp(tmpI[:], tmpF[:], reads=[R_tab], writes=[R_tab])
                cp(tab[:], tmpI[:], reads=[R_tab], writes=[R_tab])
                tt(tmpF[:], tmpF[:], tab[:], ALU.subtract, reads=[R_tab], writes=[R_tab])
                ts(tab[:], tmpF[:], 0.5, None, ALU.is_gt, reads=[R_tab], writes=[R_tab])
                tt(tmpF[:], tmpF[:], tab[:], ALU.subtract, reads=[R_tab], writes=[R_tab])
                ts(tab[:], tmpF[:], -0.5, None, ALU.is_lt, reads=[R_tab], writes=[R_tab])
                tt(tmpF[:], tmpF[:], tab[:], ALU.add, reads=[R_tab], writes=[R_tab])
                act(tab[:], tmpF[:], AF.Sin, scale=6.283185, reads=[R_tab], writes=[R_tab])
                for i in range(NT):
                    b = i % 2
                    S.dma("sp", xt[b][:], x_d[seq, i * 128:(i + 1) * 128, :], writes=[R_xt[b]])
                    act(junk[:], xt[b][:], AF.Square, accum_out=stat[:, 0:1], reads=[R_xt[b]], writes=[R_stat])
                    rstd_from_ss(stat[:, 1:2], stat[:, 0:1], D, [R_stat])
                    ts(hn[:], xt[b][:], stat[:, 1:2], None, ALU.mult, reads=[R_xt[b], R_stat], writes=[R_hn])
                    pb = ps[7][:].bitcast(BF16)
                    for k in range(8):
                        transpose(pb[:, k * 128:(k + 1) * 128], hn[:, k * 128:(k + 1) * 128], ident, reads=[R_hn], writes=[R_ps[7]])
                    tt(hT[:, :, i * 128:(i + 1) * 128], pb.rearrange("p (k t) -> p k t", k=8),
                       g1T[:].unsqueeze(2).to_broadcast([128, 8, 128]), ALU.mult, reads=[R_ps[7], R_const], writes=[R_hT[i]])
            S.barrier()

            kcT = [psb(f"kcT{g}", [128, 128], BF16) for g in range(2)]
            vca = [psb(f"vca{g}", [128, 97], BF16) for g in range(2)]
            ph1 = contextlib.ExitStack()

            def psb1(name, shape, dt):
                return ph1.enter_context(nc.sbuf_tensor(f"s_{name}_{seq}", list(shape), dt))
            QT = psb("QT", [128, 4, T], BF16)
            KT = psb("KT", [128, 6, T], BF16)
            VcT = psb("VcT", [128, T], BF16)
            Vtok = psb("Vtok", [128, NT, 4, 65], BF16)
            gat = psb("gat", [128, NT, 24], F32)
            R_QT = [[Res() for _ in range(4)] for _ in range(4)]
            R_KT = [[Res() for _ in range(4)] for _ in range(6)]
            R_Vc = [Res() for _ in range(4)]
            R_Vtok = [Res() for _ in range(NT)]
            R_gat = [Res() for _ in range(NT)]
            wb = [psb1(f"wb{i}", [128, 1024], BF16) for i in range(4)]
            R_wb = [Res() for _ in range(4)]
            wtm = psb1("wtm", [128, 8, NTM], BF16)
            R_wtm = Res()
            rt = [psb1(f"rt{i}", [128, 512], F32) for i in range(4)]
            R_rt = [Res() for _ in range(4)]
            S.dma("pool", wtm[:].rearrange("p k n -> p (k n)"), wtm_d[:], writes=[R_wtm])
            memset(Vtok[:, :, :, 64:65], 1.0, writes=R_Vtok)
            wctr = [0]

            def load_w(chunk):
                r = wctr[0] % 4
                wctr[0] += 1
                S.dma("pool", wb[r][:], wfm_d[chunk], writes=[R_wb[r]])
                return r

            def proj_fm(r, tb, bank):
                for kc in range(8):
                    mm(ps[bank][:], wb[r][:, kc * 128:(kc + 1) * 128], hT[:, kc, tb * 512:(tb + 1) * 512], kc == 0, kc == 7,
                       reads=[R_wb[r]] + R_hT[tb * 4:tb * 4 + 4], writes=[R_ps[bank]])

            pairs = [(FM_Q + j, FM_QSW + j, QT, j, R_QT[j]) for j in range(4)] + [(FM_K + m, FM_KSW + m, KT, m, R_KT[m]) for m in range(6)]
            bctr = 0
            plist = pairs if nsa_attn else []
            pre_loaded = {}
            if plist:
                pre_loaded[0] = (load_w(plist[0][0]), load_w(plist[0][1]))
            for pi, (ca, cb_, dst, di, rdst) in enumerate(plist):
                ra, rb = pre_loaded.pop(pi)
                if pi + 1 < len(plist):
                    pre_loaded[pi + 1] = (load_w(plist[pi + 1][0]), load_w(plist[pi + 1][1]))
                for tb in range(4):
                    ba = (bctr % 3) * 2
                    bctr += 1
                    proj_fm(ra, tb, ba)
                    proj_fm(rb, tb, ba + 1)
                    i0 = (bctr % 2) * 2
                    tt(rt[i0][:], ps[ba][:], tab[:, 1, tb * 512:(tb + 1) * 512], ALU.mult, reads=[R_ps[ba], R_tab], writes=[R_rt[i0]])
                    tt(rt[i0 + 1][:], ps[ba + 1][:], tab[:, 0, tb * 512:(tb + 1) * 512], ALU.mult, reads=[R_ps[ba + 1], R_tab], writes=[R_rt[i0 + 1]])
                    tt(dst[:, di, tb * 512:(tb + 1) * 512], rt[i0][:], rt[i0 + 1][:], ALU.add, reads=[R_rt[i0], R_rt[i0 + 1]], writes=[rdst[tb]])
            rv = load_w(FM_VC)
            for tb in (range(4) if nsa_attn else []):
                proj_fm(rv, tb, 6)
                act(VcT[:, tb * 512:(tb + 1) * 512], ps[6][:], AF.Copy, reads=[R_ps[6]], writes=[R_Vc[tb]])
            sg2 = [psb1(f"sg{i}", [128, 32], F32) for i in range(2)]
            R_sg2 = [Res(), Res()]
            for i in range(NT):
                bank = 6 + (i % 2)
                sg, R_sg = sg2[i % 2], R_sg2[i % 2]
                for kc in range(8):
                    mm(ps[bank][:, 0:NTM], hT[:, kc, i * 128:(i + 1) * 128], wtm[:, kc, :], kc == 0, kc == 7,
                       reads=[R_hT[i], R_wtm], writes=[R_ps[bank]])
                act(Vtok[:, i, :, 0:64], ps[bank][:, 0:256].rearrange("p (a b) -> p a b", a=4), AF.Copy, reads=[R_ps[bank]], writes=[R_Vtok[i]])
                act(sg[:, 0:28], ps[bank][:, 256:284], AF.Exp, scale=-1.0, reads=[R_ps[bank]], writes=[R_sg])
                ts(sg[:, 0:28], sg[:, 0:28], 1.0, None, ALU.add, reads=[R_sg], writes=[R_sg])
                recip(gat[:, i, :], sg[:, 0:24], reads=[R_sg], writes=[R_gat[i]])
                recip(beta_all[:, i, :], sg[:, 24:28], reads=[R_sg], writes=[R_bg[i]])
                tt(sg[:, 28:32], ps[bank][:, 284:288], dtb[:], ALU.add, reads=[R_ps[bank], R_const], writes=[R_sg])
                act(sg[:, 28:32], sg[:, 28:32], AF.Exp, reads=[R_sg], writes=[R_sg])
                act(sg[:, 28:32], sg[:, 28:32], AF.Ln, bias=one_ap, reads=[R_sg], writes=[R_sg])
                tt(g_all[:, i, :], sg[:, 28:32], negA[:], ALU.mult, reads=[R_sg, R_const], writes=[R_bg[i]])

            if nsa_attn:
                w1 = psb1("w1", [128, 2, 32, 128], BF16)
                w2 = psb1("w2", [128, 192], BF16)
                posT = psb1("posT", [64, 2, 32], BF16)
                R_cw = Res()
                S.dma("pool", w1[:].rearrange("p a l h -> p (a l h)"), w1_d[:], writes=[R_cw])
                S.dma("pool", w2[:], w2_d[:], writes=[R_cw])
                S.dma("pool", posT[:].rearrange("p a l -> p (a l)"), posT_d[:], writes=[R_cw])
                R_kc = [Res(), Res()]
                R_vc = [Res(), Res()]
                cbias = psb1("cbias", [128, 2], F32)
                hid = psb1("hid", [128, 128], BF16)
                R_hid = Res()
                R_cb = Res()
                for g in range(2):
                    memset(kcT[g][:], 0.0, writes=[R_kc[g]])
                    memset(vca[g][:], 0.0, writes=[R_vc[g]])
                    memset(vca[g][:, 64:65], 1.0, writes=[R_vc[g]])
                    cp(vca[g][:, 65:97], ovl, reads=[R_const], writes=[R_vc[g]], eng="pool")
                memset(hid[:], 0.0, writes=[R_hid])
                for i2 in range(2):
                    for l in range(32):
                        mm(ps[6][:, i2:i2 + 1], w1[0:64, i2, l, :], posT[:, i2, l:l + 1], l == 0, l == 31, reads=[R_cw], writes=[R_ps[6]])
                cp(cbias[:], ps[6][:, 0:2], reads=[R_ps[6]], writes=[R_cb])
                for g in range(2):
                    for i2 in range(2):
                        if i2 == 0:
                            src, pb0, rsrc = KT[0:64, g, :], 0, R_KT[g]
                        else:
                            pb0 = g * 64
                            src, rsrc = VcT[pb0:pb0 + 64, :], R_Vc
                        for l in range(32):
                            mm(ps[6][:, 0:127], w1[pb0:pb0 + 64, i2, l, :], src[:, l:l + 16 * 126 + 1:16], l == 0, l == 31,
                               reads=[R_cw] + rsrc, writes=[R_ps[6]])
                        act(hid[:, 0:127], ps[6][:, 0:127], AF.Silu, bias=cbias[:, i2:i2 + 1], reads=[R_ps[6], R_cb], writes=[R_hid])
                        if i2 == 0:
                            mm(ps[7][:, 0:127], w2[:, 0:128], hid[:, 0:127], True, True, reads=[R_cw, R_hid], writes=[R_ps[7]])
                            cp(kcT[g][:, 0:127], ps[7][:, 0:127], reads=[R_ps[7]], writes=[R_kc[g]])
                        else:
                            mm(ps[7][:, 0:64], hid[:, :], w2[:, 128:192], True, True, reads=[R_cw, R_hid], writes=[R_ps[7]])
                            cp(vca[g][0:127, 0:64], ps[7][0:127, 0:64], reads=[R_ps[7]], writes=[R_vc[g]])

                ph1.close()
                S.barrier()
                PT = [psb(f"PT{i}", [128, 512], BF16) for i in range(4)]
                R_PT = [Res() for _ in range(4)]
                STB = [0, 1, 2, 6]
                onsa = psb("onsa", [128, 4, 512], F32)
                R_onsa = Res()
                onbf = psb("onbf", [128, 4, 512], BF16)
                R_onbf = Res()
                impsum = psb("impsum", [128, 4, 32], F32)
                impt = psb("impt", [128, 4, 32], F32)
                R_imp = Res()
                R_impt = Res()
                selb = psb("selb", [128, 4, 32], BF16)
                R_selb = Res()
                m8 = psb("m8", [128, 8], F32)
                R_m8 = Res()
                selbT = [psb(f"selbT{g}", [32, 512], BF16) for g in range(2)]
                R_selbT = [Res(), Res()]
                sm = [psb(f"sm{i}", [128, 16], F32) for i in range(2)]
                R_sm = [Res(), Res()]
                tmpo = [psb(f"tmpo{i}", [128, 4, 64], F32) for i in range(2)]
                R_tmpo = [Res(), Res()]
                fst = psb("fst", [128, 8], F32)
                R_fst = Res()
                ctr = {"st": 0, "pt": 0, "ob": 0, "sm": 0}

                def nxt(k, n):
                    v = ctr[k] % n
                    ctr[k] += 1
                    return v

                def evac_branch(ob, width, c, hh, br, first):
                    ov = ps[ob][:, 0:4 * width].rearrange("p (q f) -> p q f", q=4)
                    si = nxt("sm", 2)
                    smt = sm[si]
                    ts(smt[:, 0:4], ov[:, :, 64], 1e-30, None, ALU.add, reads=[R_ps[ob]], writes=[R_sm[si]])
                    recip(smt[:, 4:8], smt[:, 0:4], reads=[R_sm[si]], writes=[R_sm[si]])
                    tt(smt[:, 8:12], smt[:, 4:8], gat[:, 4 * c:4 * c + 4, hh * 3 + br], ALU.mult, reads=[R_sm[si]] + R_gat[4 * c:4 * c + 4], writes=[R_sm[si]])
                    cb = smt[:, 8:12].unsqueeze(2).to_broadcast([128, 4, 64])
                    if first:
                        tt(onsa[:, :, hh * 64:(hh + 1) * 64], ov[:, :, 0:64], cb, ALU.mult, reads=[R_ps[ob], R_sm[si]], writes=[R_onsa])
                    else:
                        tt(tmpo[si][:], ov[:, :, 0:64], cb, ALU.mult, reads=[R_ps[ob], R_sm[si]], writes=[R_tmpo[si]])
                        tt(onsa[:, :, hh * 64:(hh + 1) * 64], onsa[:, :, hh * 64:(hh + 1) * 64], tmpo[si][:], ALU.add,
                           reads=[R_tmpo[si], R_onsa], writes=[R_onsa], eng="pool")
                    return smt, si

                for c in range(4):
                    for g in range(2):
                        cmp_pt = []
                        for h in range(4):
                            hh = 4 * g + h
                            j, hb = hh // 2, (hh % 2) * 64
                            qT = QT[hb:hb + 64, j, c * 512:(c + 1) * 512]
                            st = STB[nxt("st", 4)]
                            mm(ps[st][:], kcT[g][hb:hb + 64, :], qT, True, False, reads=[R_kc[g], R_QT[j][c]], writes=[R_ps[st]])
                            mm(ps[st][:], ident, cmpmask[:, c * 512:(c + 1) * 512], False, True, reads=[R_const], writes=[R_ps[st]])
                            pt = nxt("pt", 4)
                            act(PT[pt][:], ps[st][:], AF.Exp, scale=0.125, reads=[R_ps[st]], writes=[R_PT[pt]])
                            cmp_pt.append(pt)
                        for h in range(4):
                            hh = 4 * g + h
                            pt = cmp_pt[h]
                            ob = 3 + nxt("ob", 2)
                            for tq in range(4):
                                mm(ps[ob][:, tq * 97:(tq + 1) * 97], PT[pt][:, tq * 128:(tq + 1) * 128], vca[g][:, 0:97], tq == 0, tq == 3,
                                   reads=[R_PT[pt], R_vc[g]], writes=[R_ps[ob]], skip=True)
                            smt, si = evac_branch(ob, 97, c, hh, 0, True)
                            ov = ps[ob][:, 0:388].rearrange("p (q f) -> p q f", q=4)
                            rb_ = smt[:, 4:8].unsqueeze(2).to_broadcast([128, 4, 32])
                            if h == 0:
                                tt(impsum[:], ov[:, :, 65:97], rb_, ALU.mult, reads=[R_ps[ob], R_sm[si]], writes=[R_imp])
                            else:
                                tt(impt[:], ov[:, :, 65:97], rb_, ALU.mult, reads=[R_ps[ob], R_sm[si]], writes=[R_impt])
                                tt(impsum[:], impsum[:], impt[:], ALU.add, reads=[R_imp, R_impt], writes=[R_imp])
                        tt(impsum[:], impsum[:], selc[:, 4 * c:4 * c + 4, :], ALU.add, reads=[R_imp, R_const], writes=[R_imp])
                        p5 = ps[5][:].bitcast(BF16)
                        for tq in range(4):
                            max8(m8[:], impsum[:, tq, :], reads=[R_imp], writes=[R_m8])
                            ts(selb[:, tq, :], impsum[:, tq, :], m8[:, 7:8], NEG, ALU.is_lt, ALU.mult, reads=[R_imp, R_m8], writes=[R_selb])
                        for tq in range(4):
                            transpose(p5[0:32, tq * 128:(tq + 1) * 128], selb[:, tq, :], ident, reads=[R_selb], writes=[R_ps[5]])
                        cp(selbT[g][:], p5[0:32, 0:512], reads=[R_ps[5]], writes=[R_selbT[g]])
                        def branch_stream(c, g, h, br, ob):
                            hh = 4 * g + h
                            j, hb = hh // 2, (hh % 2) * 64
                            kts = list(range(0, 4 * c + 4)) if br == 1 else list(range(max(0, 4 * c - 4), 4 * c + 4))
                            plan = []
                            for kt in kts:
                                dq = kt - 4 * c
                                lo = max(0, dq)
                                hi = 3 if br == 1 else min(3, dq + 4)
                                plan.append((kt, dq, lo, hi))
                            npv = sum(hi - lo + 1 for (_, _, lo, hi) in plan)
                            ipv = [0]

                            def emit_pv(item, pt):
                                kt, dq, lo, hi = item
                                for tq in range(lo, hi + 1):
                                    mm(ps[ob][:, tq * 65:(tq + 1) * 65], PT[pt][:, tq * 128:(tq + 1) * 128], Vtok[:, kt, (br - 1) * 2 + g, :],
                                       ipv[0] == 0, ipv[0] == npv - 1, reads=[R_PT[pt], R_Vtok[kt]], writes=[R_ps[ob]], skip=True)
                                    ipv[0] += 1
                            pend = None
                            for item in plan:
                                kt, dq, lo, hi = item
                                c0, c1 = lo * 128, (hi + 1) * 128
                                st = STB[nxt("st", 4)]
                                km = 2 * br + g
                                mm(ps[st][:, c0:c1], KT[hb:hb + 64, km, kt * 128:(kt + 1) * 128], QT[hb:hb + 64, j, c * 512 + c0:c * 512 + c1],
                                   True, br == 2, reads=[R_KT[km][kt // 4], R_QT[j][c]], writes=[R_ps[st]])
                                if br == 1:
                                    mm(ps[st][:, c0:c1], esel[0:32, kt * 128:(kt + 1) * 128], selbT[g][0:32, c0:c1], False, True,
                                       reads=[R_const, R_selbT[g]], writes=[R_ps[st]])
                                pt = nxt("pt", 4)
                                act(PT[pt][:, c0:c1], ps[st][:, c0:c1], AF.Exp, scale=0.125, reads=[R_ps[st]], writes=[R_PT[pt]])
                                if dq >= 0:
                                    dsl = slice(dq * 128, (dq + 1) * 128)
                                    tt(PT[pt][:, dsl], PT[pt][:, dsl], causal01, ALU.mult, reads=[R_PT[pt], R_const], writes=[R_PT[pt]])
                                if br == 2 and dq + 4 <= 3:
                                    dsl = slice((dq + 4) * 128, (dq + 5) * 128)
                                    tt(PT[pt][:, dsl], PT[pt][:, dsl], anti01, ALU.mult, reads=[R_PT[pt], R_const], writes=[R_PT[pt]])
                                yield
                                if pend is not None:
                                    emit_pv(*pend)
                                pend = (item, pt)
                            yield
                            emit_pv(*pend)
                            yield
                            evac_branch(ob, 65, c, hh, br, False)
                            yield

                        def lane(items, ob):
                            for (h, br) in items:
                                yield from branch_stream(c, g, h, br, ob)
                        interleave([lane([(0, 1), (1, 2), (2, 1), (3, 2)], 3), lane([(0, 2), (1, 1), (2, 2), (3, 1)], 4)])
                    for tq in range(4):
                        act(junk2[:], onsa[:, tq, :], AF.Square, accum_out=fst[:, tq:tq + 1], reads=[R_onsa], writes=[R_fst])
                    rstd_from_ss(fst[:, 4:8], fst[:, 0:4], 512, [R_fst])
                    tt(onbf[:], onsa[:], fst[:, 4:8].unsqueeze(2).to_broadcast([128, 4, 512]), ALU.mult, reads=[R_onsa, R_fst], writes=[R_onbf])
                    if "onsa" in dbg_d and seq == 0:
                        S.dma("sp", dbg_d["onsa"][:, c * 2048:(c + 1) * 2048], onsa[:].rearrange("p a b -> p (a b)"), reads=[R_onsa])
                    p7 = ps[7][:].bitcast(BF16)
                    for tq in range(4):
                        i = 4 * c + tq
                        for k in range(4):
                            transpose(p7[:, k * 128:(k + 1) * 128], onbf[:, tq, k * 128:(k + 1) * 128], ident, reads=[R_onbf], writes=[R_ps[7]])
                        tt(mixT[:, 0:4, i * 128:(i + 1) * 128], p7[:, 0:512].rearrange("p (k t) -> p k t", k=4),
                           nsag[:].unsqueeze(2).to_broadcast([128, 4, 128]), ALU.mult, reads=[R_ps[7], R_const], writes=[R_mix[0][i]])

            ph1.close()
            if dbg_d and seq == 0:
                rt = [psb("dbgrt", [128, 512], F32)]
                R_rt = [Res()]

                def dump(name, src3, nchunk, rlist):
                    for k in range(nchunk):
                        for q4 in range(4):
                            cp(rt[0][:], src3[:, k, q4 * 512:(q4 + 1) * 512], reads=rlist + [R_rt[0]], writes=[R_rt[0]])
                            S.dma("sp", dbg_d[name][:, k * T + q4 * 512:k * T + (q4 + 1) * 512], rt[0][:], reads=[R_rt[0]], writes=[R_rt[0]])
                if "QT" in dbg_d:
                    dump("QT", QT, 4, [x for y in R_QT for x in y])
                if "KT" in dbg_d:
                    dump("KT", KT, 6, [x for y in R_KT for x in y])
                if "mixN" in dbg_d:
                    dump("mixN", mixT, 4, R_mix[0])
        S.barrier()


        if stage >= 2 and mixers:
          with contextlib.ExitStack() as ph:
            def psb(name, shape, dt):
                return ph.enter_context(nc.sbuf_tensor(f"s_g{name}_{seq}", list(shape), dt))
            phg1 = contextlib.ExitStack()

            def psbg1(name, shape, dt):
                return phg1.enter_context(nc.sbuf_tensor(f"s_g{name}_{seq}", list(shape), dt))
            bctr2 = [0]

            def cut(n):
                if gdn_cut == n:
                    S.dead = True

            def bank():
                b_ = bctr2[0] % 8
                bctr2[0] += 1
                return b_

            gq = psb("gq", [128, 8, T], BF16)
            R_gq = [[Res() for _ in range(4)] for _ in range(8)]
            vtok = psb("vtok", [128, NT, 4, 128], BF16)
            R_vtok = [Res() for _ in range(NT)]
            zT = psb("zT", [128, 4, T], BF16)
            R_zT = [[Res() for _ in range(4)] for _ in range(4)]
            wb = [psbg1(f"wb{i}", [128, 1024], BF16) for i in range(4)]
            R_wb = [Res() for _ in range(4)]
            xbuf = [psbg1(f"xbuf{i}", [128, 515], F32) for i in range(2)]
            R_xb = [Res(), Res()]
            acc = [psbg1(f"acc{i}", [128, 512], F32) for i in range(2)]
            R_acc = [Res(), Res()]
            vtmp = psbg1("vtmp", [128, 512], BF16)
            R_vtmp = Res()
            wctr = [0]

            def load_w(chunk):
                r = wctr[0] % 4
                wctr[0] += 1
                S.dma("pool", wb[r][:], wfm_d[chunk], writes=[R_wb[r]])
                return r

            gl_ = {0: load_w(FM_GQKV + 0), 1: load_w(FM_GQKV + 1)}
            for cg in range(16):
                r = gl_.pop(cg)
                if cg + 2 < 16:
                    gl_[cg + 2] = load_w(FM_GQKV + cg + 2)
                for tb in range(4):
                    bk = bank()
                    for kc in range(8):
                        mm(ps[bk][:], wb[r][:, kc * 128:(kc + 1) * 128], hT[:, kc, tb * 512:(tb + 1) * 512], kc == 0, kc == 7,
                           reads=[R_wb[r]] + R_hT[tb * 4:tb * 4 + 4], writes=[R_ps[bk]])
                    if cg >= 12:
                        act(zT[:, cg - 12, tb * 512:(tb + 1) * 512], ps[bk][:], AF.Silu, reads=[R_ps[bk]], writes=[R_zT[cg - 12][tb]])
                        continue
                    b = tb % 2
                    if tb == 0:
                        memset(xbuf[b][:, 0:3], 0.0, writes=[R_xb[b]])
                    else:
                        cp(xbuf[b][:, 0:3], xbuf[1 - b][:, 512:515], reads=[R_xb[1 - b]], writes=[R_xb[b]], eng="pool")
                    act(xbuf[b][:, 3:515], ps[bk][:], AF.Copy, reads=[R_ps[bk]], writes=[R_xb[b]])
                    ts(acc[b][:], xbuf[b][:, 0:512], convw[:, cg, 0:1], None, ALU.mult, reads=[R_xb[b], R_const], writes=[R_acc[b]])
                    for tap in (1, 2, 3):
                        stt(acc[b][:], xbuf[b][:, tap:tap + 512], convw[:, cg, tap:tap + 1], acc[b][:], ALU.mult, ALU.add,
                            reads=[R_xb[b], R_acc[b], R_const], writes=[R_acc[b]])
                    if cg < 8:
                        act(gq[:, cg, tb * 512:(tb + 1) * 512], acc[b][:], AF.Silu, reads=[R_acc[b]], writes=[R_gq[cg][tb]])
                    else:
                        act(vtmp[:], acc[b][:], AF.Silu, reads=[R_acc[b]], writes=[R_vtmp])
                        bk2 = bank()
                        pbf = ps[bk2][:].bitcast(BF16)
                        for i4 in range(4):
                            transpose(pbf[:, i4 * 128:(i4 + 1) * 128], vtmp[:, i4 * 128:(i4 + 1) * 128], ident, reads=[R_vtmp], writes=[R_ps[bk2]])
                        cp(vtok[:, tb * 4:tb * 4 + 4, cg - 8, :], pbf[:, 0:512].rearrange("p (a b) -> p a b", a=4), reads=[R_ps[bk2]],
                           writes=R_vtok[tb * 4:tb * 4 + 4])
            cut(1)
            sqb = [psbg1(f"sqb{i}", [128, 512], BF16) for i in range(2)]
            R_sq = [Res(), Res()]
            lnb = [psbg1(f"lnb{i}", [128, 512], F32) for i in range(2)]
            R_ln = [Res(), Res()]
            it = 0
            for cg in range(8):
                for tb in range(4):
                    b = it % 2
                    it += 1
                    src = gq[:, cg, tb * 512:(tb + 1) * 512]
                    tt(sqb[b][:], src, src, ALU.mult, reads=[R_gq[cg][tb]], writes=[R_sq[b]], eng="pool")
                    bk = bank()
                    mm(ps[bk][:], ones_bf, sqb[b][:], True, True, reads=[R_sq[b], R_const], writes=[R_ps[bk]])
                    act(lnb[b][:], ps[bk][:], AF.Ln, bias=eps_ap, reads=[R_ps[bk]], writes=[R_ln[b]])
                    act(lnb[b][:], lnb[b][:], AF.Exp, scale=-0.5, reads=[R_ln[b]], writes=[R_ln[b]])
                    tt(src, src, lnb[b][:], ALU.mult, reads=[R_gq[cg][tb], R_ln[b]], writes=[R_gq[cg][tb]])

            cut(2)
            phg1.close()
            S.barrier()
            def t4(name, dt=BF16):
                return psb(name, [128, 4, 128], dt)
            Sst = t4("Sst", F32)
            Stmp = t4("Stmp", F32)
            Sbf = [t4("Sbf0"), t4("Sbf1")]
            R_S, R_Stmp, R_Sbf = Res(), Res(), [Res(), Res()]
            memset(Sst[:], 0.0, writes=[R_S])
            memset(Sbf[0][:], 0.0, writes=[R_Sbf[0]])
            sbi = [0]
            NSET = 3
            B = []
            for k_ in range(NSET):
                d_ = {}
                for nm, dt_ in (("EmA", BF16), ("EmQ", BF16), ("X0", BF16), ("X1", BF16), ("Y0", BF16), ("Y1", BF16), ("M0", BF16), ("M1", BF16),
                                ("qkT", BF16), ("kbg", BF16), ("kdd", BF16), ("vb", BF16), ("wTn", BF16)):
                    d_[nm] = t4(f"{nm}_{k_}", dt_)
                    d_["R_" + nm] = Res()
                d_["gs"] = psb(f"gs_{k_}", [128, 48], F32)
                d_["R_gs"] = Res()
                B.append(d_)
            shared = {}
            for nm, dt_ in (("Lg", F32),):
                shared[nm] = t4(nm, dt_)
                shared["R_" + nm] = Res()
            for d_ in B:
                d_.update(shared)
            vnew = t4("vnew")
            osb = t4("osb", F32)
            ofin = t4("ofin", F32)
            onb = t4("onb")
            R_vnew, R_osb, R_ofin, R_onb = [Res() for _ in range(4)]
            id4 = ident4.rearrange("p (a b) -> p a b", a=4)
            Mfin = {}

            def b4(ap):
                return ap.unsqueeze(2).to_broadcast([128, 4, 128])

            def v4(bk):
                return ps[bk][:].rearrange("p (a b) -> p a b", a=4)

            def pre_tile(i):
                D_ = B[i % NSET]
                gs, R_gs = D_["gs"], D_["R_gs"]
                Lg, EmA, EmQ, qkT, kbg, kdd, vb, wTn = (D_[k] for k in ("Lg", "EmA", "EmQ", "qkT", "kbg", "kdd", "vb", "wTn"))
                R_Lg, R_EmA, R_EmQ, R_qkT, R_kbg, R_kdd, R_vb, R_wTn = (D_["R_" + k] for k in ("Lg", "EmA", "EmQ", "qkT", "kbg", "kdd", "vb", "wTn"))
                Xb, Yb, Mb = [D_["X0"], D_["X1"]], [D_["Y0"], D_["Y1"]], [D_["M0"], D_["M1"]]
                R_X, R_Y, R_M = [D_["R_X0"], D_["R_X1"]], [D_["R_Y0"], D_["R_Y1"]], [D_["R_M0"], D_["R_M1"]]
                tsl = slice(i * 128, (i + 1) * 128)
                g_i = g_all[:, i, :]
                be_i = beta_all[:, i, :]
                Rk = [R_gq[4 + h][i // 4] for h in range(4)]
                Rq = [R_gq[h][i // 4] for h in range(4)]
                bk = bank()
                mm(ps[bk][:, 0:4], ltriT, g_i, True, True, reads=[R_const, R_bg[i]], writes=[R_ps[bk]])
                mm(ps[bk][:, 4:8], bones, g_i, True, True, reads=[R_const, R_bg[i]], writes=[R_ps[bk]])
                cp(gs[:, 0:4], ps[bk][:, 0:4], reads=[R_ps[bk]], writes=[R_gs])
                ts(gs[:, 4:8], be_i, -1.0, None, ALU.mult, reads=[R_bg[i]], writes=[R_gs])
                act(gs[:, 8:12], gs[:, 0:4], AF.Exp, reads=[R_gs], writes=[R_gs])
                tt(gs[:, 12:16], gs[:, 8:12], be_i, ALU.mult, reads=[R_gs, R_bg[i]], writes=[R_gs])
                tt(gs[:, 16:20], ps[bk][:, 4:8], gs[:, 0:4], ALU.subtract, reads=[R_ps[bk], R_gs], writes=[R_gs])
                act(gs[:, 16:20], gs[:, 16:20], AF.Exp, reads=[R_gs], writes=[R_gs])
                ts(gs[:, 20:24], gs[:, 0:4], -1.0, None, ALU.mult, reads=[R_gs], writes=[R_gs])
                tt(gs[:, 24:32].rearrange("p (h j) -> p h j", h=4), g_i.unsqueeze(2).to_broadcast([128, 4, 2]),
                   cind.unsqueeze(1).to_broadcast([128, 4, 2]), ALU.mult, reads=[R_bg[i], R_const], writes=[R_gs])
                tt(Lg[:], ltriT.unsqueeze(1).to_broadcast([128, 4, 128]), b4(g_i), ALU.mult, reads=[R_const, R_bg[i]], writes=[R_Lg], eng="pool")
                bk = bank()
                mm(ps[bk][:, 0:8], ones_f, gs[:, 24:32], True, True, reads=[R_const, R_gs], writes=[R_ps[bk]])
                act(gs[:, 32:40], ps[bk][:, 0:8], AF.Exp, reads=[R_ps[bk]], writes=[R_gs])
                bA = bank()
                mm(ps[bA][:], ones_f, Lg[:].rearrange("p a b -> p (a b)"), True, False, reads=[R_const, R_Lg], writes=[R_ps[bA]])
                mm(ps[bA][:], ident, maskA4, False, True, reads=[R_const], writes=[R_ps[bA]])
                for h in range(4):
                    act(EmA[:, h, :], ps[bA][:, h * 128:(h + 1) * 128], AF.Exp, bias=gs[:, h:h + 1], scale=-1.0, reads=[R_ps[bA], R_gs], writes=[R_EmA])
                bQ = bank()
                mm(ps[bQ][:], ones_f, Lg[:].rearrange("p a b -> p (a b)"), True, False, reads=[R_const, R_Lg], writes=[R_ps[bQ]])
                mm(ps[bQ][:], ident, maskQ4, False, True, reads=[R_const], writes=[R_ps[bQ]])
                for h in range(4):
                    act(EmQ[:, h, :], ps[bQ][:, h * 128:(h + 1) * 128], AF.Exp, bias=gs[:, 20 + h:21 + h], scale=1.0, reads=[R_ps[bQ], R_gs], writes=[R_EmQ])
                yield
                bK = bank()
                for h in range(4):
                    kT = gq[:, 4 + h, tsl]
                    mm(ps[bK][:, h * 128:(h + 1) * 128], kT, kT, True, True, reads=[Rk[h]], writes=[R_ps[bK]])
                for h in range(4):
                    stt(Xb[0][:, h, :], ps[bK][:, h * 128:(h + 1) * 128], gs[:, 4 + h:5 + h], EmA[:, h, :], ALU.mult, ALU.mult,
                        reads=[R_ps[bK], R_gs, R_EmA], writes=[R_X[0]])
                yield
                bKQ = bank()
                for h in range(4):
                    mm(ps[bKQ][:, h * 128:(h + 1) * 128], gq[:, 4 + h, tsl], gq[:, h, tsl], True, True, reads=[Rk[h], Rq[h]], writes=[R_ps[bKQ]])
                tt(qkT[:], v4(bKQ), EmQ[:], ALU.mult, reads=[R_ps[bKQ], R_EmQ], writes=[R_qkT])
                bT = bank()
                pT = ps[bT][:].bitcast(BF16)
                for h in range(4):
                    transpose(pT[:, h * 128:(h + 1) * 128], gq[:, 4 + h, tsl], ident, reads=[Rk[h]], writes=[R_ps[bT]])
                pT4 = pT[:, 0:512].rearrange("p (a b) -> p a b", a=4)
                tt(kbg[:], pT4, b4(gs[:, 12:16]), ALU.mult, reads=[R_ps[bT], R_gs], writes=[R_kbg])
                tt(kdd[:], pT4, b4(gs[:, 16:20]), ALU.mult, reads=[R_ps[bT], R_gs], writes=[R_kdd])
                tt(vb[:], vtok[:, i, :, :], b4(be_i), ALU.mult, reads=[R_vtok[i], R_bg[i]], writes=[R_vb], eng="pool")
                yield
                bT = bank()
                pT = ps[bT][:].bitcast(BF16)
                for h in range(4):
                    transpose(pT[:, h * 128:(h + 1) * 128], Xb[0][:, h, :], ident, reads=[R_X[0]], writes=[R_ps[bT]])
                pT4 = pT[:, 0:512].rearrange("p (a b) -> p a b", a=4)
                act(Yb[0][:], pT4, AF.Copy, reads=[R_ps[bT]], writes=[R_Y[0]])
                tt(Mb[0][:], Yb[0][:], id4, ALU.add, reads=[R_Y[0], R_const], writes=[R_M[0]])
                yield
                xc, yc, mc = 0, 0, 0
                for lvl in range(5):
                    xn, yn, mn = 1 - xc, 1 - yc, 1 - mc
                    bX = bank()
                    for h in range(4):
                        mm(ps[bX][:, h * 128:(h + 1) * 128], Yb[yc][:, h, :], Xb[xc][:, h, :], True, True, reads=[R_Y[yc], R_X[xc]], writes=[R_ps[bX]])
                    act(Xb[xn][:], v4(bX), AF.Copy, reads=[R_ps[bX]], writes=[R_X[xn]])
                    if lvl < 4:
                        bY = bank()
                        for h in range(4):
                            mm(ps[bY][:, h * 128:(h + 1) * 128], Xb[xc][:, h, :], Yb[yc][:, h, :], True, True, reads=[R_Y[yc], R_X[xc]], writes=[R_ps[bY]])
                        act(Yb[yn][:], v4(bY), AF.Copy, reads=[R_ps[bY]], writes=[R_Y[yn]])
                    yield
                    bM = bank()
                    for h in range(4):
                        mm(ps[bM][:, h * 128:(h + 1) * 128], Xb[xn][:, h, :], Mb[mc][:, h, :], True, True, reads=[R_X[xn], R_M[mc]], writes=[R_ps[bM]])
                    tt(Mb[mn][:], v4(bM), Mb[mc][:], ALU.add, reads=[R_ps[bM], R_M[mc]], writes=[R_M[mn]])
                    xc, yc, mc = xn, (yn if lvl < 4 else yc), mn
                    yield
                M, R_Mf = Mb[mc], R_M[mc]
                Mfin[i] = (M, R_Mf)
                bW = bank()
                for h in range(4):
                    mm(ps[bW][:, h * 128:(h + 1) * 128], kbg[:, h, :], M[:, h, :], True, True, reads=[R_kbg, R_Mf], writes=[R_ps[bW]])
                act(wTn[:], v4(bW), AF.Copy, scale=-1.0, reads=[R_ps[bW]], writes=[R_wTn])
                yield

            def scan_tile(i):
                D_ = B[i % NSET]
                gs, R_gs = D_["gs"], D_["R_gs"]
                qkT, kdd, vb, wTn = (D_[k] for k in ("qkT", "kdd", "vb", "wTn"))
                R_qkT, R_kdd, R_vb, R_wTn = (D_["R_" + k] for k in ("qkT", "kdd", "vb", "wTn"))
                M, R_Mf = Mfin[i]
                tsl = slice(i * 128, (i + 1) * 128)
                Rq = [R_gq[h][i // 4] for h in range(4)]
                decS = gs[:, 32:40].rearrange("p (h j) -> p h j", h=4)
                for jc in range(2):
                    rows = slice(jc * 64, (jc + 1) * 64)
                    so, sn = sbi[0], 1 - sbi[0]
                    bV = bank()
                    for h in range(4):
                        mm(ps[bV][:, h * 128:(h + 1) * 128], M[:, h, :], vb[:, h, :], True, False, reads=[R_Mf, R_vb], writes=[R_ps[bV]])
                        mm(ps[bV][:, h * 128:(h + 1) * 128], wTn[:, h, :], Sbf[so][:, h, :], False, True, reads=[R_wTn, R_Sbf[so]], writes=[R_ps[bV]])
                    act(vnew[rows, :, :], ps[bV][rows, :].rearrange("p (a b) -> p a b", a=4), AF.Copy, reads=[R_ps[bV]], writes=[R_vnew])
                    tt(Stmp[:], Sst[:], decS[:, :, jc].unsqueeze(2).to_broadcast([128, 4, 128]), ALU.mult, reads=[R_S, R_gs], writes=[R_Stmp])
                    yield
                    bO = bank()
                    for h in range(4):
                        mm(ps[bO][:, h * 128:(h + 1) * 128], gq[:, h, tsl], Sbf[so][:, h, :], True, True, reads=[Rq[h], R_Sbf[so]], writes=[R_ps[bO]])
                    tt(osb[rows, :, :], ps[bO][rows, :].rearrange("p (a b) -> p a b", a=4), gs[rows, 8:12].unsqueeze(2).to_broadcast([64, 4, 128]),
                       ALU.mult, reads=[R_ps[bO], R_gs], writes=[R_osb])
                    yield
                    bD = bank()
                    for h in range(4):
                        mm(ps[bD][:, h * 128:(h + 1) * 128], kdd[rows, h, :], vnew[rows, h, :], True, True, reads=[R_kdd, R_vnew], writes=[R_ps[bD]])
                    tt(Sbf[sn][:], v4(bD), Stmp[:], ALU.add, reads=[R_ps[bD], R_Stmp], writes=[R_Sbf[sn]])
                    tt(Sst[:], v4(bD), Stmp[:], ALU.add, reads=[R_ps[bD], R_Stmp], writes=[R_S])
                    sbi[0] = sn
                    yield
                bO2 = bank()
                for h in range(4):
                    mm(ps[bO2][:, h * 128:(h + 1) * 128], qkT[:, h, :], vnew[:, h, :], True, True, reads=[R_qkT, R_vnew], writes=[R_ps[bO2]])
                tt(ofin[:], v4(bO2), osb[:], ALU.add, reads=[R_ps[bO2], R_osb], writes=[R_ofin])
                for h in range(4):
                    act(junk2[:, 0:128], ofin[:, h, :], AF.Square, accum_out=gs[:, 40 + h:41 + h], reads=[R_ofin], writes=[R_gs])
                act(gs[:, 44:48], gs[:, 40:44], AF.Ln, bias=eps128_ap, scale=1.0 / 128, reads=[R_gs], writes=[R_gs])
                act(gs[:, 44:48], gs[:, 44:48], AF.Exp, scale=-0.5, reads=[R_gs], writes=[R_gs])
                tt(onb[:], ofin[:], b4(gs[:, 44:48]), ALU.mult, reads=[R_ofin, R_gs], writes=[R_onb])
                if "ogdn" in dbg_d and seq == 0:
                    S.dma("sp", dbg_d["ogdn"][:, i * 512:(i + 1) * 512], ofin[:].rearrange("p a b -> p (a b)"), reads=[R_ofin])
                yield
                bT = bank()
                pT = ps[bT][:].bitcast(BF16)
                for h in range(4):
                    transpose(pT[:, h * 128:(h + 1) * 128], onb[:, h, :], ident, reads=[R_onb], writes=[R_ps[bT]])
                stt(mixT[:, 4:8, tsl], pT[:, 0:512].rearrange("p (a b) -> p a b", a=4), gng[:, 0:1], zT[:, :, tsl], ALU.mult, ALU.mult,
                    reads=[R_ps[bT], R_const] + [R_zT[h][i // 4] for h in range(4)], writes=[R_mix[1][i]])
                yield

            pre_done = set()
            scan_done = -1
            active_pre = {}
            cur_scan, cur_i, next_pre = None, 0, 0
            while cur_i < NT:
                while next_pre < NT and len(active_pre) < NSET - 1 and next_pre - NSET <= scan_done:
                    active_pre[next_pre] = pre_tile(next_pre)
                    next_pre += 1
                if cur_scan is None and cur_i in pre_done:
                    cur_scan = scan_tile(cur_i)
                if cur_scan is not None:
                    try:
                        next(cur_scan)
                    except StopIteration:
                        scan_done = cur_i
                        cur_i += 1
                        cur_scan = None
                for k_ in list(active_pre):
                    try:
                        next(active_pre[k_])
                    except StopIteration:
                        pre_done.add(k_)
                        del active_pre[k_]
            if "mixG" in dbg_d and seq == 0:
                for k in range(4):
                    for q4 in range(4):
                        cp(ofin[:].rearrange("p a b -> p (a b)"), mixT[:, 4 + k, q4 * 512:(q4 + 1) * 512], reads=R_mix[1] + [R_ofin], writes=[R_ofin])
                        S.dma("sp", dbg_d["mixG"][:, k * T + q4 * 512:k * T + (q4 + 1) * 512], ofin[:].rearrange("p a b -> p (a b)"), reads=[R_ofin], writes=[R_ofin])
          S.dead = False
          S.barrier()


        if stage >= 3:
          with contextlib.ExitStack() as ph:
            def psb(name, shape, dt):
                return ph.enter_context(nc.sbuf_tensor(f"s_o{name}_{seq}", list(shape), dt))
            wo = psb("wo", [128, 8, D], BF16)
            R_wo = Res()
            for q4 in range(4):
                S.dma("pool", wo[:, 2 * q4:2 * q4 + 2, :].rearrange("p a b -> p (a b)"), wout_d[:, q4 * 2048:(q4 + 1) * 2048], writes=[R_wo])
            xt = [psb(f"xt{i}", [128, D], F32) for i in range(2)]
            x2t = [psb(f"x2t{i}", [128, D], F32) for i in range(2)]
            R_xt, R_x2t = [Res(), Res()], [Res(), Res()]
            hn = psb("hn", [128, D], BF16)
            R_hn = Res()
            junk = psb("junk", [128, D], BF16)
            stat = psb("stat", [128, 8], F32)
            R_stat = Res()
            pend_fin = []
            for i in range(NT):
                b = i % 2
                tsl = slice(i * 128, (i + 1) * 128)
                S.dma("sp", xt[b][:], x_d[seq, tsl, :], writes=[R_xt[b]])
                for half in range(2):
                    bk = (2 * i + half) % 6
                    for kc in range(8):
                        mm(ps[bk][:], mixT[:, kc, tsl], wo[:, kc, half * 512:(half + 1) * 512], kc == 0, kc == 7,
                           reads=[R_mix[kc // 4][i], R_wo], writes=[R_ps[bk]])
                    tt(x2t[b][:, half * 512:(half + 1) * 512], ps[bk][:], xt[b][:, half * 512:(half + 1) * 512], ALU.add,
                       reads=[R_ps[bk], R_xt[b]], writes=[R_x2t[b]])
                if pend_fin:
                    pend_fin.pop(0)()
                S.dma("sp", x2_d[seq, tsl, :], x2t[b][:], reads=[R_x2t[b]], writes=[R_x2s[i]])
                act(junk[:], x2t[b][:], AF.Square, accum_out=stat[:, 0:1], reads=[R_x2t[b]], writes=[R_stat])
                rstd_from_ss(stat[:, 1:2], stat[:, 0:1], D, [R_stat])
                ts(hn[:], x2t[b][:], stat[:, 1:2], None, ALU.mult, reads=[R_x2t[b], R_stat], writes=[R_hn])
                def fin_(tsl=tsl, i=i):
                    pb = ps[7][:].bitcast(BF16)
                    for k in range(8):
                        transpose(pb[:, k * 128:(k + 1) * 128], hn[:, k * 128:(k + 1) * 128], ident, reads=[R_hn], writes=[R_ps[7]])
                    tt(hT[:, :, tsl], pb.rearrange("p (k t) -> p k t", k=8), g2T[:].unsqueeze(2).to_broadcast([128, 8, 128]), ALU.mult,
                       reads=[R_ps[7], R_const], writes=[R_hT[i]])
                pend_fin.append(fin_)
            pend_fin.pop(0)()
          S.barrier()

        if stage >= 4:
          with contextlib.ExitStack() as ph:
            def psb(name, shape, dt):
                return ph.enter_context(nc.sbuf_tensor(f"s_f{name}_{seq}", list(shape), dt))
            actT = psb("actT", [128, NFC, 1024], BF16)
            R_act = [[Res() for _ in range(2)] for _ in range(NFC)]
            wd = psb("wd", [128, NFC, D], BF16)
            R_wd = [Res() for _ in range(NFC)]
            wgu = [psb(f"wgu{i}", [128, 1024], BF16) for i in range(6)]
            R_wgu = [Res() for _ in range(6)]
            sgt = [psb(f"sgt{i}", [128, 512], BF16) for i in range(2)]
            R_sgt = [Res(), Res()]
            x2t = [psb(f"x2t{i}", [128, D], F32) for i in range(2)]
            x3t = [psb(f"x3t{i}", [128, D], F32) for i in range(2)]
            R_x2t, R_x3t = [Res(), Res()], [Res(), Res()]
            junk = psb("junk", [128, D], BF16)
            stat = psb("stat", [128, 8], F32)
            R_stat = Res()
            wi = 0
            si = 0
            for th in range(2):
                for fc in range(NFC):
                    rg = wi % 6
                    ru = (wi + 1) % 6
                    wi += 2
                    S.dma("pool", wgu[rg][:], wg_d[fc], writes=[R_wgu[rg]])
                    S.dma("pool", wgu[ru][:], wu_d[fc], writes=[R_wgu[ru]])
                    if th == 0:
                        S.dma("pool", wd[:, fc, :], wd_d[:, fc * D:(fc + 1) * D], writes=[R_wd[fc]])
                    for tb in range(2):
                        t0 = th * 1024 + tb * 512
                        bg_, bu_ = (4 * fc + 2 * tb) % 6, (4 * fc + 2 * tb + 1) % 6
                        for kc in range(8):
                            mm(ps[bg_][:], wgu[rg][:, kc * 128:(kc + 1) * 128], hT[:, kc, t0:t0 + 512], kc == 0, kc == 7,
                               reads=[R_wgu[rg]] + R_hT[t0 // 128:t0 // 128 + 4], writes=[R_ps[bg_]])
                        for kc in range(8):
                            mm(ps[bu_][:], wgu[ru][:, kc * 128:(kc + 1) * 128], hT[:, kc, t0:t0 + 512], kc == 0, kc == 7,
                               reads=[R_wgu[ru]] + R_hT[t0 // 128:t0 // 128 + 4], writes=[R_ps[bu_]])
                        sb_ = si % 2
                        si += 1
                        act(sgt[sb_][:], ps[bg_][:], AF.Silu, reads=[R_ps[bg_]], writes=[R_sgt[sb_]])
                        tt(actT[:, fc, tb * 512:(tb + 1) * 512], ps[bu_][:], sgt[sb_][:], ALU.mult, reads=[R_ps[bu_], R_sgt[sb_]], writes=[R_act[fc][tb]])
                for tl in range(8):
                    i = th * 8 + tl
                    b = i % 2
                    tsl = slice(i * 128, (i + 1) * 128)
                    S.dma("sp", x2t[b][:], x2_d[seq, tsl, :], reads=[R_x2s[i]], writes=[R_x2t[b]])
                    for half in range(2):
                        bk = 6 + half
                        for fc in range(NFC):
                            mm(ps[bk][:], actT[:, fc, tl * 128:(tl + 1) * 128], wd[:, fc, half * 512:(half + 1) * 512], fc == 0, fc == NFC - 1,
                               reads=[R_act[fc][tl // 4], R_wd[fc]], writes=[R_ps[bk]])
                        tt(x3t[b][:, half * 512:(half + 1) * 512], ps[bk][:], x2t[b][:, half * 512:(half + 1) * 512], ALU.add,
                           reads=[R_ps[bk], R_x2t[b]], writes=[R_x3t[b]])
                    act(junk[:], x3t[b][:], AF.Square, accum_out=stat[:, 0:1], reads=[R_x3t[b]], writes=[R_stat])
                    rstd_from_ss(stat[:, 1:2], stat[:, 0:1], D, [R_stat])
                    stt(x3t[b][:], x3t[b][:], stat[:, 1:2], fgb[:], ALU.mult, ALU.mult, reads=[R_x3t[b], R_stat, R_const], writes=[R_x3t[b]])
                    S.dma("sp", out_d[seq, tsl, :], x3t[b][:], reads=[R_x3t[b]], writes=[R_x3t[b]])
          S.barrier()

    S.barrier()
    S.run()
    es.close()
    return nc


def _bf(a):
    return np.asarray(a, dtype=np.float32).astype(ml_dtypes.bfloat16)


def _tile_w(w):
    return np.ascontiguousarray(w.reshape(8, 128, -1).transpose(1, 0, 2))


def _consts():
    c = {}
    ident = np.eye(128, dtype=np.float32)
    ones = np.ones((128, 128), np.float32)
    k = np.arange(128)[:, None]
    t = np.arange(128)[None, :]
    causal = np.where(k <= t, 0.0, NEG).astype(np.float32)
    anti = np.where(k > t, 0.0, NEG).astype(np.float32)
    same = (k // 64) == (t // 64)
    maskA = np.where(same & (t < k), 0.0, -NEG).astype(np.float32)
    maskQ = np.where(same & (t >= k), 0.0, NEG).astype(np.float32)
    n = np.arange(128)[:, None]
    s = np.arange(32)[None, :]
    ovl = ((16 * n < 64 * s + 64) & (16 * n + 32 > 64 * s) & (n < 127)).astype(np.float32)
    esel = np.zeros((128, 2048), np.float32)
    esel[:32] = (np.arange(2048)[None, :] // 64 == np.arange(32)[:, None]).astype(np.float32)
    tt = np.arange(2048)[None, :]
    cmpmask = np.where((16 * n + 31 <= tt) & (n < 127), 0.0, NEG).astype(np.float32)
    c["cbf"] = _bf(np.concatenate([ident, ones, causal, anti, np.tile(maskA, (1, 4)), np.tile(maskQ, (1, 4)), ovl, esel, cmpmask, np.tile(ident, (1, 4)), (k <= t).astype(np.float32), (k > t).astype(np.float32)], axis=1))
    assert c["cbf"].shape[1] == NCBF
    ltriT = (same & (k <= t)).astype(np.float32)
    bones = same.astype(np.float32)
    cind = (np.arange(128)[:, None] // 64 == np.arange(2)[None, :]).astype(np.float32)
    c["cf32"] = np.ascontiguousarray(np.concatenate([ltriT, bones, ones, cind], axis=1))
    tq = np.arange(2048)[:, None]
    cur = tq // 64
    j = np.arange(32)[None, :]
    forced = (j == 0) | (j == cur) | (j == cur - 1)
    caus = j <= cur
    selb = np.where(forced, 1e9, np.where(caus, 0.0, -1e30)).astype(np.float32)
    c["selc"] = np.ascontiguousarray(selb.reshape(16, 128, 32).transpose(1, 0, 2).reshape(128, 16 * 32))
    inv = (500000.0 ** (-np.arange(8, dtype=np.float64) * (2.0 / 16))) / (2 * np.pi)
    rc = np.zeros((128, 4), np.float32)
    for base in (0, 64):
        rc[base:base + 8, 0] = -inv
        rc[base + 8:base + 16, 0] = inv
        rc[base:base + 8, 1] = inv
        rc[base + 8:base + 16, 1] = inv
    rc[:, 2] = 0.25
    c["ropec"] = rc
    return c


def _swap_cols(wq, nheads):
    out = np.zeros_like(wq)
    for h in range(nheads):
        b = h * 64
        out[:, b:b + 8] = wq[:, b + 8:b + 16]
        out[:, b + 8:b + 16] = wq[:, b:b + 8]
    return out


def prep_shared(inp):
    w_in = np.asarray(inp["w_in"], np.float32)[0]
    sh = dict(_consts())
    cols = []
    wq = w_in[:, 0:512]
    cols.append(wq)
    cols.append(_swap_cols(wq, 8))
    kd, ksd = [], []
    for kind in (0, 2, 4):
        for g in range(2):
            o = OFF_KV + kind * 128 + g * 64
            wk = w_in[:, o:o + 64]
            kd.append(np.concatenate([wk, wk], axis=1))
            ws = _swap_cols(wk, 1)
            ksd.append(np.concatenate([ws, ws], axis=1))
    cols += kd + ksd
    cols.append(w_in[:, OFF_KV + 128:OFF_KV + 256])
    cols.append(w_in[:, OFF_GQKV:OFF_GQKV + 1536])
    cols.append(w_in[:, OFF_GZ:OFF_GZ + 512])
    wfm = np.concatenate(cols, axis=1)
    assert wfm.shape[1] == NFM * 128
    wt = _tile_w(wfm)
    sh["wfm"] = np.ascontiguousarray(wt.reshape(128, 8, NFM, 128).transpose(2, 0, 1, 3).reshape(NFM, 128, 1024))
    wtm = np.concatenate([w_in[:, OFF_KV + 3 * 128:OFF_KV + 4 * 128], w_in[:, OFF_KV + 5 * 128:OFF_KV + 6 * 128],
                          w_in[:, OFF_GATE:OFF_GATE + 24], w_in[:, OFF_GB:OFF_GB + 4], w_in[:, OFF_GA:OFF_GA + 4]], axis=1)
    assert wtm.shape[1] == NTM
    sh["wtm"] = np.ascontiguousarray(_tile_w(wtm).reshape(128, 8 * NTM))
    sh["g1"] = np.ascontiguousarray(np.asarray(inp["norm1_g"], np.float32)[0].reshape(8, 128).T)
    w1 = np.asarray(inp["cmp_w1"], np.float32)[0]
    w1t = w1.reshape(2, 32, 64, 128).transpose(2, 0, 1, 3).reshape(64, 2 * 32 * 128)
    sh["w1"] = np.ascontiguousarray(np.concatenate([w1t, w1t], axis=0))
    w2 = np.asarray(inp["cmp_w2"], np.float32)[0]
    sh["w2"] = np.ascontiguousarray(np.concatenate([w2[0], w2[0], w2[1]], axis=1))
    cp = np.asarray(inp["cmp_pos"], np.float32)[0]
    sh["posT"] = np.ascontiguousarray(cp.transpose(2, 0, 1).reshape(64, 64))
    sh["gdnc"] = np.ascontiguousarray(np.concatenate([np.asarray(inp["gdn_a_log"], np.float32)[0], np.asarray(inp["gdn_dt_bias"], np.float32)[0]]).reshape(1, 8))
    cw = np.asarray(inp["gdn_conv_w"], np.float32)[0]
    sh["convw"] = np.ascontiguousarray(cw.reshape(4, 12, 128).transpose(2, 1, 0).reshape(128, 48))
    sh["gng"] = np.ascontiguousarray(np.asarray(inp["gdn_norm_g"], np.float32)[0].reshape(128, 1))
    sh["wout"] = np.ascontiguousarray(_tile_w(np.asarray(inp["w_out"], np.float32)[0]).reshape(128, 8 * D))
    sh["g2"] = np.ascontiguousarray(np.asarray(inp["norm2_g"], np.float32)[0].reshape(8, 128).T)
    sh["fg"] = np.ascontiguousarray(np.asarray(inp["final_g"], np.float32).reshape(1, D))
    for nm, key in (("wg", "w_gate"), ("wu", "w_up")):
        wt_ = _tile_w(np.asarray(inp[key], np.float32)[0])
        sh[nm] = np.ascontiguousarray(wt_.reshape(128, 8, NFC, 128).transpose(2, 0, 1, 3).reshape(NFC, 128, 1024))
    wdn = np.asarray(inp["w_down"], np.float32)[0]
    sh["wd"] = np.ascontiguousarray(wdn.reshape(NFC, 128, D).transpose(1, 0, 2).reshape(128, NFC * D))
    sh["nsag"] = np.ascontiguousarray(np.asarray(inp["nsa_norm_g"], np.float32)[0].reshape(4, 128).T)
    return sh


def kernel(**inputs):
    x = np.asarray(inputs["x"], np.float32)
    pos = np.asarray(inputs["positions"], np.int32)
    sh = prep_shared(inputs)
    nc = build_program()
    in_maps = []
    for c in range(NCORES):
        m = dict(sh)
        m["x"] = np.ascontiguousarray(x[c * NSEQ:(c + 1) * NSEQ])
        m["pos"] = np.ascontiguousarray(pos[c * NSEQ:(c + 1) * NSEQ])
        in_maps.append(m)
    res = run_bass_kernel_spmd(nc, in_maps, core_ids=list(range(NCORES)))
    return np.concatenate([r["out"] for r in res.results], axis=0)
```

```python
import contextlib
import numpy as np
import ml_dtypes
import concourse.bass as bass
import concourse.mybir as mybir
from concourse.bass_utils import run_bass_kernel_spmd

F32 = mybir.dt.float32
BF16 = mybir.dt.bfloat16
I32 = mybir.dt.int32
AF = mybir.ActivationFunctionType
ALU = mybir.AluOpType
AX = mybir.AxisListType

NCORES = 8
NSEQ = 2
T = 2048
D = 1024
NT = T // 128
DFF = 2816
NFC = DFF // 128
NEG = -30000.0
EPS = 1e-6
OFF_KV = 512
OFF_GATE = OFF_KV + 768
OFF_GQKV = OFF_GATE + 24
OFF_GZ = OFF_GQKV + 1536
OFF_GB = OFF_GZ + 512
OFF_GA = OFF_GB + 4
FM_Q, FM_QSW, FM_K, FM_KSW, FM_VC, FM_GQKV, FM_Z = 0, 4, 8, 14, 20, 21, 33
NFM = 37
NTM = 288
NCBF = 1568 + 2048 + 2048 + 512 + 256


class Res:
    __slots__ = ("name", "w", "r")

    def __init__(self, name=""):
        self.name = name
        self.w = {}
        self.r = {}


class Sched:
    ENGS = ("pe", "act", "dve", "pool", "sp")

    def __init__(self, nc, ring=6):
        self.nc = nc
        self.prog = {k: [] for k in self.ENGS}
        self.esem = {k: nc.alloc_semaphore(name=f"es_{k}") for k in self.ENGS}
        self.ecnt = {k: 0 for k in self.ENGS}
        self.known = {k: {} for k in self.ENGS}
        self.R = ring
        self.rsem = {q: [nc.alloc_semaphore(name=f"rs_{q}{i}") for i in range(ring)] for q in ("sp", "pool", "act")}
        self.rcnt = {q: [0] * ring for q in self.rsem}
        self.rpos = {q: 0 for q in self.rsem}
        self.n_wait = 0
        self.dead = False

    def _collect(self, reads, writes):
        deps = {}

        def add(d):
            for k, (s, v) in d.items():
                if k not in deps or deps[k][1] < v:
                    deps[k] = (s, v)

        for r in reads:
            add(r.w)
        for w in writes:
            add(w.w)
            add(w.r)
        return deps

    def _emit_waits(self, eng, deps):
        kn = self.known[eng]
        for k, (s, v) in deps.items():
            if kn.get(k, 0) < v:
                kn[k] = v
                self.n_wait += 1
                self.prog[eng].append(lambda e, s=s, v=v: e.wait_ge(s, v))

    def op(self, eng, fn, reads=(), writes=()):
        if self.dead:
            return
        deps = self._collect(reads, writes)
        if eng in deps and eng == "pe":
            del deps[eng]
        self._emit_waits(eng, deps)
        self.ecnt[eng] += 1
        cnt = self.ecnt[eng]
        sem = self.esem[eng]
        self.prog[eng].append(lambda e, fn=fn, sem=sem: fn(e).then_inc(sem, 1))
        tok = (sem, cnt)
        for r in reads:
            r.r[eng] = tok
        for w in writes:
            w.w = {eng: tok}
            w.r = {}

    def dma(self, q, out, in_, reads=(), writes=()):
        if self.dead:
            return
        j = self.rpos[q] % self.R
        self.rpos[q] += 1
        sem = self.rsem[q][j]
        key = ("ring", q, j)
        deps = self._collect(reads, writes)
        if self.rcnt[q][j] > 0:
            deps[key] = (sem, 16 * self.rcnt[q][j])
        self._emit_waits(q, deps)
        self.rcnt[q][j] += 1
        val = 16 * self.rcnt[q][j]
        self.prog[q].append(lambda e, out=out, in_=in_, sem=sem: e.dma_start(out=out, in_=in_).then_inc(sem, 16))
        tok = (sem, val)
        for r in reads:
            r.r[key] = tok
        for w in writes:
            w.w = {key: tok}
            w.r = {}

    def barrier(self):
        deps = {}
        for k in self.ENGS:
            if self.ecnt[k]:
                deps[k] = (self.esem[k], self.ecnt[k])
        for q in self.rsem:
            for j in range(self.R):
                if self.rcnt[q][j]:
                    deps[("ring", q, j)] = (self.rsem[q][j], 16 * self.rcnt[q][j])
        for e in self.ENGS:
            d = dict(deps)
            d.pop(e, None)
            self._emit_waits(e, d)

    def run(self):
        nc = self.nc
        with nc.Block() as block:
            @block.tensor
            def _(e):
                for f in self.prog["pe"]:
                    f(e)

            @block.scalar
            def _(e):
                for f in self.prog["act"]:
                    f(e)

            @block.vector
            def _(e):
                for f in self.prog["dve"]:
                    f(e)

            @block.gpsimd
            def _(e):
                for f in self.prog["pool"]:
                    f(e)

            @block.sync
            def _(e):
                for f in self.prog["sp"]:
                    f(e)


def interleave(gens):
    gens = list(gens)
    while gens:
        for g_ in list(gens):
            try:
                next(g_)
            except StopIteration:
                gens.remove(g_)


def build_program(nseq=NSEQ, stage=99, dbg=(), nsa_attn=True, gdn_cut=0, mixers=True):
    nc = bass.Bass("TRN2", target_bir_lowering=False)
    S = Sched(nc)
    es = contextlib.ExitStack()

    def dram(name, shape, dt, kind="ExternalInput"):
        return nc.dram_tensor(name, list(shape), dt, kind=kind).ap()

    def sb(name, shape, dt):
        return es.enter_context(nc.sbuf_tensor("s_" + name, list(shape), dt))

    x_d = dram("x", [nseq, T, D], F32)
    pos_d = dram("pos", [nseq, T], I32)
    wfm_d = dram("wfm", [NFM, 128, 1024], F32)
    wtm_d = dram("wtm", [128, 8 * NTM], F32)
    g1_d = dram("g1", [128, 8], F32)
    ropec_d = dram("ropec", [128, 4], F32)
    cbf_d = dram("cbf", [128, NCBF], BF16)
    selc_d = dram("selc", [128, NT * 32], F32)
    w1_d = dram("w1", [128, 2 * 32 * 128], F32)
    w2_d = dram("w2", [128, 128 + 64], F32)
    posT_d = dram("posT", [64, 2 * 32], F32)
    nsag_d = dram("nsag", [128, 4], F32)
    gdnc_d = dram("gdnc", [1, 8], F32)
    cf32_d = dram("cf32", [128, 386], F32)
    convw_d = dram("convw", [128, 48], F32)
    gng_d = dram("gng", [128, 1], F32)
    wout_d = dram("wout", [128, 8 * D], F32)
    g2_d = dram("g2", [128, 8], F32)
    fg_d = dram("fg", [1, D], F32)
    wg_d = dram("wg", [NFC, 128, 1024], F32)
    wu_d = dram("wu", [NFC, 128, 1024], F32)
    wd_d = dram("wd", [128, NFC * D], F32)
    x2_d = dram("x2s", [nseq, T, D], F32, kind="Internal")
    R_x2s = [Res() for _ in range(NT)]
    out_d = dram("out", [nseq, T, D], F32, kind="ExternalOutput")
    dbg_d = {}
    for name, shape in dbg:
        dbg_d[name] = dram("dbg_" + name, shape, F32, kind="ExternalOutput")

    cbf = sb("cbf", [128, NCBF], BF16)
    ident = cbf[:, 0:128]
    ones_bf = cbf[:, 128:256]
    causal = cbf[:, 256:384]
    anti = cbf[:, 384:512]
    maskA4 = cbf[:, 512:1024]
    maskQ4 = cbf[:, 1024:1536]
    ovl = cbf[:, 1536:1568]
    esel = cbf[:, 1568:1568 + 2048]
    cmpmask = cbf[:, 3616:3616 + 2048]
    ident4 = cbf[:, 5664:5664 + 512]
    causal01 = cbf[:, 6176:6304]
    anti01 = cbf[:, 6304:6432]
    cf32 = sb("cf32", [128, 3 * 128 + 2], F32)
    ltriT = cf32[:, 0:128]
    bones = cf32[:, 128:256]
    ones_f = cf32[:, 256:384]
    cind = cf32[:, 384:386]
    convw = sb("convw", [128, 12, 4], F32)
    gng = sb("gng", [128, 1], F32)
    g2T = sb("g2T", [128, 8], F32)
    fgb = sb("fgb", [128, D], F32)
    selc = sb("selc", [128, NT, 32], F32)
    g1T = sb("g1T", [128, 8], F32)
    ropec = sb("ropec", [128, 4], F32)
    cst = sb("cst", [128, 8], F32)
    nsag = sb("nsag", [128, 4], F32)
    hT = sb("hT", [128, 8, T], BF16)
    mixT = sb("mixT", [128, 8, T], BF16)
    beta_all = sb("beta_all", [128, NT, 4], F32)
    g_all = sb("g_all", [128, NT, 4], F32)
    R_bg = [Res() for _ in range(NT)]
    dtb = sb("dtb", [128, 4], F32)
    negA = sb("negA", [128, 4], F32)
    junk2 = sb("junk2", [128, 512], BF16)
    R_const = Res("const")
    R_hT = [Res(f"hT{i}") for i in range(NT)]
    R_mix = [[Res(f"mix{c}_{i}") for i in range(NT)] for c in range(2)]

    ps = [es.enter_context(nc.psum_tensor(f"ps{i}", [128, 512], F32)) for i in range(8)]
    R_ps = [Res(f"ps{i}") for i in range(8)]

    S.dma("sp", cbf[:], cbf_d[:], writes=[R_const])
    S.dma("sp", selc[:].rearrange("p a b -> p (a b)"), selc_d[:], writes=[R_const])
    S.dma("sp", g1T[:], g1_d[:], writes=[R_const])
    S.dma("sp", ropec[:], ropec_d[:], writes=[R_const])
    S.dma("sp", nsag[:], nsag_d[:], writes=[R_const])
    S.dma("sp", cf32[:], cf32_d[:], writes=[R_const])
    S.dma("sp", convw[:].rearrange("p a b -> p (a b)"), convw_d[:], writes=[R_const])
    S.dma("sp", gng[:], gng_d[:], writes=[R_const])
    S.dma("sp", g2T[:], g2_d[:], writes=[R_const])
    S.dma("sp", fgb[:], fg_d[0:1, :].to_broadcast([128, D]), writes=[R_const])
    S.dma("sp", dtb[:], gdnc_d[0:1, 4:8].to_broadcast([128, 4]), writes=[R_const])
    S.dma("sp", negA[:], gdnc_d[0:1, 0:4].to_broadcast([128, 4]), writes=[R_const])
    S.op("dve", lambda e: e.memset(cst[:, 0:1], EPS), writes=[R_const])
    S.op("dve", lambda e: e.memset(cst[:, 1:2], 1.0), writes=[R_const])
    S.op("dve", lambda e: e.memset(cst[:, 2:3], 0.0), writes=[R_const])
    S.op("dve", lambda e: e.memset(cst[:, 3:4], 1e-30), writes=[R_const])
    S.op("dve", lambda e: e.memset(cst[:, 4:5], EPS * 128), writes=[R_const])
    eps_ap, one_ap, zero_ap, tiny_ap, eps128_ap = cst[:, 0:1], cst[:, 1:2], cst[:, 2:3], cst[:, 3:4], cst[:, 4:5]

    def act(out, in_, func, bias=None, scale=1.0, accum_out=None, reads=(), writes=(), eng="act"):
        kw = {}
        if bias is not None:
            kw["bias"] = bias
        if accum_out is not None:
            kw["accum_out"] = accum_out
        S.op("act", lambda e: e.activation(out=out, in_=in_, func=func, scale=scale, **kw), reads=list(reads) + [R_const], writes=writes)

    def mm(out, lhsT, rhs, start, stop, reads=(), writes=(), skip=False):
        S.op("pe", lambda e: e.matmul(out, lhsT, rhs, start=start, stop=stop, skip_group_check=skip), reads=reads, writes=writes)

    def transpose(out, in_, idn, reads=(), writes=()):
        S.op("pe", lambda e: e.transpose(out, in_, idn), reads=list(reads) + [R_const], writes=writes)

    def rstd_from_ss(rstd, ss, n, reads_writes):
        act(rstd, ss, AF.Ln, bias=eps_ap, scale=1.0 / n, reads=reads_writes, writes=reads_writes)
        act(rstd, rstd, AF.Exp, scale=-0.5, reads=reads_writes, writes=reads_writes)

    act(negA[:], negA[:], AF.Exp, reads=[R_const], writes=[R_const])
    S.op("dve", lambda e: e.tensor_scalar(out=negA[:], in0=negA[:], scalar1=-1.0, scalar2=None, op0=ALU.mult), reads=[R_const], writes=[R_const])

    def tt(out, in0, in1, op, reads=(), writes=(), eng="dve"):
        S.op(eng, lambda e: e.tensor_tensor(out=out, in0=in0, in1=in1, op=op), reads=reads, writes=writes)

    def ts(out, in0, s1, s2, op0, op1=None, reads=(), writes=(), eng="dve"):
        if op1 is None:
            S.op(eng, lambda e: e.tensor_scalar(out=out, in0=in0, scalar1=s1, scalar2=None, op0=op0), reads=reads, writes=writes)
        else:
            S.op(eng, lambda e: e.tensor_scalar(out=out, in0=in0, scalar1=s1, scalar2=s2, op0=op0, op1=op1), reads=reads, writes=writes)

    def stt(out, in0, scalar, in1, op0, op1, reads=(), writes=()):
        S.op("dve", lambda e: e.scalar_tensor_tensor(out=out, in0=in0, scalar=scalar, in1=in1, op0=op0, op1=op1), reads=reads, writes=writes)

    def cp(out, in_, reads=(), writes=(), eng="dve"):
        S.op(eng, lambda e: e.tensor_copy(out=out, in_=in_), reads=reads, writes=writes)

    def memset(ap, val, writes=(), eng="pool"):
        S.op(eng, lambda e: e.memset(ap, val), writes=writes)

    def max8(out, in_, reads=(), writes=()):
        S.op("dve", lambda e: e.max(out=out, in_=in_), reads=reads, writes=writes)

    def recip(out, in_, reads=(), writes=()):
        S.op("dve", lambda e: e.reciprocal(out=out, in_=in_), reads=reads, writes=writes)

    if not mixers:
        nsa_attn = False
        S.op("pool", lambda e: e.memset(mixT[:], 0.0), writes=[x for y in R_mix for x in y])
    for seq in range(nseq):
        with contextlib.ExitStack() as ph:
            def psb(name, shape, dt):
                return ph.enter_context(nc.sbuf_tensor(f"s_{name}_{seq}", list(shape), dt))

            tab = psb("tab", [128, 2, T], F32)
            R_tab = Res()
            with contextlib.ExitStack() as ph0:
                def psb0(name, shape, dt):
                    return ph0.enter_context(nc.sbuf_tensor(f"s_{name}_{seq}", list(shape), dt))
                xt = [psb0(f"xt{i}", [128, D], F32) for i in range(2)]
                R_xt = [Res(), Res()]
                junk = psb0("junk", [128, D], BF16)
                hn = psb0("hn", [128, D], BF16)
                R_hn = Res()
                stat = psb0("stat", [128, 8], F32)
                R_stat = Res()
                posi = psb0("posi", [128, T], I32)
                tmpF = psb0("tmpF", [128, 2, T], F32)
                tmpI = psb0("tmpI", [128, 2, T], I32)
                S.dma("sp", posi[:], pos_d[seq:seq + 1, :].to_broadcast([128, T]), writes=[R_tab])
                ts(tmpF[:, 0, :], posi[:], ropec[:, 0:1], None, ALU.mult, reads=[R_tab, R_const], writes=[R_tab])
                ts(tmpF[:, 1, :], posi[:], ropec[:, 1:2], ropec[:, 2:3], ALU.mult, ALU.add, reads=[R_tab, R_const], writes=[R_tab])
                cp(tmpI[:], tmpF[:], reads=[R_tab], writes=[R_tab])
                cp(tab[:], tmpI[:], reads=[R_tab], writes=[R_tab])
                tt(tmpF[:], tmpF[:], tab[:], ALU.subtract, reads=[R_tab], writes=[R_tab])
                ts(tab[:], tmpF[:], 0.5, None, ALU.is_gt, reads=[R_tab], writes=[R_tab])
                tt(tmpF[:], tmpF[:], tab[:], ALU.subtract, reads=[R_tab], writes=[R_tab])
                ts(tab[:], tmpF[:], -0.5, None, ALU.is_lt, reads=[R_tab], writes=[R_tab])
                tt(tmpF[:], tmpF[:], tab[:], ALU.add, reads=[R_tab], writes=[R_tab])
                act(tab[:], tmpF[:], AF.Sin, scale=6.283185, reads=[R_tab], writes=[R_tab])
                for i in range(NT):
                    b = i % 2
                    S.dma("sp", xt[b][:], x_d[seq, i * 128:(i + 1) * 128, :], writes=[R_xt[b]])
                    act(junk[:], xt[b][:], AF.Square, accum_out=stat[:, 0:1], reads=[R_xt[b]], writes=[R_stat])
                    rstd_from_ss(stat[:, 1:2], stat[:, 0:1], D, [R_stat])
                    ts(hn[:], xt[b][:], stat[:, 1:2], None, ALU.mult, reads=[R_xt[b], R_stat], writes=[R_hn])
                    pb = ps[7][:].bitcast(BF16)
                    for k in range(8):
                        transpose(pb[:, k * 128:(k + 1) * 128], hn[:, k * 128:(k + 1) * 128], ident, reads=[R_hn], writes=[R_ps[7]])
                    tt(hT[:, :, i * 128:(i + 1) * 128], pb.rearrange("p (k t) -> p k t", k=8),
                       g1T[:].unsqueeze(2).to_broadcast([128, 8, 128]), ALU.mult, reads=[R_ps[7], R_const], writes=[R_hT[i]])
            S.barrier()

            kcT = [psb(f"kcT{g}", [128, 128], BF16) for g in range(2)]
            vca = [psb(f"vca{g}", [128, 97], BF16) for g in range(2)]
            ph1 = contextlib.ExitStack()

            def psb1(name, shape, dt):
                return ph1.enter_context(nc.sbuf_tensor(f"s_{name}_{seq}", list(shape), dt))
            QT = psb("QT", [128, 4, T], BF16)
            KT = psb("KT", [128, 6, T], BF16)
            VcT = psb("VcT", [128, T], BF16)
            Vtok = psb("Vtok", [128, NT, 4, 65], BF16)
            gat = psb("gat", [128, NT, 24], F32)
            R_QT = [[Res() for _ in range(4)] for _ in range(4)]
            R_KT = [[Res() for _ in range(4)] for _ in range(6)]
            R_Vc = [Res() for _ in range(4)]
            R_Vtok = [Res() for _ in range(NT)]
            R_gat = [Res() for _ in range(NT)]
            wb = [psb1(f"wb{i}", [128, 1024], BF16) for i in range(4)]
            R_wb = [Res() for _ in range(4)]
            wtm = psb1("wtm", [128, 8, NTM], BF16)
            R_wtm = Res()
            rt = [psb1(f"rt{i}", [128, 512], F32) for i in range(4)]
            R_rt = [Res() for _ in range(4)]
            S.dma("pool", wtm[:].rearrange("p k n -> p (k n)"), wtm_d[:], writes=[R_wtm])
            memset(Vtok[:, :, :, 64:65], 1.0, writes=R_Vtok)
            wctr = [0]

            def load_w(chunk):
                r = wctr[0] % 4
                wctr[0] += 1
                S.dma("pool", wb[r][:], wfm_d[chunk], writes=[R_wb[r]])
                return r

            def proj_fm(r, tb, bank):
                for kc in range(8):
                    mm(ps[bank][:], wb[r][:, kc * 128:(kc + 1) * 128], hT[:, kc, tb * 512:(tb + 1) * 512], kc == 0, kc == 7,
                       reads=[R_wb[r]] + R_hT[tb * 4:tb * 4 + 4], writes=[R_ps[bank]])

            pairs = [(FM_Q + j, FM_QSW + j, QT, j, R_QT[j]) for j in range(4)] + [(FM_K + m, FM_KSW + m, KT, m, R_KT[m]) for m in range(6)]
            bctr = 0
            plist = pairs if nsa_attn else []
            pre_loaded = {}
            if plist:
                pre_loaded[0] = (load_w(plist[0][0]), load_w(plist[0][1]))
            for pi, (ca, cb_, dst, di, rdst) in enumerate(plist):
                ra, rb = pre_loaded.pop(pi)
                if pi + 1 < len(plist):
                    pre_loaded[pi + 1] = (load_w(plist[pi + 1][0]), load_w(plist[pi + 1][1]))
                for tb in range(4):
                    ba = (bctr % 3) * 2
                    bctr += 1
                    proj_fm(ra, tb, ba)
                    proj_fm(rb, tb, ba + 1)
                    i0 = (bctr % 2) * 2
                    tt(rt[i0][:], ps[ba][:], tab[:, 1, tb * 512:(tb + 1) * 512], ALU.mult, reads=[R_ps[ba], R_tab], writes=[R_rt[i0]])
                    tt(rt[i0 + 1][:], ps[ba + 1][:], tab[:, 0, tb * 512:(tb + 1) * 512], ALU.mult, reads=[R_ps[ba + 1], R_tab], writes=[R_rt[i0 + 1]])
                    tt(dst[:, di, tb * 512:(tb + 1) * 512], rt[i0][:], rt[i0 + 1][:], ALU.add, reads=[R_rt[i0], R_rt[i0 + 1]], writes=[rdst[tb]])
            rv = load_w(FM_VC)
            for tb in (range(4) if nsa_attn else []):
                proj_fm(rv, tb, 6)
                act(VcT[:, tb * 512:(tb + 1) * 512], ps[6][:], AF.Copy, reads=[R_ps[6]], writes=[R_Vc[tb]])
            sg2 = [psb1(f"sg{i}", [128, 32], F32) for i in range(2)]
            R_sg2 = [Res(), Res()]
            for i in range(NT):
                bank = 6 + (i % 2)
                sg, R_sg = sg2[i % 2], R_sg2[i % 2]
                for kc in range(8):
                    mm(ps[bank][:, 0:NTM], hT[:, kc, i * 128:(i + 1) * 128], wtm[:, kc, :], kc == 0, kc == 7,
                       reads=[R_hT[i], R_wtm], writes=[R_ps[bank]])
                act(Vtok[:, i, :, 0:64], ps[bank][:, 0:256].rearrange("p (a b) -> p a b", a=4), AF.Copy, reads=[R_ps[bank]], writes=[R_Vtok[i]])
                act(sg[:, 0:28], ps[bank][:, 256:284], AF.Exp, scale=-1.0, reads=[R_ps[bank]], writes=[R_sg])
                ts(sg[:, 0:28], sg[:, 0:28], 1.0, None, ALU.add, reads=[R_sg], writes=[R_sg])
                recip(gat[:, i, :], sg[:, 0:24], reads=[R_sg], writes=[R_gat[i]])
                recip(beta_all[:, i, :], sg[:, 24:28], reads=[R_sg], writes=[R_bg[i]])
                tt(sg[:, 28:32], ps[bank][:, 284:288], dtb[:], ALU.add, reads=[R_ps[bank], R_const], writes=[R_sg])
                act(sg[:, 28:32], sg[:, 28:32], AF.Exp, reads=[R_sg], writes=[R_sg])
                act(sg[:, 28:32], sg[:, 28:32], AF.Ln, bias=one_ap, reads=[R_sg], writes=[R_sg])
                tt(g_all[:, i, :], sg[:, 28:32], negA[:], ALU.mult, reads=[R_sg, R_const], writes=[R_bg[i]])

            if nsa_attn:
                w1 = psb1("w1", [128, 2, 32, 128], BF16)
                w2 = psb1("w2", [128, 192], BF16)
                posT = psb1("posT", [64, 2, 32], BF16)
                R_cw = Res()
                S.dma("pool", w1[:].rearrange("p a l h -> p (a l h)"), w1_d[:], writes=[R_cw])
                S.dma("pool", w2[:], w2_d[:], writes=[R_cw])
                S.dma("pool", posT[:].rearrange("p a l -> p (a l)"), posT_d[:], writes=[R_cw])
                R_kc = [Res(), Res()]
                R_vc = [Res(), Res()]
                cbias = psb1("cbias", [128, 2], F32)
                hid = psb1("hid", [128, 128], BF16)
                R_hid = Res()
                R_cb = Res()
                for g in range(2):
                    memset(kcT[g][:], 0.0, writes=[R_kc[g]])
                    memset(vca[g][:], 0.0, writes=[R_vc[g]])
                    memset(vca[g][:, 64:65], 1.0, writes=[R_vc[g]])
                    cp(vca[g][:, 65:97], ovl, reads=[R_const], writes=[R_vc[g]], eng="pool")
                memset(hid[:], 0.0, writes=[R_hid])
                for i2 in range(2):
                    for l in range(32):
                        mm(ps[6][:, i2:i2 + 1], w1[0:64, i2, l, :], posT[:, i2, l:l + 1], l == 0, l == 31, reads=[R_cw], writes=[R_ps[6]])
                cp(cbias[:], ps[6][:, 0:2], reads=[R_ps[6]], writes=[R_cb])
                for g in range(2):
                    for i2 in range(2):
                        if i2 == 0:
                            src, pb0, rsrc = KT[0:64, g, :], 0, R_KT[g]
                        else:
                            pb0 = g * 64
                            src, rsrc = VcT[pb0:pb0 + 64, :], R_Vc
                        for l in range(32):
                            mm(ps[6][:, 0:127], w1[pb0:pb0 + 64, i2, l, :], src[:, l:l + 16 * 126 + 1:16], l == 0, l == 31,
                               reads=[R_cw] + rsrc, writes=[R_ps[6]])
                        act(hid[:, 0:127], ps[6][:, 0:127], AF.Silu, bias=cbias[:, i2:i2 + 1], reads=[R_ps[6], R_cb], writes=[R_hid])
                        if i2 == 0:
                            mm(ps[7][:, 0:127], w2[:, 0:128], hid[:, 0:127], True, True, reads=[R_cw, R_hid], writes=[R_ps[7]])
                            cp(kcT[g][:, 0:127], ps[7][:, 0:127], reads=[R_ps[7]], writes=[R_kc[g]])
                        else:
                            mm(ps[7][:, 0:64], hid[:, :], w2[:, 128:192], True, True, reads=[R_cw, R_hid], writes=[R_ps[7]])
                            cp(vca[g][0:127, 0:64], ps[7][0:127, 0:64], reads=[R_ps[7]], writes=[R_vc[g]])

                ph1.close()
                S.barrier()
                PT = [psb(f"PT{i}", [128, 512], BF16) for i in range(4)]
                R_PT = [Res() for _ in range(4)]
                STB = [0, 1, 2, 6]
                onsa = psb("onsa", [128, 4, 512], F32)
                R_onsa = Res()
                onbf = psb("onbf", [128, 4, 512], BF16)
                R_onbf = Res()
                impsum = psb("impsum", [128, 4, 32], F32)
                impt = psb("impt", [128, 4, 32], F32)
                R_imp = Res()
                R_impt = Res()
                selb = psb("selb", [128, 4, 32], BF16)
                R_selb = Res()
                m8 = psb("m8", [128, 8], F32)
                R_m8 = Res()
                selbT = [psb(f"selbT{g}", [32, 512], BF16) for g in range(2)]
                R_selbT = [Res(), Res()]
                sm = [psb(f"sm{i}", [128, 16], F32) for i in range(2)]
                R_sm = [Res(), Res()]
                tmpo = [psb(f"tmpo{i}", [128, 4, 64], F32) for i in range(2)]
                R_tmpo = [Res(), Res()]
                fst = psb("fst", [128, 8], F32)
                R_fst = Res()
                ctr = {"st": 0, "pt": 0, "ob": 0, "sm": 0}

                def nxt(k, n):
                    v = ctr[k] % n
                    ctr[k] += 1
                    return v

                def evac_branch(ob, width, c, hh, br, first):
                    ov = ps[ob][:, 0:4 * width].rearrange("p (q f) -> p q f", q=4)
                    si = nxt("sm", 2)
                    smt = sm[si]
                    ts(smt[:, 0:4], ov[:, :, 64], 1e-30, None, ALU.add, reads=[R_ps[ob]], writes=[R_sm[si]])
                    recip(smt[:, 4:8], smt[:, 0:4], reads=[R_sm[si]], writes=[R_sm[si]])
                    tt(smt[:, 8:12], smt[:, 4:8], gat[:, 4 * c:4 * c + 4, hh * 3 + br], ALU.mult, reads=[R_sm[si]] + R_gat[4 * c:4 * c + 4], writes=[R_sm[si]])
                    cb = smt[:, 8:12].unsqueeze(2).to_broadcast([128, 4, 64])
                    if first:
                        tt(onsa[:, :, hh * 64:(hh + 1) * 64], ov[:, :, 0:64], cb, ALU.mult, reads=[R_ps[ob], R_sm[si]], writes=[R_onsa])
                    else:
                        tt(tmpo[si][:], ov[:, :, 0:64], cb, ALU.mult, reads=[R_ps[ob], R_sm[si]], writes=[R_tmpo[si]])
                        tt(onsa[:, :, hh * 64:(hh + 1) * 64], onsa[:, :, hh * 64:(hh + 1) * 64], tmpo[si][:], ALU.add,
                           reads=[R_tmpo[si], R_onsa], writes=[R_onsa], eng="pool")
                    return smt, si

                for c in range(4):
                    for g in range(2):
                        cmp_pt = []
                        for h in range(4):
                            hh = 4 * g + h
                            j, hb = hh // 2, (hh % 2) * 64
                            qT = QT[hb:hb + 64, j, c * 512:(c + 1) * 512]
                            st = STB[nxt("st", 4)]
                            mm(ps[st][:], kcT[g][hb:hb + 64, :], qT, True, False, reads=[R_kc[g], R_QT[j][c]], writes=[R_ps[st]])
                            mm(ps[st][:], ident, cmpmask[:, c * 512:(c + 1) * 512], False, True, reads=[R_const], writes=[R_ps[st]])
                            pt = nxt("pt", 4)
                            act(PT[pt][:], ps[st][:], AF.Exp, scale=0.125, reads=[R_ps[st]], writes=[R_PT[pt]])
                            cmp_pt.append(pt)
                        for h in range(4):
                            hh = 4 * g + h
                            pt = cmp_pt[h]
                            ob = 3 + nxt("ob", 2)
                            for tq in range(4):
                                mm(ps[ob][:, tq * 97:(tq + 1) * 97], PT[pt][:, tq * 128:(tq + 1) * 128], vca[g][:, 0:97], tq == 0, tq == 3,
                                   reads=[R_PT[pt], R_vc[g]], writes=[R_ps[ob]], skip=True)
                            smt, si = evac_branch(ob, 97, c, hh, 0, True)
                            ov = ps[ob][:, 0:388].rearrange("p (q f) -> p q f", q=4)
                            rb_ = smt[:, 4:8].unsqueeze(2).to_broadcast([128, 4, 32])
                            if h == 0:
                                tt(impsum[:], ov[:, :, 65:97], rb_, ALU.mult, reads=[R_ps[ob], R_sm[si]], writes=[R_imp])
                            else:
                                tt(impt[:], ov[:, :, 65:97], rb_, ALU.mult, reads=[R_ps[ob], R_sm[si]], writes=[R_impt])
                                tt(impsum[:], impsum[:], impt[:], ALU.add, reads=[R_imp, R_impt], writes=[R_imp])
                        tt(impsum[:], impsum[:], selc[:, 4 * c:4 * c + 4, :], ALU.add, reads=[R_imp, R_const], writes=[R_imp])
                        p5 = ps[5][:].bitcast(BF16)
                        for tq in range(4):
                            max8(m8[:], impsum[:, tq, :], reads=[R_imp], writes=[R_m8])
                            ts(selb[:, tq, :], impsum[:, tq, :], m8[:, 7:8], NEG, ALU.is_lt, ALU.mult, reads=[R_imp, R_m8], writes=[R_selb])
                        for tq in range(4):
                            transpose(p5[0:32, tq * 128:(tq + 1) * 128], selb[:, tq, :], ident, reads=[R_selb], writes=[R_ps[5]])
                        cp(selbT[g][:], p5[0:32, 0:512], reads=[R_ps[5]], writes=[R_selbT[g]])
                        def branch_stream(c, g, h, br, ob):
                            hh = 4 * g + h
                            j, hb = hh // 2, (hh % 2) * 64
                            kts = list(range(0, 4 * c + 4)) if br == 1 else list(range(max(0, 4 * c - 4), 4 * c + 4))
                            plan = []
                            for kt in kts:
                                dq = kt - 4 * c
                                lo = max(0, dq)
                                hi = 3 if br == 1 else min(3, dq + 4)
                                plan.append((kt, dq, lo, hi))
                            npv = sum(hi - lo + 1 for (_, _, lo, hi) in plan)
                            ipv = [0]

                            def emit_pv(item, pt):
                                kt, dq, lo, hi = item
                                for tq in range(lo, hi + 1):
                                    mm(ps[ob][:, tq * 65:(tq + 1) * 65], PT[pt][:, tq * 128:(tq + 1) * 128], Vtok[:, kt, (br - 1) * 2 + g, :],
                                       ipv[0] == 0, ipv[0] == npv - 1, reads=[R_PT[pt], R_Vtok[kt]], writes=[R_ps[ob]], skip=True)
                                    ipv[0] += 1
                            pend = None
                            for item in plan:
                                kt, dq, lo, hi = item
                                c0, c1 = lo * 128, (hi + 1) * 128
                                st = STB[nxt("st", 4)]
                                km = 2 * br + g
                                mm(ps[st][:, c0:c1], KT[hb:hb + 64, km, kt * 128:(kt + 1) * 128], QT[hb:hb + 64, j, c * 512 + c0:c * 512 + c1],
                                   True, br == 2, reads=[R_KT[km][kt // 4], R_QT[j][c]], writes=[R_ps[st]])
                                if br == 1:
                                    mm(ps[st][:, c0:c1], esel[0:32, kt * 128:(kt + 1) * 128], selbT[g][0:32, c0:c1], False, True,
                                       reads=[R_const, R_selbT[g]], writes=[R_ps[st]])
                                pt = nxt("pt", 4)
                                act(PT[pt][:, c0:c1], ps[st][:, c0:c1], AF.Exp, scale=0.125, reads=[R_ps[st]], writes=[R_PT[pt]])
                                if dq >= 0:
                                    dsl = slice(dq * 128, (dq + 1) * 128)
                                    tt(PT[pt][:, dsl], PT[pt][:, dsl], causal01, ALU.mult, reads=[R_PT[pt], R_const], writes=[R_PT[pt]])
                                if br == 2 and dq + 4 <= 3:
                                    dsl = slice((dq + 4) * 128, (dq + 5) * 128)
                                    tt(PT[pt][:, dsl], PT[pt][:, dsl], anti01, ALU.mult, reads=[R_PT[pt], R_const], writes=[R_PT[pt]])
                                yield
                                if pend is not None:
                                    emit_pv(*pend)
                                pend = (item, pt)
                            yield
                            emit_pv(*pend)
                            yield
                            evac_branch(ob, 65, c, hh, br, False)
                            yield

                        def lane(items, ob):
                            for (h, br) in items:
                                yield from branch_stream(c, g, h, br, ob)
                        interleave([lane([(0, 1), (1, 2), (2, 1), (3, 2)], 3), lane([(0, 2), (1, 1), (2, 2), (3, 1)], 4)])
                    for tq in range(4):
                        act(junk2[:], onsa[:, tq, :], AF.Square, accum_out=fst[:, tq:tq + 1], reads=[R_onsa], writes=[R_fst])
                    rstd_from_ss(fst[:, 4:8], fst[:, 0:4], 512, [R_fst])
                    tt(onbf[:], onsa[:], fst[:, 4:8].unsqueeze(2).to_broadcast([128, 4, 512]), ALU.mult, reads=[R_onsa, R_fst], writes=[R_onbf])
                    if "onsa" in dbg_d and seq == 0:
                        S.dma("sp", dbg_d["onsa"][:, c * 2048:(c + 1) * 2048], onsa[:].rearrange("p a b -> p (a b)"), reads=[R_onsa])
                    p7 = ps[7][:].bitcast(BF16)
                    for tq in range(4):
                        i = 4 * c + tq
                        for k in range(4):
                            transpose(p7[:, k * 128:(k + 1) * 128], onbf[:, tq, k * 128:(k + 1) * 128], ident, reads=[R_onbf], writes=[R_ps[7]])
                        tt(mixT[:, 0:4, i * 128:(i + 1) * 128], p7[:, 0:512].rearrange("p (k t) -> p k t", k=4),
                           nsag[:].unsqueeze(2).to_broadcast([128, 4, 128]), ALU.mult, reads=[R_ps[7], R_const], writes=[R_mix[0][i]])

            ph1.close()
            if dbg_d and seq == 0:
                rt = [psb("dbgrt", [128, 512], F32)]
                R_rt = [Res()]

                def dump(name, src3, nchunk, rlist):
                    for k in range(nchunk):
                        for q4 in range(4):
                            cp(rt[0][:], src3[:, k, q4 * 512:(q4 + 1) * 512], reads=rlist + [R_rt[0]], writes=[R_rt[0]])
                            S.dma("sp", dbg_d[name][:, k * T + q4 * 512:k * T + (q4 + 1) * 512], rt[0][:], reads=[R_rt[0]], writes=[R_rt[0]])
                if "QT" in dbg_d:
                    dump("QT", QT, 4, [x for y in R_QT for x in y])
                if "KT" in dbg_d:
                    dump("KT", KT, 6, [x for y in R_KT for x in y])
                if "mixN" in dbg_d:
                    dump("mixN", mixT, 4, R_mix[0])
        S.barrier()


        if stage >= 2 and mixers:
          with contextlib.ExitStack() as ph:
            def psb(name, shape, dt):
                return ph.enter_context(nc.sbuf_tensor(f"s_g{name}_{seq}", list(shape), dt))
            phg1 = contextlib.ExitStack()

            def psbg1(name, shape, dt):
                return phg1.enter_context(nc.sbuf_tensor(f"s_g{name}_{seq}", list(shape), dt))
            bctr2 = [0]

            def cut(n):
                if gdn_cut == n:
                    S.dead = True

            def bank():
                b_ = bctr2[0] % 8
                bctr2[0] += 1
                return b_

            gq = psb("gq", [128, 8, T], BF16)
            R_gq = [[Res() for _ in range(4)] for _ in range(8)]
            vtok = psb("vtok", [128, NT, 4, 128], BF16)
            R_vtok = [Res() for _ in range(NT)]
            zT = psb("zT", [128, 4, T], BF16)
            R_zT = [[Res() for _ in range(4)] for _ in range(4)]
            wb = [psbg1(f"wb{i}", [128, 1024], BF16) for i in range(4)]
            R_wb = [Res() for _ in range(4)]
            xbuf = [psbg1(f"xbuf{i}", [128, 515], F32) for i in range(2)]
            R_xb = [Res(), Res()]
            acc = [psbg1(f"acc{i}", [128, 512], F32) for i in range(2)]
            R_acc = [Res(), Res()]
            vtmp = psbg1("vtmp", [128, 512], BF16)
            R_vtmp = Res()
            wctr = [0]

            def load_w(chunk):
                r = wctr[0] % 4
                wctr[0] += 1
                S.dma("pool", wb[r][:], wfm_d[chunk], writes=[R_wb[r]])
                return r

            gl_ = {0: load_w(FM_GQKV + 0), 1: load_w(FM_GQKV + 1)}
            for cg in range(16):
                r = gl_.pop(cg)
                if cg + 2 < 16:
                    gl_[cg + 2] = load_w(FM_GQKV + cg + 2)
                for tb in range(4):
                    bk = bank()
                    for kc in range(8):
                        mm(ps[bk][:], wb[r][:, kc * 128:(kc + 1) * 128], hT[:, kc, tb * 512:(tb + 1) * 512], kc == 0, kc == 7,
                           reads=[R_wb[r]] + R_hT[tb * 4:tb * 4 + 4], writes=[R_ps[bk]])
                    if cg >= 12:
                        act(zT[:, cg - 12, tb * 512:(tb + 1) * 512], ps[bk][:], AF.Silu, reads=[R_ps[bk]], writes=[R_zT[cg - 12][tb]])
                        continue
                    b = tb % 2
                    if tb == 0:
                        memset(xbuf[b][:, 0:3], 0.0, writes=[R_xb[b]])
                    else:
                        cp(xbuf[b][:, 0:3], xbuf[1 - b][:, 512:515], reads=[R_xb[1 - b]], writes=[R_xb[b]], eng="pool")
                    act(xbuf[b][:, 3:515], ps[bk][:], AF.Copy, reads=[R_ps[bk]], writes=[R_xb[b]])
                    ts(acc[b][:], xbuf[b][:, 0:512], convw[:, cg, 0:1], None, ALU.mult, reads=[R_xb[b], R_const], writes=[R_acc[b]])
                    for tap in (1, 2, 3):
                        stt(acc[b][:], xbuf[b][:, tap:tap + 512], convw[:, cg, tap:tap + 1], acc[b][:], ALU.mult, ALU.add,
                            reads=[R_xb[b], R_acc[b], R_const], writes=[R_acc[b]])
                    if cg < 8:
                        act(gq[:, cg, tb * 512:(tb + 1) * 512], acc[b][:], AF.Silu, reads=[R_acc[b]], writes=[R_gq[cg][tb]])
                    else:
                        act(vtmp[:], acc[b][:], AF.Silu, reads=[R_acc[b]], writes=[R_vtmp])
                        bk2 = bank()
                        pbf = ps[bk2][:].bitcast(BF16)
                        for i4 in range(4):
                            transpose(pbf[:, i4 * 128:(i4 + 1) * 128], vtmp[:, i4 * 128:(i4 + 1) * 128], ident, reads=[R_vtmp], writes=[R_ps[bk2]])
                        cp(vtok[:, tb * 4:tb * 4 + 4, cg - 8, :], pbf[:, 0:512].rearrange("p (a b) -> p a b", a=4), reads=[R_ps[bk2]],
                           writes=R_vtok[tb * 4:tb * 4 + 4])
            cut(1)
            sqb = [psbg1(f"sqb{i}", [128, 512], BF16) for i in range(2)]
            R_sq = [Res(), Res()]
            lnb = [psbg1(f"lnb{i}", [128, 512], F32) for i in range(2)]
            R_ln = [Res(), Res()]
            it = 0
            for cg in range(8):
                for tb in range(4):
                    b = it % 2
                    it += 1
                    src = gq[:, cg, tb * 512:(tb + 1) * 512]
                    tt(sqb[b][:], src, src, ALU.mult, reads=[R_gq[cg][tb]], writes=[R_sq[b]], eng="pool")
                    bk = bank()
                    mm(ps[bk][:], ones_bf, sqb[b][:], True, True, reads=[R_sq[b], R_const], writes=[R_ps[bk]])
                    act(lnb[b][:], ps[bk][:], AF.Ln, bias=eps_ap, reads=[R_ps[bk]], writes=[R_ln[b]])
                    act(lnb[b][:], lnb[b][:], AF.Exp, scale=-0.5, reads=[R_ln[b]], writes=[R_ln[b]])
                    tt(src, src, lnb[b][:], ALU.mult, reads=[R_gq[cg][tb], R_ln[b]], writes=[R_gq[cg][tb]])

            cut(2)
            phg1.close()
            S.barrier()
            def t4(name, dt=BF16):
                return psb(name, [128, 4, 128], dt)
            Sst = t4("Sst", F32)
            Stmp = t4("Stmp", F32)
            Sbf = [t4("Sbf0"), t4("Sbf1")]
            R_S, R_Stmp, R_Sbf = Res(), Res(), [Res(), Res()]
            memset(Sst[:], 0.0, writes=[R_S])
            memset(Sbf[0][:], 0.0, writes=[R_Sbf[0]])
            sbi = [0]
            NSET = 5
            B = []
            for k_ in range(NSET):
                d_ = {}
                names_ = ("EmA", "EmQ", "X0", "X1", "Y0", "Y1", "M0", "M1", "qkT", "kbg", "kdd", "vb", "wTn")
                for j_, nm in enumerate(names_):
                    if k_ < 3:
                        d_[nm] = t4(f"{nm}_{k_}", BF16)
                    else:
                        ck = (k_ - 3) * 4 + j_ // 4
                        d_[nm] = hT[:, ck, (j_ % 4) * 512:(j_ % 4 + 1) * 512].rearrange("p (a b) -> p a b", a=4)
                    d_["R_" + nm] = Res()
                d_["gs"] = psb(f"gs_{k_}", [128, 48], F32)
                d_["R_gs"] = Res()
                B.append(d_)
            shared = {}
            for nm, dt_ in (("Lg", F32),):
                shared[nm] = t4(nm, dt_)
                shared["R_" + nm] = Res()
            for d_ in B:
                d_.update(shared)
            vnew = t4("vnew")
            osb = t4("osb", F32)
            ofin = t4("ofin", F32)
            onb = t4("onb")
            R_vnew, R_osb, R_ofin, R_onb = [Res() for _ in range(4)]
            id4 = ident4.rearrange("p (a b) -> p a b", a=4)
            Mfin = {}

            def b4(ap):
                return ap.unsqueeze(2).to_broadcast([128, 4, 128])

            def v4(bk):
                return ps[bk][:].rearrange("p (a b) -> p a b", a=4)

            def pre_tile(i):
                D_ = B[i % NSET]
                gs, R_gs = D_["gs"], D_["R_gs"]
                Lg, EmA, EmQ, qkT, kbg, kdd, vb, wTn = (D_[k] for k in ("Lg", "EmA", "EmQ", "qkT", "kbg", "kdd", "vb", "wTn"))
                R_Lg, R_EmA, R_EmQ, R_qkT, R_kbg, R_kdd, R_vb, R_wTn = (D_["R_" + k] for k in ("Lg", "EmA", "EmQ", "qkT", "kbg", "kdd", "vb", "wTn"))
                Xb, Yb, Mb = [D_["X0"], D_["X1"]], [D_["Y0"], D_["Y1"]], [D_["M0"], D_["M1"]]
                R_X, R_Y, R_M = [D_["R_X0"], D_["R_X1"]], [D_["R_Y0"], D_["R_Y1"]], [D_["R_M0"], D_["R_M1"]]
                tsl = slice(i * 128, (i + 1) * 128)
                g_i = g_all[:, i, :]
                be_i = beta_all[:, i, :]
                Rk = [R_gq[4 + h][i // 4] for h in range(4)]
                Rq = [R_gq[h][i // 4] for h in range(4)]
                bk = bank()
                mm(ps[bk][:, 0:4], ltriT, g_i, True, True, reads=[R_const, R_bg[i]], writes=[R_ps[bk]])
                mm(ps[bk][:, 4:8], bones, g_i, True, True, reads=[R_const, R_bg[i]], writes=[R_ps[bk]])
                cp(gs[:, 0:4], ps[bk][:, 0:4], reads=[R_ps[bk]], writes=[R_gs])
                ts(gs[:, 4:8], be_i, -1.0, None, ALU.mult, reads=[R_bg[i]], writes=[R_gs])
                act(gs[:, 8:12], gs[:, 0:4], AF.Exp, reads=[R_gs], writes=[R_gs])
                tt(gs[:, 12:16], gs[:, 8:12], be_i, ALU.mult, reads=[R_gs, R_bg[i]], writes=[R_gs])
                tt(gs[:, 16:20], ps[bk][:, 4:8], gs[:, 0:4], ALU.subtract, reads=[R_ps[bk], R_gs], writes=[R_gs])
                act(gs[:, 16:20], gs[:, 16:20], AF.Exp, reads=[R_gs], writes=[R_gs])
                ts(gs[:, 20:24], gs[:, 0:4], -1.0, None, ALU.mult, reads=[R_gs], writes=[R_gs])
                tt(gs[:, 24:32].rearrange("p (h j) -> p h j", h=4), g_i.unsqueeze(2).to_broadcast([128, 4, 2]),
                   cind.unsqueeze(1).to_broadcast([128, 4, 2]), ALU.mult, reads=[R_bg[i], R_const], writes=[R_gs])
                tt(Lg[:], ltriT.unsqueeze(1).to_broadcast([128, 4, 128]), b4(g_i), ALU.mult, reads=[R_const, R_bg[i]], writes=[R_Lg], eng="pool")
                bk = bank()
                mm(ps[bk][:, 0:8], ones_f, gs[:, 24:32], True, True, reads=[R_const, R_gs], writes=[R_ps[bk]])
                act(gs[:, 32:40], ps[bk][:, 0:8], AF.Exp, reads=[R_ps[bk]], writes=[R_gs])
                bA = bank()
                mm(ps[bA][:], ones_f, Lg[:].rearrange("p a b -> p (a b)"), True, False, reads=[R_const, R_Lg], writes=[R_ps[bA]])
                mm(ps[bA][:], ident, maskA4, False, True, reads=[R_const], writes=[R_ps[bA]])
                for h in range(4):
                    act(EmA[:, h, :], ps[bA][:, h * 128:(h + 1) * 128], AF.Exp, bias=gs[:, h:h + 1], scale=-1.0, reads=[R_ps[bA], R_gs], writes=[R_EmA])
                bQ = bank()
                mm(ps[bQ][:], ones_f, Lg[:].rearrange("p a b -> p (a b)"), True, False, reads=[R_const, R_Lg], writes=[R_ps[bQ]])
                mm(ps[bQ][:], ident, maskQ4, False, True, reads=[R_const], writes=[R_ps[bQ]])
                for h in range(4):
                    act(EmQ[:, h, :], ps[bQ][:, h * 128:(h + 1) * 128], AF.Exp, bias=gs[:, 20 + h:21 + h], scale=1.0, reads=[R_ps[bQ], R_gs], writes=[R_EmQ])
                yield
                bK = bank()
                for h in range(4):
                    kT = gq[:, 4 + h, tsl]
                    mm(ps[bK][:, h * 128:(h + 1) * 128], kT, kT, True, True, reads=[Rk[h]], writes=[R_ps[bK]])
                for h in range(4):
                    stt(Xb[0][:, h, :], ps[bK][:, h * 128:(h + 1) * 128], gs[:, 4 + h:5 + h], EmA[:, h, :], ALU.mult, ALU.mult,
                        reads=[R_ps[bK], R_gs, R_EmA], writes=[R_X[0]])
                yield
                bKQ = bank()
                for h in range(4):
                    mm(ps[bKQ][:, h * 128:(h + 1) * 128], gq[:, 4 + h, tsl], gq[:, h, tsl], True, True, reads=[Rk[h], Rq[h]], writes=[R_ps[bKQ]])
                tt(qkT[:], v4(bKQ), EmQ[:], ALU.mult, reads=[R_ps[bKQ], R_EmQ], writes=[R_qkT])
                bT = bank()
                pT = ps[bT][:].bitcast(BF16)
                for h in range(4):
                    transpose(pT[:, h * 128:(h + 1) * 128], gq[:, 4 + h, tsl], ident, reads=[Rk[h]], writes=[R_ps[bT]])
                pT4 = pT[:, 0:512].rearrange("p (a b) -> p a b", a=4)
                tt(kbg[:], pT4, b4(gs[:, 12:16]), ALU.mult, reads=[R_ps[bT], R_gs], writes=[R_kbg])
                tt(kdd[:], pT4, b4(gs[:, 16:20]), ALU.mult, reads=[R_ps[bT], R_gs], writes=[R_kdd])
                tt(vb[:], vtok[:, i, :, :], b4(be_i), ALU.mult, reads=[R_vtok[i], R_bg[i]], writes=[R_vb], eng="pool")
                yield
                bT = bank()
                pT = ps[bT][:].bitcast(BF16)
                for h in range(4):
                    transpose(pT[:, h * 128:(h + 1) * 128], Xb[0][:, h, :], ident, reads=[R_X[0]], writes=[R_ps[bT]])
                pT4 = pT[:, 0:512].rearrange("p (a b) -> p a b", a=4)
                act(Yb[0][:], pT4, AF.Copy, reads=[R_ps[bT]], writes=[R_Y[0]])
                tt(Mb[0][:], Yb[0][:], id4, ALU.add, reads=[R_Y[0], R_const], writes=[R_M[0]])
                yield
                xc, yc, mc = 0, 0, 0
                for lvl in range(5):
                    xn, yn, mn = 1 - xc, 1 - yc, 1 - mc
                    bX = bank()
                    for h in range(4):
                        mm(ps[bX][:, h * 128:(h + 1) * 128], Yb[yc][:, h, :], Xb[xc][:, h, :], True, True, reads=[R_Y[yc], R_X[xc]], writes=[R_ps[bX]])
                    act(Xb[xn][:], v4(bX), AF.Copy, reads=[R_ps[bX]], writes=[R_X[xn]])
                    if lvl < 4:
                        bY = bank()
                        for h in range(4):
                            mm(ps[bY][:, h * 128:(h + 1) * 128], Xb[xc][:, h, :], Yb[yc][:, h, :], True, True, reads=[R_Y[yc], R_X[xc]], writes=[R_ps[bY]])
                        act(Yb[yn][:], v4(bY), AF.Copy, reads=[R_ps[bY]], writes=[R_Y[yn]])
                    yield
                    bM = bank()
                    for h in range(4):
                        mm(ps[bM][:, h * 128:(h + 1) * 128], Xb[xn][:, h, :], Mb[mc][:, h, :], True, True, reads=[R_X[xn], R_M[mc]], writes=[R_ps[bM]])
                    tt(Mb[mn][:], v4(bM), Mb[mc][:], ALU.add, reads=[R_ps[bM], R_M[mc]], writes=[R_M[mn]])
                    xc, yc, mc = xn, (yn if lvl < 4 else yc), mn
                    yield
                M, R_Mf = Mb[mc], R_M[mc]
                Mfin[i] = (M, R_Mf)
                bW = bank()
                for h in range(4):
                    mm(ps[bW][:, h * 128:(h + 1) * 128], kbg[:, h, :], M[:, h, :], True, True, reads=[R_kbg, R_Mf], writes=[R_ps[bW]])
                act(wTn[:], v4(bW), AF.Copy, scale=-1.0, reads=[R_ps[bW]], writes=[R_wTn])
                yield

            def scan_tile(i):
                D_ = B[i % NSET]
                gs, R_gs = D_["gs"], D_["R_gs"]
                qkT, kdd, vb, wTn = (D_[k] for k in ("qkT", "kdd", "vb", "wTn"))
                R_qkT, R_kdd, R_vb, R_wTn = (D_["R_" + k] for k in ("qkT", "kdd", "vb", "wTn"))
                M, R_Mf = Mfin[i]
                tsl = slice(i * 128, (i + 1) * 128)
                Rq = [R_gq[h][i // 4] for h in range(4)]
                decS = gs[:, 32:40].rearrange("p (h j) -> p h j", h=4)
                for jc in range(2):
                    rows = slice(jc * 64, (jc + 1) * 64)
                    so, sn = sbi[0], 1 - sbi[0]
                    bV = bank()
                    for h in range(4):
                        mm(ps[bV][:, h * 128:(h + 1) * 128], M[:, h, :], vb[:, h, :], True, False, reads=[R_Mf, R_vb], writes=[R_ps[bV]])
                        mm(ps[bV][:, h * 128:(h + 1) * 128], wTn[:, h, :], Sbf[so][:, h, :], False, True, reads=[R_wTn, R_Sbf[so]], writes=[R_ps[bV]])
                    act(vnew[rows, :, :], ps[bV][rows, :].rearrange("p (a b) -> p a b", a=4), AF.Copy, reads=[R_ps[bV]], writes=[R_vnew])
                    tt(Stmp[:], Sst[:], decS[:, :, jc].unsqueeze(2).to_broadcast([128, 4, 128]), ALU.mult, reads=[R_S, R_gs], writes=[R_Stmp])
                    yield
                    bO = bank()
                    for h in range(4):
                        mm(ps[bO][:, h * 128:(h + 1) * 128], gq[:, h, tsl], Sbf[so][:, h, :], True, True, reads=[Rq[h], R_Sbf[so]], writes=[R_ps[bO]])
                    tt(osb[rows, :, :], ps[bO][rows, :].rearrange("p (a b) -> p a b", a=4), gs[rows, 8:12].unsqueeze(2).to_broadcast([64, 4, 128]),
                       ALU.mult, reads=[R_ps[bO], R_gs], writes=[R_osb])
                    yield
                    bD = bank()
                    for h in range(4):
                        mm(ps[bD][:, h * 128:(h + 1) * 128], kdd[rows, h, :], vnew[rows, h, :], True, True, reads=[R_kdd, R_vnew], writes=[R_ps[bD]])
                    tt(Sbf[sn][:], v4(bD), Stmp[:], ALU.add, reads=[R_ps[bD], R_Stmp], writes=[R_Sbf[sn]])
                    tt(Sst[:], v4(bD), Stmp[:], ALU.add, reads=[R_ps[bD], R_Stmp], writes=[R_S])
                    sbi[0] = sn
                    yield
                bO2 = bank()
                for h in range(4):
                    mm(ps[bO2][:, h * 128:(h + 1) * 128], qkT[:, h, :], vnew[:, h, :], True, True, reads=[R_qkT, R_vnew], writes=[R_ps[bO2]])
                tt(ofin[:], v4(bO2), osb[:], ALU.add, reads=[R_ps[bO2], R_osb], writes=[R_ofin])
                for h in range(4):
                    act(junk2[:, 0:128], ofin[:, h, :], AF.Square, accum_out=gs[:, 40 + h:41 + h], reads=[R_ofin], writes=[R_gs])
                act(gs[:, 44:48], gs[:, 40:44], AF.Ln, bias=eps128_ap, scale=1.0 / 128, reads=[R_gs], writes=[R_gs])
                act(gs[:, 44:48], gs[:, 44:48], AF.Exp, scale=-0.5, reads=[R_gs], writes=[R_gs])
                tt(onb[:], ofin[:], b4(gs[:, 44:48]), ALU.mult, reads=[R_ofin, R_gs], writes=[R_onb])
                if "ogdn" in dbg_d and seq == 0:
                    S.dma("sp", dbg_d["ogdn"][:, i * 512:(i + 1) * 512], ofin[:].rearrange("p a b -> p (a b)"), reads=[R_ofin])
                yield
                bT = bank()
                pT = ps[bT][:].bitcast(BF16)
                for h in range(4):
                    transpose(pT[:, h * 128:(h + 1) * 128], onb[:, h, :], ident, reads=[R_onb], writes=[R_ps[bT]])
                stt(mixT[:, 4:8, tsl], pT[:, 0:512].rearrange("p (a b) -> p a b", a=4), gng[:, 0:1], zT[:, :, tsl], ALU.mult, ALU.mult,
                    reads=[R_ps[bT], R_const] + [R_zT[h][i // 4] for h in range(4)], writes=[R_mix[1][i]])
                yield

            pre_done = set()
            scan_done = -1
            active_pre = {}
            cur_scan, cur_i, next_pre = None, 0, 0
            while cur_i < NT:
                while next_pre < NT and len(active_pre) < NSET - 1 and next_pre - NSET <= scan_done:
                    active_pre[next_pre] = pre_tile(next_pre)
                    next_pre += 1
                if cur_scan is None and cur_i in pre_done:
                    cur_scan = scan_tile(cur_i)
                if cur_scan is not None:
                    try:
                        next(cur_scan)
                    except StopIteration:
                        scan_done = cur_i
                        cur_i += 1
                        cur_scan = None
                for k_ in list(active_pre):
                    try:
                        next(active_pre[k_])
                    except StopIteration:
                        pre_done.add(k_)
                        del active_pre[k_]
            if "mixG" in dbg_d and seq == 0:
                for k in range(4):
                    for q4 in range(4):
                        cp(ofin[:].rearrange("p a b -> p (a b)"), mixT[:, 4 + k, q4 * 512:(q4 + 1) * 512], reads=R_mix[1] + [R_ofin], writes=[R_ofin])
                        S.dma("sp", dbg_d["mixG"][:, k * T + q4 * 512:k * T + (q4 + 1) * 512], ofin[:].rearrange("p a b -> p (a b)"), reads=[R_ofin], writes=[R_ofin])
          S.dead = False
          S.barrier()


        if stage >= 3:
          with contextlib.ExitStack() as ph:
            def psb(name, shape, dt):
                return ph.enter_context(nc.sbuf_tensor(f"s_o{name}_{seq}", list(shape), dt))
            wo = psb("wo", [128, 8, D], BF16)
            R_wo = Res()
            for q4 in range(4):
                S.dma("pool", wo[:, 2 * q4:2 * q4 + 2, :].rearrange("p a b -> p (a b)"), wout_d[:, q4 * 2048:(q4 + 1) * 2048], writes=[R_wo])
            xt = [psb(f"xt{i}", [128, D], F32) for i in range(2)]
            x2t = [psb(f"x2t{i}", [128, D], F32) for i in range(2)]
            R_xt, R_x2t = [Res(), Res()], [Res(), Res()]
            hn = psb("hn", [128, D], BF16)
            R_hn = Res()
            junk = psb("junk", [128, D], BF16)
            stat = psb("stat", [128, 8], F32)
            R_stat = Res()
            pend_fin = []
            for i in range(NT):
                b = i % 2
                tsl = slice(i * 128, (i + 1) * 128)
                S.dma("sp", xt[b][:], x_d[seq, tsl, :], writes=[R_xt[b]])
                for half in range(2):
                    bk = (2 * i + half) % 6
                    for kc in range(8):
                        mm(ps[bk][:], mixT[:, kc, tsl], wo[:, kc, half * 512:(half + 1) * 512], kc == 0, kc == 7,
                           reads=[R_mix[kc // 4][i], R_wo], writes=[R_ps[bk]])
                    tt(x2t[b][:, half * 512:(half + 1) * 512], ps[bk][:], xt[b][:, half * 512:(half + 1) * 512], ALU.add,
                       reads=[R_ps[bk], R_xt[b]], writes=[R_x2t[b]])
                if pend_fin:
                    pend_fin.pop(0)()
                S.dma("sp", x2_d[seq, tsl, :], x2t[b][:], reads=[R_x2t[b]], writes=[R_x2s[i]])
                act(junk[:], x2t[b][:], AF.Square, accum_out=stat[:, 0:1], reads=[R_x2t[b]], writes=[R_stat])
                rstd_from_ss(stat[:, 1:2], stat[:, 0:1], D, [R_stat])
                ts(hn[:], x2t[b][:], stat[:, 1:2], None, ALU.mult, reads=[R_x2t[b], R_stat], writes=[R_hn])
                def fin_(tsl=tsl, i=i):
                    pb = ps[7][:].bitcast(BF16)
                    for k in range(8):
                        transpose(pb[:, k * 128:(k + 1) * 128], hn[:, k * 128:(k + 1) * 128], ident, reads=[R_hn], writes=[R_ps[7]])
                    tt(hT[:, :, tsl], pb.rearrange("p (k t) -> p k t", k=8), g2T[:].unsqueeze(2).to_broadcast([128, 8, 128]), ALU.mult,
                       reads=[R_ps[7], R_const], writes=[R_hT[i]])
                pend_fin.append(fin_)
            pend_fin.pop(0)()
          S.barrier()

        if stage >= 4:
          with contextlib.ExitStack() as ph:
            def psb(name, shape, dt):
                return ph.enter_context(nc.sbuf_tensor(f"s_f{name}_{seq}", list(shape), dt))
            actT = psb("actT", [128, NFC, 1024], BF16)
            R_act = [[Res() for _ in range(2)] for _ in range(NFC)]
            wd = psb("wd", [128, NFC, D], BF16)
            R_wd = [Res() for _ in range(NFC)]
            wgu = [psb(f"wgu{i}", [128, 1024], BF16) for i in range(6)]
            R_wgu = [Res() for _ in range(6)]
            sgt = [psb(f"sgt{i}", [128, 512], BF16) for i in range(2)]
            R_sgt = [Res(), Res()]
            x2t = [psb(f"x2t{i}", [128, D], F32) for i in range(2)]
            x3t = [psb(f"x3t{i}", [128, D], F32) for i in range(2)]
            R_x2t, R_x3t = [Res(), Res()], [Res(), Res()]
            junk = psb("junk", [128, D], BF16)
            stat = psb("stat", [128, 8], F32)
            R_stat = Res()
            wi = 0
            si = 0
            for th in range(2):
                for fc in range(NFC):
                    rg = wi % 6
                    ru = (wi + 1) % 6
                    wi += 2
                    S.dma("pool", wgu[rg][:], wg_d[fc], writes=[R_wgu[rg]])
                    S.dma("pool", wgu[ru][:], wu_d[fc], writes=[R_wgu[ru]])
                    if th == 0:
                        S.dma("pool", wd[:, fc, :], wd_d[:, fc * D:(fc + 1) * D], writes=[R_wd[fc]])
                    for tb in range(2):
                        t0 = th * 1024 + tb * 512
                        bg_, bu_ = (4 * fc + 2 * tb) % 6, (4 * fc + 2 * tb + 1) % 6
                        for kc in range(8):
                            mm(ps[bg_][:], wgu[rg][:, kc * 128:(kc + 1) * 128], hT[:, kc, t0:t0 + 512], kc == 0, kc == 7,
                               reads=[R_wgu[rg]] + R_hT[t0 // 128:t0 // 128 + 4], writes=[R_ps[bg_]])
                        for kc in range(8):
                            mm(ps[bu_][:], wgu[ru][:, kc * 128:(kc + 1) * 128], hT[:, kc, t0:t0 + 512], kc == 0, kc == 7,
                               reads=[R_wgu[ru]] + R_hT[t0 // 128:t0 // 128 + 4], writes=[R_ps[bu_]])
                        sb_ = si % 2
                        si += 1
                        act(sgt[sb_][:], ps[bg_][:], AF.Silu, reads=[R_ps[bg_]], writes=[R_sgt[sb_]])
                        tt(actT[:, fc, tb * 512:(tb + 1) * 512], ps[bu_][:], sgt[sb_][:], ALU.mult, reads=[R_ps[bu_], R_sgt[sb_]], writes=[R_act[fc][tb]])
                for tl in range(8):
                    i = th * 8 + tl
                    b = i % 2
                    tsl = slice(i * 128, (i + 1) * 128)
                    S.dma("sp", x2t[b][:], x2_d[seq, tsl, :], reads=[R_x2s[i]], writes=[R_x2t[b]])
                    for half in range(2):
                        bk = 6 + half
                        for fc in range(NFC):
                            mm(ps[bk][:], actT[:, fc, tl * 128:(tl + 1) * 128], wd[:, fc, half * 512:(half + 1) * 512], fc == 0, fc == NFC - 1,
                               reads=[R_act[fc][tl // 4], R_wd[fc]], writes=[R_ps[bk]])
                        tt(x3t[b][:, half * 512:(half + 1) * 512], ps[bk][:], x2t[b][:, half * 512:(half + 1) * 512], ALU.add,
                           reads=[R_ps[bk], R_x2t[b]], writes=[R_x3t[b]])
                    act(junk[:], x3t[b][:], AF.Square, accum_out=stat[:, 0:1], reads=[R_x3t[b]], writes=[R_stat])
                    rstd_from_ss(stat[:, 1:2], stat[:, 0:1], D, [R_stat])
                    stt(x3t[b][:], x3t[b][:], stat[:, 1:2], fgb[:], ALU.mult, ALU.mult, reads=[R_x3t[b], R_stat, R_const], writes=[R_x3t[b]])
                    S.dma("sp", out_d[seq, tsl, :], x3t[b][:], reads=[R_x3t[b]], writes=[R_x3t[b]])
          S.barrier()

    S.barrier()
    S.run()
    es.close()
    return nc


def _bf(a):
    return np.asarray(a, dtype=np.float32).astype(ml_dtypes.bfloat16)


def _tile_w(w):
    return np.ascontiguousarray(w.reshape(8, 128, -1).transpose(1, 0, 2))


def _consts():
    c = {}
    ident = np.eye(128, dtype=np.float32)
    ones = np.ones((128, 128), np.float32)
    k = np.arange(128)[:, None]
    t = np.arange(128)[None, :]
    causal = np.where(k <= t, 0.0, NEG).astype(np.float32)
    anti = np.where(k > t, 0.0, NEG).astype(np.float32)
    same = (k // 64) == (t // 64)
    maskA = np.where(same & (t < k), 0.0, -NEG).astype(np.float32)
    maskQ = np.where(same & (t >= k), 0.0, NEG).astype(np.float32)
    n = np.arange(128)[:, None]
    s = np.arange(32)[None, :]
    ovl = ((16 * n < 64 * s + 64) & (16 * n + 32 > 64 * s) & (n < 127)).astype(np.float32)
    esel = np.zeros((128, 2048), np.float32)
    esel[:32] = (np.arange(2048)[None, :] // 64 == np.arange(32)[:, None]).astype(np.float32)
    tt = np.arange(2048)[None, :]
    cmpmask = np.where((16 * n + 31 <= tt) & (n < 127), 0.0, NEG).astype(np.float32)
    c["cbf"] = _bf(np.concatenate([ident, ones, causal, anti, np.tile(maskA, (1, 4)), np.tile(maskQ, (1, 4)), ovl, esel, cmpmask, np.tile(ident, (1, 4)), (k <= t).astype(np.float32), (k > t).astype(np.float32)], axis=1))
    assert c["cbf"].shape[1] == NCBF
    ltriT = (same & (k <= t)).astype(np.float32)
    bones = same.astype(np.float32)
    cind = (np.arange(128)[:, None] // 64 == np.arange(2)[None, :]).astype(np.float32)
    c["cf32"] = np.ascontiguousarray(np.concatenate([ltriT, bones, ones, cind], axis=1))
    tq = np.arange(2048)[:, None]
    cur = tq // 64
    j = np.arange(32)[None, :]
    forced = (j == 0) | (j == cur) | (j == cur - 1)
    caus = j <= cur
    selb = np.where(forced, 1e9, np.where(caus, 0.0, -1e30)).astype(np.float32)
    c["selc"] = np.ascontiguousarray(selb.reshape(16, 128, 32).transpose(1, 0, 2).reshape(128, 16 * 32))
    inv = (500000.0 ** (-np.arange(8, dtype=np.float64) * (2.0 / 16))) / (2 * np.pi)
    rc = np.zeros((128, 4), np.float32)
    for base in (0, 64):
        rc[base:base + 8, 0] = -inv
        rc[base + 8:base + 16, 0] = inv
        rc[base:base + 8, 1] = inv
        rc[base + 8:base + 16, 1] = inv
    rc[:, 2] = 0.25
    c["ropec"] = rc
    return c


def _swap_cols(wq, nheads):
    out = np.zeros_like(wq)
    for h in range(nheads):
        b = h * 64
        out[:, b:b + 8] = wq[:, b + 8:b + 16]
        out[:, b + 8:b + 16] = wq[:, b:b + 8]
    return out


def prep_shared(inp):
    w_in = np.asarray(inp["w_in"], np.float32)[0]
    sh = dict(_consts())
    cols = []
    wq = w_in[:, 0:512]
    cols.append(wq)
    cols.append(_swap_cols(wq, 8))
    kd, ksd = [], []
    for kind in (0, 2, 4):
        for g in range(2):
            o = OFF_KV + kind * 128 + g * 64
            wk = w_in[:, o:o + 64]
            kd.append(np.concatenate([wk, wk], axis=1))
            ws = _swap_cols(wk, 1)
            ksd.append(np.concatenate([ws, ws], axis=1))
    cols += kd + ksd
    cols.append(w_in[:, OFF_KV + 128:OFF_KV + 256])
    cols.append(w_in[:, OFF_GQKV:OFF_GQKV + 1536])
    cols.append(w_in[:, OFF_GZ:OFF_GZ + 512])
    wfm = np.concatenate(cols, axis=1)
    assert wfm.shape[1] == NFM * 128
    wt = _tile_w(wfm)
    sh["wfm"] = np.ascontiguousarray(wt.reshape(128, 8, NFM, 128).transpose(2, 0, 1, 3).reshape(NFM, 128, 1024))
    wtm = np.concatenate([w_in[:, OFF_KV + 3 * 128:OFF_KV + 4 * 128], w_in[:, OFF_KV + 5 * 128:OFF_KV + 6 * 128],
                          w_in[:, OFF_GATE:OFF_GATE + 24], w_in[:, OFF_GB:OFF_GB + 4], w_in[:, OFF_GA:OFF_GA + 4]], axis=1)
    assert wtm.shape[1] == NTM
    sh["wtm"] = np.ascontiguousarray(_tile_w(wtm).reshape(128, 8 * NTM))
    sh["g1"] = np.ascontiguousarray(np.asarray(inp["norm1_g"], np.float32)[0].reshape(8, 128).T)
    w1 = np.asarray(inp["cmp_w1"], np.float32)[0]
    w1t = w1.reshape(2, 32, 64, 128).transpose(2, 0, 1, 3).reshape(64, 2 * 32 * 128)
    sh["w1"] = np.ascontiguousarray(np.concatenate([w1t, w1t], axis=0))
    w2 = np.asarray(inp["cmp_w2"], np.float32)[0]
    sh["w2"] = np.ascontiguousarray(np.concatenate([w2[0], w2[0], w2[1]], axis=1))
    cp = np.asarray(inp["cmp_pos"], np.float32)[0]
    sh["posT"] = np.ascontiguousarray(cp.transpose(2, 0, 1).reshape(64, 64))
    sh["gdnc"] = np.ascontiguousarray(np.concatenate([np.asarray(inp["gdn_a_log"], np.float32)[0], np.asarray(inp["gdn_dt_bias"], np.float32)[0]]).reshape(1, 8))
    cw = np.asarray(inp["gdn_conv_w"], np.float32)[0]
    sh["convw"] = np.ascontiguousarray(cw.reshape(4, 12, 128).transpose(2, 1, 0).reshape(128, 48))
    sh["gng"] = np.ascontiguousarray(np.asarray(inp["gdn_norm_g"], np.float32)[0].reshape(128, 1))
    sh["wout"] = np.ascontiguousarray(_tile_w(np.asarray(inp["w_out"], np.float32)[0]).reshape(128, 8 * D))
    sh["g2"] = np.ascontiguousarray(np.asarray(inp["norm2_g"], np.float32)[0].reshape(8, 128).T)
    sh["fg"] = np.ascontiguousarray(np.asarray(inp["final_g"], np.float32).reshape(1, D))
    for nm, key in (("wg", "w_gate"), ("wu", "w_up")):
        wt_ = _tile_w(np.asarray(inp[key], np.float32)[0])
        sh[nm] = np.ascontiguousarray(wt_.reshape(128, 8, NFC, 128).transpose(2, 0, 1, 3).reshape(NFC, 128, 1024))
    wdn = np.asarray(inp["w_down"], np.float32)[0]
    sh["wd"] = np.ascontiguousarray(wdn.reshape(NFC, 128, D).transpose(1, 0, 2).reshape(128, NFC * D))
    sh["nsag"] = np.ascontiguousarray(np.asarray(inp["nsa_norm_g"], np.float32)[0].reshape(4, 128).T)
    return sh


def kernel(**inputs):
    x = np.asarray(inputs["x"], np.float32)
    pos = np.asarray(inputs["positions"], np.int32)
    sh = prep_shared(inputs)
    nc = build_program()
    in_maps = []
    for c in range(NCORES):
        m = dict(sh)
        m["x"] = np.ascontiguousarray(x[c * NSEQ:(c + 1) * NSEQ])
        m["pos"] = np.ascontiguousarray(pos[c * NSEQ:(c + 1) * NSEQ])
        in_maps.append(m)
    res = run_bass_kernel_spmd(nc, in_maps, core_ids=list(range(NCORES)))
    return np.concatenate([r["out"] for r in res.results], axis=0)
```

```python
import contextlib
import numpy as np
import ml_dtypes
import concourse.bass as bass
import concourse.mybir as mybir
from concourse.bass_utils import run_bass_kernel_spmd

F32 = mybir.dt.float32
BF16 = mybir.dt.bfloat16
I32 = mybir.dt.int32
AF = mybir.ActivationFunctionType
ALU = mybir.AluOpType
AX = mybir.AxisListType

NCORES = 8
NSEQ = 2
T = 2048
D = 1024
NT = T // 128
DFF = 2816
NFC = DFF // 128
NEG = -30000.0
EPS = 1e-6
OFF_KV = 512
OFF_GATE = OFF_KV + 768
OFF_GQKV = OFF_GATE + 24
OFF_GZ = OFF_GQKV + 1536
OFF_GB = OFF_GZ + 512
OFF_GA = OFF_GB + 4
FM_Q, FM_QSW, FM_K, FM_KSW, FM_VC, FM_GQKV, FM_Z = 0, 4, 8, 14, 20, 21, 33
NFM = 37
NTM = 288
NCBF = 1568 + 2048 + 2048 + 512 + 256


class Res:
    __slots__ = ("name", "w", "r")

    def __init__(self, name=""):
        self.name = name
        self.w = {}
        self.r = {}


class Sched:
    ENGS = ("pe", "act", "dve", "pool", "sp")

    def __init__(self, nc, ring=6):
        self.nc = nc
        self.prog = {k: [] for k in self.ENGS}
        self.esem = {k: nc.alloc_semaphore(name=f"es_{k}") for k in self.ENGS}
        self.ecnt = {k: 0 for k in self.ENGS}
        self.known = {k: {} for k in self.ENGS}
        self.R = ring
        self.rsem = {q: [nc.alloc_semaphore(name=f"rs_{q}{i}") for i in range(ring)] for q in ("sp", "pool", "act")}
        self.rcnt = {q: [0] * ring for q in self.rsem}
        self.rpos = {q: 0 for q in self.rsem}
        self.n_wait = 0
        self.dead = False

    def _collect(self, reads, writes):
        deps = {}

        def add(d):
            for k, (s, v) in d.items():
                if k not in deps or deps[k][1] < v:
                    deps[k] = (s, v)

        for r in reads:
            add(r.w)
        for w in writes:
            add(w.w)
            add(w.r)
        return deps

    def _emit_waits(self, eng, deps):
        kn = self.known[eng]
        for k, (s, v) in deps.items():
            if kn.get(k, 0) < v:
                kn[k] = v
                self.n_wait += 1
                self.prog[eng].append(lambda e, s=s, v=v: e.wait_ge(s, v))

    def op(self, eng, fn, reads=(), writes=()):
        if self.dead:
            return
        deps = self._collect(reads, writes)
        if eng in deps and eng == "pe":
            del deps[eng]
        self._emit_waits(eng, deps)
        self.ecnt[eng] += 1
        cnt = self.ecnt[eng]
        sem = self.esem[eng]
        self.prog[eng].append(lambda e, fn=fn, sem=sem: fn(e).then_inc(sem, 1))
        tok = (sem, cnt)
        for r in reads:
            r.r[eng] = tok
        for w in writes:
            w.w = {eng: tok}
            w.r = {}

    def dma(self, q, out, in_, reads=(), writes=()):
        if self.dead:
            return
        j = self.rpos[q] % self.R
        self.rpos[q] += 1
        sem = self.rsem[q][j]
        key = ("ring", q, j)
        deps = self._collect(reads, writes)
        if self.rcnt[q][j] > 0:
            deps[key] = (sem, 16 * self.rcnt[q][j])
        self._emit_waits(q, deps)
        self.rcnt[q][j] += 1
        val = 16 * self.rcnt[q][j]
        self.prog[q].append(lambda e, out=out, in_=in_, sem=sem: e.dma_start(out=out, in_=in_).then_inc(sem, 16))
        tok = (sem, val)
        for r in reads:
            r.r[key] = tok
        for w in writes:
            w.w = {key: tok}
            w.r = {}

    def barrier(self):
        deps = {}
        for k in self.ENGS:
            if self.ecnt[k]:
                deps[k] = (self.esem[k], self.ecnt[k])
        for q in self.rsem:
            for j in range(self.R):
                if self.rcnt[q][j]:
                    deps[("ring", q, j)] = (self.rsem[q][j], 16 * self.rcnt[q][j])
        for e in self.ENGS:
            d = dict(deps)
            d.pop(e, None)
            self._emit_waits(e, d)

    def run(self):
        nc = self.nc
        with nc.Block() as block:
            @block.tensor
            def _(e):
                for f in self.prog["pe"]:
                    f(e)

            @block.scalar
            def _(e):
                for f in self.prog["act"]:
                    f(e)

            @block.vector
            def _(e):
                for f in self.prog["dve"]:
                    f(e)

            @block.gpsimd
            def _(e):
                for f in self.prog["pool"]:
                    f(e)

            @block.sync
            def _(e):
                for f in self.prog["sp"]:
                    f(e)


def interleave(gens):
    gens = list(gens)
    while gens:
        for g_ in list(gens):
            try:
                next(g_)
            except StopIteration:
                gens.remove(g_)


def build_program(nseq=NSEQ, stage=99, dbg=(), nsa_attn=True, gdn_cut=0, mixers=True):
    nc = bass.Bass("TRN2", target_bir_lowering=False)
    S = Sched(nc)
    es = contextlib.ExitStack()

    def dram(name, shape, dt, kind="ExternalInput"):
        return nc.dram_tensor(name, list(shape), dt, kind=kind).ap()

    def sb(name, shape, dt):
        return es.enter_context(nc.sbuf_tensor("s_" + name, list(shape), dt))

    x_d = dram("x", [nseq, T, D], F32)
    pos_d = dram("pos", [nseq, T], I32)
    wfm_d = dram("wfm", [NFM, 128, 1024], F32)
    wtm_d = dram("wtm", [128, 8 * NTM], F32)
    g1_d = dram("g1", [128, 8], F32)
    ropec_d = dram("ropec", [128, 4], F32)
    cbf_d = dram("cbf", [128, NCBF], BF16)
    selc_d = dram("selc", [128, NT * 32], F32)
    w1_d = dram("w1", [128, 2 * 32 * 128], F32)
    w2_d = dram("w2", [128, 128 + 64], F32)
    posT_d = dram("posT", [64, 2 * 32], F32)
    nsag_d = dram("nsag", [128, 4], F32)
    gdnc_d = dram("gdnc", [1, 8], F32)
    cf32_d = dram("cf32", [128, 386], F32)
    convw_d = dram("convw", [128, 48], F32)
    gng_d = dram("gng", [128, 1], F32)
    wout_d = dram("wout", [128, 8 * D], F32)
    g2_d = dram("g2", [128, 8], F32)
    fg_d = dram("fg", [1, D], F32)
    wg_d = dram("wg", [NFC, 128, 1024], F32)
    wu_d = dram("wu", [NFC, 128, 1024], F32)
    wd_d = dram("wd", [128, NFC * D], F32)
    x2_d = dram("x2s", [nseq, T, D], F32, kind="Internal")
    R_x2s = [Res() for _ in range(NT)]
    out_d = dram("out", [nseq, T, D], F32, kind="ExternalOutput")
    dbg_d = {}
    for name, shape in dbg:
        dbg_d[name] = dram("dbg_" + name, shape, F32, kind="ExternalOutput")

    cbf = sb("cbf", [128, NCBF], BF16)
    ident = cbf[:, 0:128]
    ones_bf = cbf[:, 128:256]
    causal = cbf[:, 256:384]
    anti = cbf[:, 384:512]
    maskA4 = cbf[:, 512:1024]
    maskQ4 = cbf[:, 1024:1536]
    ovl = cbf[:, 1536:1568]
    esel = cbf[:, 1568:1568 + 2048]
    cmpmask = cbf[:, 3616:3616 + 2048]
    ident4 = cbf[:, 5664:5664 + 512]
    causal01 = cbf[:, 6176:6304]
    anti01 = cbf[:, 6304:6432]
    cf32 = sb("cf32", [128, 3 * 128 + 2], F32)
    ltriT = cf32[:, 0:128]
    bones = cf32[:, 128:256]
    ones_f = cf32[:, 256:384]
    cind = cf32[:, 384:386]
    convw = sb("convw", [128, 12, 4], F32)
    gng = sb("gng", [128, 1], F32)
    g2T = sb("g2T", [128, 8], F32)
    fgb = sb("fgb", [128, D], F32)
    selc = sb("selc", [128, NT, 32], F32)
    g1T = sb("g1T", [128, 8], F32)
    ropec = sb("ropec", [128, 4], F32)
    cst = sb("cst", [128, 8], F32)
    nsag = sb("nsag", [128, 4], F32)
    hT = sb("hT", [128, 8, T], BF16)
    mixT = sb("mixT", [128, 8, T], BF16)
    beta_all = sb("beta_all", [128, NT, 4], F32)
    g_all = sb("g_all", [128, NT, 4], F32)
    R_bg = [Res() for _ in range(NT)]
    dtb = sb("dtb", [128, 4], F32)
    negA = sb("negA", [128, 4], F32)
    junk2 = sb("junk2", [128, 512], BF16)
    R_const = Res("const")
    R_hT = [Res(f"hT{i}") for i in range(NT)]
    R_mix = [[Res(f"mix{c}_{i}") for i in range(NT)] for c in range(2)]

    ps = [es.enter_context(nc.psum_tensor(f"ps{i}", [128, 512], F32)) for i in range(8)]
    R_ps = [Res(f"ps{i}") for i in range(8)]

    S.dma("sp", cbf[:], cbf_d[:], writes=[R_const])
    S.dma("sp", selc[:].rearrange("p a b -> p (a b)"), selc_d[:], writes=[R_const])
    S.dma("sp", g1T[:], g1_d[:], writes=[R_const])
    S.dma("sp", ropec[:], ropec_d[:], writes=[R_const])
    S.dma("sp", nsag[:], nsag_d[:], writes=[R_const])
    S.dma("sp", cf32[:], cf32_d[:], writes=[R_const])
    S.dma("sp", convw[:].rearrange("p a b -> p (a b)"), convw_d[:], writes=[R_const])
    S.dma("sp", gng[:], gng_d[:], writes=[R_const])
    S.dma("sp", g2T[:], g2_d[:], writes=[R_const])
    S.dma("sp", fgb[:], fg_d[0:1, :].to_broadcast([128, D]), writes=[R_const])
    S.dma("sp", dtb[:], gdnc_d[0:1, 4:8].to_broadcast([128, 4]), writes=[R_const])
    S.dma("sp", negA[:], gdnc_d[0:1, 0:4].to_broadcast([128, 4]), writes=[R_const])
    S.op("dve", lambda e: e.memset(cst[:, 0:1], EPS), writes=[R_const])
    S.op("dve", lambda e: e.memset(cst[:, 1:2], 1.0), writes=[R_const])
    S.op("dve", lambda e: e.memset(cst[:, 2:3], 0.0), writes=[R_const])
    S.op("dve", lambda e: e.memset(cst[:, 3:4], 1e-30), writes=[R_const])
    S.op("dve", lambda e: e.memset(cst[:, 4:5], EPS * 128), writes=[R_const])
    eps_ap, one_ap, zero_ap, tiny_ap, eps128_ap = cst[:, 0:1], cst[:, 1:2], cst[:, 2:3], cst[:, 3:4], cst[:, 4:5]

    def act(out, in_, func, bias=None, scale=1.0, accum_out=None, reads=(), writes=(), eng="act"):
        kw = {}
        if bias is not None:
            kw["bias"] = bias
        if accum_out is not None:
            kw["accum_out"] = accum_out
        S.op("act", lambda e: e.activation(out=out, in_=in_, func=func, scale=scale, **kw), reads=list(reads) + [R_const], writes=writes)

    def mm(out, lhsT, rhs, start, stop, reads=(), writes=(), skip=False):
        S.op("pe", lambda e: e.matmul(out, lhsT, rhs, start=start, stop=stop, skip_group_check=skip), reads=reads, writes=writes)

    def transpose(out, in_, idn, reads=(), writes=()):
        S.op("pe", lambda e: e.transpose(out, in_, idn), reads=list(reads) + [R_const], writes=writes)

    def rstd_from_ss(rstd, ss, n, reads_writes):
        act(rstd, ss, AF.Ln, bias=eps_ap, scale=1.0 / n, reads=reads_writes, writes=reads_writes)
        act(rstd, rstd, AF.Exp, scale=-0.5, reads=reads_writes, writes=reads_writes)

    act(negA[:], negA[:], AF.Exp, reads=[R_const], writes=[R_const])
    S.op("dve", lambda e: e.tensor_scalar(out=negA[:], in0=negA[:], scalar1=-1.0, scalar2=None, op0=ALU.mult), reads=[R_const], writes=[R_const])

    def tt(out, in0, in1, op, reads=(), writes=(), eng="dve"):
        S.op(eng, lambda e: e.tensor_tensor(out=out, in0=in0, in1=in1, op=op), reads=reads, writes=writes)

    def ts(out, in0, s1, s2, op0, op1=None, reads=(), writes=(), eng="dve"):
        if op1 is None:
            S.op(eng, lambda e: e.tensor_scalar(out=out, in0=in0, scalar1=s1, scalar2=None, op0=op0), reads=reads, writes=writes)
        else:
            S.op(eng, lambda e: e.tensor_scalar(out=out, in0=in0, scalar1=s1, scalar2=s2, op0=op0, op1=op1), reads=reads, writes=writes)

    def stt(out, in0, scalar, in1, op0, op1, reads=(), writes=()):
        S.op("dve", lambda e: e.scalar_tensor_tensor(out=out, in0=in0, scalar=scalar, in1=in1, op0=op0, op1=op1), reads=reads, writes=writes)

    def cp(out, in_, reads=(), writes=(), eng="dve"):
        S.op(eng, lambda e: e.tensor_copy(out=out, in_=in_), reads=reads, writes=writes)

    def memset(ap, val, writes=(), eng="pool"):
        S.op(eng, lambda e: e.memset(ap, val), writes=writes)

    def max8(out, in_, reads=(), writes=()):
        S.op("dve", lambda e: e.max(out=out, in_=in_), reads=reads, writes=writes)

    def recip(out, in_, reads=(), writes=()):
        S.op("dve", lambda e: e.reciprocal(out=out, in_=in_), reads=reads, writes=writes)

    if not mixers:
        nsa_attn = False
        S.op("pool", lambda e: e.memset(mixT[:], 0.0), writes=[x for y in R_mix for x in y])
    for seq in range(nseq):
        with contextlib.ExitStack() as ph:
            def psb(name, shape, dt):
                return ph.enter_context(nc.sbuf_tensor(f"s_{name}_{seq}", list(shape), dt))

            tab = psb("tab", [128, 2, T], F32)
            R_tab = Res()
            with contextlib.ExitStack() as ph0:
                def psb0(name, shape, dt):
                    return ph0.enter_context(nc.sbuf_tensor(f"s_{name}_{seq}", list(shape), dt))
                xt = [psb0(f"xt{i}", [128, D], F32) for i in range(2)]
                R_xt = [Res(), Res()]
                junk = psb0("junk", [128, D], BF16)
                hn = psb0("hn", [128, D], BF16)
                R_hn = Res()
                stat = psb0("stat", [128, 8], F32)
                R_stat = Res()
                posi = psb0("posi", [128, T], I32)
                tmpF = psb0("tmpF", [128, 2, T], F32)
                tmpI = psb0("tmpI", [128, 2, T], I32)
                S.dma("sp", posi[:], pos_d[seq:seq + 1, :].to_broadcast([128, T]), writes=[R_tab])
                ts(tmpF[:, 0, :], posi[:], ropec[:, 0:1], None, ALU.mult, reads=[R_tab, R_const], writes=[R_tab])
                ts(tmpF[:, 1, :], posi[:], ropec[:, 1:2], ropec[:, 2:3], ALU.mult, ALU.add, reads=[R_tab, R_const], writes=[R_tab])
                cp(tmpI[:], tmpF[:], reads=[R_tab], writes=[R_tab])
                cp(tab[:], tmpI[:], reads=[R_tab], writes=[R_tab])
                tt(tmpF[:], tmpF[:], tab[:], ALU.subtract, reads=[R_tab], writes=[R_tab])
                ts(tab[:], tmpF[:], 0.5, None, ALU.is_gt, reads=[R_tab], writes=[R_tab])
                tt(tmpF[:], tmpF[:], tab[:], ALU.subtract, reads=[R_tab], writes=[R_tab])
                ts(tab[:], tmpF[:], -0.5, None, ALU.is_lt, reads=[R_tab], writes=[R_tab])
                tt(tmpF[:], tmpF[:], tab[:], ALU.add, reads=[R_tab], writes=[R_tab])
                act(tab[:], tmpF[:], AF.Sin, scale=6.283185, reads=[R_tab], writes=[R_tab])
                for i in range(NT):
                    b = i % 2
                    S.dma("sp", xt[b][:], x_d[seq, i * 128:(i + 1) * 128, :], writes=[R_xt[b]])
                    act(junk[:], xt[b][:], AF.Square, accum_out=stat[:, 0:1], reads=[R_xt[b]], writes=[R_stat])
                    rstd_from_ss(stat[:, 1:2], stat[:, 0:1], D, [R_stat])
                    ts(hn[:], xt[b][:], stat[:, 1:2], None, ALU.mult, reads=[R_xt[b], R_stat], writes=[R_hn])
                    pb = ps[7][:].bitcast(BF16)
                    for k in range(8):
                        transpose(pb[:, k * 128:(k + 1) * 128], hn[:, k * 128:(k + 1) * 128], ident, reads=[R_hn], writes=[R_ps[7]])
                    tt(hT[:, :, i * 128:(i + 1) * 128], pb.rearrange("p (k t) -> p k t", k=8),
                       g1T[:].unsqueeze(2).to_broadcast([128, 8, 128]), ALU.mult, reads=[R_ps[7], R_const], writes=[R_hT[i]])
            S.barrier()

            kcT = [psb(f"kcT{g}", [128, 128], BF16) for g in range(2)]
            vca = [psb(f"vca{g}", [128, 97], BF16) for g in range(2)]
            ph1 = contextlib.ExitStack()

            def psb1(name, shape, dt):
                return ph1.enter_context(nc.sbuf_tensor(f"s_{name}_{seq}", list(shape), dt))
            QT = psb("QT", [128, 4, T], BF16)
            KT = psb("KT", [128, 6, T], BF16)
            VcT = psb("VcT", [128, T], BF16)
            Vtok = psb("Vtok", [128, NT, 4, 65], BF16)
            gat = psb("gat", [128, NT, 24], F32)
            R_QT = [[Res() for _ in range(4)] for _ in range(4)]
            R_KT = [[Res() for _ in range(4)] for _ in range(6)]
            R_Vc = [Res() for _ in range(4)]
            R_Vtok = [Res() for _ in range(NT)]
            R_gat = [Res() for _ in range(NT)]
            wb = [psb1(f"wb{i}", [128, 1024], BF16) for i in range(4)]
            R_wb = [Res() for _ in range(4)]
            wtm = psb1("wtm", [128, 8, NTM], BF16)
            R_wtm = Res()
            rt = [psb1(f"rt{i}", [128, 512], F32) for i in range(4)]
            R_rt = [Res() for _ in range(4)]
            S.dma("pool", wtm[:].rearrange("p k n -> p (k n)"), wtm_d[:], writes=[R_wtm])
            memset(Vtok[:, :, :, 64:65], 1.0, writes=R_Vtok)
            wctr = [0]

            def load_w(chunk):
                r = wctr[0] % 4
                wctr[0] += 1
                S.dma("pool", wb[r][:], wfm_d[chunk], writes=[R_wb[r]])
                return r

            def proj_fm(r, tb, bank):
                for kc in range(8):
                    mm(ps[bank][:], wb[r][:, kc * 128:(kc + 1) * 128], hT[:, kc, tb * 512:(tb + 1) * 512], kc == 0, kc == 7,
                       reads=[R_wb[r]] + R_hT[tb * 4:tb * 4 + 4], writes=[R_ps[bank]])

            pairs = [(FM_Q + j, FM_QSW + j, QT, j, R_QT[j]) for j in range(4)] + [(FM_K + m, FM_KSW + m, KT, m, R_KT[m]) for m in range(6)]
            bctr = 0
            plist = pairs if nsa_attn else []
            pre_loaded = {}
            if plist:
                pre_loaded[0] = (load_w(plist[0][0]), load_w(plist[0][1]))
            for pi, (ca, cb_, dst, di, rdst) in enumerate(plist):
                ra, rb = pre_loaded.pop(pi)
                if pi + 1 < len(plist):
                    pre_loaded[pi + 1] = (load_w(plist[pi + 1][0]), load_w(plist[pi + 1][1]))
                for tb in range(4):
                    ba = (bctr % 3) * 2
                    bctr += 1
                    proj_fm(ra, tb, ba)
                    proj_fm(rb, tb, ba + 1)
                    i0 = (bctr % 2) * 2
                    tt(rt[i0][:], ps[ba][:], tab[:, 1, tb * 512:(tb + 1) * 512], ALU.mult, reads=[R_ps[ba], R_tab], writes=[R_rt[i0]])
                    tt(rt[i0 + 1][:], ps[ba + 1][:], tab[:, 0, tb * 512:(tb + 1) * 512], ALU.mult, reads=[R_ps[ba + 1], R_tab], writes=[R_rt[i0 + 1]])
                    tt(dst[:, di, tb * 512:(tb + 1) * 512], rt[i0][:], rt[i0 + 1][:], ALU.add, reads=[R_rt[i0], R_rt[i0 + 1]], writes=[rdst[tb]])
            rv = load_w(FM_VC)
            for tb in (range(4) if nsa_attn else []):
                proj_fm(rv, tb, 6)
                act(VcT[:, tb * 512:(tb + 1) * 512], ps[6][:], AF.Copy, reads=[R_ps[6]], writes=[R_Vc[tb]])
            sg2 = [psb1(f"sg{i}", [128, 32], F32) for i in range(2)]
            R_sg2 = [Res(), Res()]
            for i in range(NT):
                bank = 6 + (i % 2)
                sg, R_sg = sg2[i % 2], R_sg2[i % 2]
                for kc in range(8):
                    mm(ps[bank][:, 0:NTM], hT[:, kc, i * 128:(i + 1) * 128], wtm[:, kc, :], kc == 0, kc == 7,
                       reads=[R_hT[i], R_wtm], writes=[R_ps[bank]])
                act(Vtok[:, i, :, 0:64], ps[bank][:, 0:256].rearrange("p (a b) -> p a b", a=4), AF.Copy, reads=[R_ps[bank]], writes=[R_Vtok[i]])
                act(sg[:, 0:28], ps[bank][:, 256:284], AF.Exp, scale=-1.0, reads=[R_ps[bank]], writes=[R_sg])
                ts(sg[:, 0:28], sg[:, 0:28], 1.0, None, ALU.add, reads=[R_sg], writes=[R_sg])
                recip(gat[:, i, :], sg[:, 0:24], reads=[R_sg], writes=[R_gat[i]])
                recip(beta_all[:, i, :], sg[:, 24:28], reads=[R_sg], writes=[R_bg[i]])
                tt(sg[:, 28:32], ps[bank][:, 284:288], dtb[:], ALU.add, reads=[R_ps[bank], R_const], writes=[R_sg])
                act(sg[:, 28:32], sg[:, 28:32], AF.Exp, reads=[R_sg], writes=[R_sg])
                act(sg[:, 28:32], sg[:, 28:32], AF.Ln, bias=one_ap, reads=[R_sg], writes=[R_sg])
                tt(g_all[:, i, :], sg[:, 28:32], negA[:], ALU.mult, reads=[R_sg, R_const], writes=[R_bg[i]])

            if nsa_attn:
                w1 = psb1("w1", [128, 2, 32, 128], BF16)
                w2 = psb1("w2", [128, 192], BF16)
                posT = psb1("posT", [64, 2, 32], BF16)
                R_cw = Res()
                S.dma("pool", w1[:].rearrange("p a l h -> p (a l h)"), w1_d[:], writes=[R_cw])
                S.dma("pool", w2[:], w2_d[:], writes=[R_cw])
                S.dma("pool", posT[:].rearrange("p a l -> p (a l)"), posT_d[:], writes=[R_cw])
                R_kc = [Res(), Res()]
                R_vc = [Res(), Res()]
                cbias = psb1("cbias", [128, 2], F32)
                hid = psb1("hid", [128, 128], BF16)
                R_hid = Res()
                R_cb = Res()
                for g in range(2):
                    memset(kcT[g][:], 0.0, writes=[R_kc[g]])
                    memset(vca[g][:], 0.0, writes=[R_vc[g]])
                    memset(vca[g][:, 64:65], 1.0, writes=[R_vc[g]])
                    cp(vca[g][:, 65:97], ovl, reads=[R_const], writes=[R_vc[g]], eng="pool")
                memset(hid[:], 0.0, writes=[R_hid])
                for i2 in range(2):
                    for l in range(32):
                        mm(ps[6][:, i2:i2 + 1], w1[0:64, i2, l, :], posT[:, i2, l:l + 1], l == 0, l == 31, reads=[R_cw], writes=[R_ps[6]])
                cp(cbias[:], ps[6][:, 0:2], reads=[R_ps[6]], writes=[R_cb])
                for g in range(2):
                    for i2 in range(2):
                        if i2 == 0:
                            src, pb0, rsrc = KT[0:64, g, :], 0, R_KT[g]
                        else:
                            pb0 = g * 64
                            src, rsrc = VcT[pb0:pb0 + 64, :], R_Vc
                        for l in range(32):
                            mm(ps[6][:, 0:127], w1[pb0:pb0 + 64, i2, l, :], src[:, l:l + 16 * 126 + 1:16], l == 0, l == 31,
                               reads=[R_cw] + rsrc, writes=[R_ps[6]])
                        act(hid[:, 0:127], ps[6][:, 0:127], AF.Silu, bias=cbias[:, i2:i2 + 1], reads=[R_ps[6], R_cb], writes=[R_hid])
                        if i2 == 0:
                            mm(ps[7][:, 0:127], w2[:, 0:128], hid[:, 0:127], True, True, reads=[R_cw, R_hid], writes=[R_ps[7]])
                            cp(kcT[g][:, 0:127], ps[7][:, 0:127], reads=[R_ps[7]], writes=[R_kc[g]])
                        else:
                            mm(ps[7][:, 0:64], hid[:, :], w2[:, 128:192], True, True, reads=[R_cw, R_hid], writes=[R_ps[7]])
                            cp(vca[g][0:127, 0:64], ps[7][0:127, 0:64], reads=[R_ps[7]], writes=[R_vc[g]])

                ph1.close()
                S.barrier()
                PT = [psb(f"PT{i}", [128, 512], BF16) for i in range(4)]
                R_PT = [Res() for _ in range(4)]
                STB = [0, 1, 2, 6]
                onsa = psb("onsa", [128, 4, 512], F32)
                R_onsa = Res()
                onbf = psb("onbf", [128, 4, 512], BF16)
                R_onbf = Res()
                impsum = psb("impsum", [128, 4, 32], F32)
                impt = psb("impt", [128, 4, 32], F32)
                R_imp = Res()
                R_impt = Res()
                selb = psb("selb", [128, 4, 32], BF16)
                R_selb = Res()
                m8 = psb("m8", [128, 8], F32)
                R_m8 = Res()
                selbT = [psb(f"selbT{g}", [32, 512], BF16) for g in range(2)]
                R_selbT = [Res(), Res()]
                sm = [psb(f"sm{i}", [128, 16], F32) for i in range(2)]
                R_sm = [Res(), Res()]
                tmpo = [psb(f"tmpo{i}", [128, 4, 64], F32) for i in range(2)]
                R_tmpo = [Res(), Res()]
                fst = psb("fst", [128, 8], F32)
                R_fst = Res()
                ctr = {"st": 0, "pt": 0, "ob": 0, "sm": 0}

                def nxt(k, n):
                    v = ctr[k] % n
                    ctr[k] += 1
                    return v

                def evac_branch(ob, width, c, hh, br, first):
                    ov = ps[ob][:, 0:4 * width].rearrange("p (q f) -> p q f", q=4)
                    si = nxt("sm", 2)
                    smt = sm[si]
                    ts(smt[:, 0:4], ov[:, :, 64], 1e-30, None, ALU.add, reads=[R_ps[ob]], writes=[R_sm[si]])
                    recip(smt[:, 4:8], smt[:, 0:4], reads=[R_sm[si]], writes=[R_sm[si]])
                    tt(smt[:, 8:12], smt[:, 4:8], gat[:, 4 * c:4 * c + 4, hh * 3 + br], ALU.mult, reads=[R_sm[si]] + R_gat[4 * c:4 * c + 4], writes=[R_sm[si]])
                    cb = smt[:, 8:12].unsqueeze(2).to_broadcast([128, 4, 64])
                    if first:
                        tt(onsa[:, :, hh * 64:(hh + 1) * 64], ov[:, :, 0:64], cb, ALU.mult, reads=[R_ps[ob], R_sm[si]], writes=[R_onsa])
                    else:
                        tt(tmpo[si][:], ov[:, :, 0:64], cb, ALU.mult, reads=[R_ps[ob], R_sm[si]], writes=[R_tmpo[si]])
                        tt(onsa[:, :, hh * 64:(hh + 1) * 64], onsa[:, :, hh * 64:(hh + 1) * 64], tmpo[si][:], ALU.add,
                           reads=[R_tmpo[si], R_onsa], writes=[R_onsa], eng="pool")
                    return smt, si

                for c in range(4):
                    for g in range(2):
                        cmp_pt = []
                        for h in range(4):
                            hh = 4 * g + h
                            j, hb = hh // 2, (hh % 2) * 64
                            qT = QT[hb:hb + 64, j, c * 512:(c + 1) * 512]
                            st = STB[nxt("st", 4)]
                            mm(ps[st][:], kcT[g][hb:hb + 64, :], qT, True, False, reads=[R_kc[g], R_QT[j][c]], writes=[R_ps[st]])
                            mm(ps[st][:], ident, cmpmask[:, c * 512:(c + 1) * 512], False, True, reads=[R_const], writes=[R_ps[st]])
                            pt = nxt("pt", 4)
                            act(PT[pt][:], ps[st][:], AF.Exp, scale=0.125, reads=[R_ps[st]], writes=[R_PT[pt]])
                            cmp_pt.append(pt)
                        for h in range(4):
                            hh = 4 * g + h
                            pt = cmp_pt[h]
                            ob = 3 + nxt("ob", 2)
                            for tq in range(4):
                                mm(ps[ob][:, tq * 97:(tq + 1) * 97], PT[pt][:, tq * 128:(tq + 1) * 128], vca[g][:, 0:97], tq == 0, tq == 3,
                                   reads=[R_PT[pt], R_vc[g]], writes=[R_ps[ob]], skip=True)
                            smt, si = evac_branch(ob, 97, c, hh, 0, True)
                            ov = ps[ob][:, 0:388].rearrange("p (q f) -> p q f", q=4)
                            rb_ = smt[:, 4:8].unsqueeze(2).to_broadcast([128, 4, 32])
                            if h == 0:
                                tt(impsum[:], ov[:, :, 65:97], rb_, ALU.mult, reads=[R_ps[ob], R_sm[si]], writes=[R_imp])
                            else:
                                tt(impt[:], ov[:, :, 65:97], rb_, ALU.mult, reads=[R_ps[ob], R_sm[si]], writes=[R_impt])
                                tt(impsum[:], impsum[:], impt[:], ALU.add, reads=[R_imp, R_impt], writes=[R_imp])
                        tt(impsum[:], impsum[:], selc[:, 4 * c:4 * c + 4, :], ALU.add, reads=[R_imp, R_const], writes=[R_imp])
                        p5 = ps[5][:].bitcast(BF16)
                        for tq in range(4):
                            max8(m8[:], impsum[:, tq, :], reads=[R_imp], writes=[R_m8])
                            ts(selb[:, tq, :], impsum[:, tq, :], m8[:, 7:8], NEG, ALU.is_lt, ALU.mult, reads=[R_imp, R_m8], writes=[R_selb])
                        for tq in range(4):
                            transpose(p5[0:32, tq * 128:(tq + 1) * 128], selb[:, tq, :], ident, reads=[R_selb], writes=[R_ps[5]])
                        cp(selbT[g][:], p5[0:32, 0:512], reads=[R_ps[5]], writes=[R_selbT[g]])
                        def branch_stream(c, g, h, br, ob):
                            hh = 4 * g + h
                            j, hb = hh // 2, (hh % 2) * 64
                            kts = list(range(0, 4 * c + 4)) if br == 1 else list(range(max(0, 4 * c - 4), 4 * c + 4))
                            plan = []
                            for kt in kts:
                                dq = kt - 4 * c
                                lo = max(0, dq)
                                hi = 3 if br == 1 else min(3, dq + 4)
                                plan.append((kt, dq, lo, hi))
                            npv = sum(hi - lo + 1 for (_, _, lo, hi) in plan)
                            ipv = [0]

                            def emit_pv(item, pt):
                                kt, dq, lo, hi = item
                                for tq in range(lo, hi + 1):
                                    mm(ps[ob][:, tq * 65:(tq + 1) * 65], PT[pt][:, tq * 128:(tq + 1) * 128], Vtok[:, kt, (br - 1) * 2 + g, :],
                                       ipv[0] == 0, ipv[0] == npv - 1, reads=[R_PT[pt], R_Vtok[kt]], writes=[R_ps[ob]], skip=True)
                                    ipv[0] += 1
                            pend = None
                            for item in plan:
                                kt, dq, lo, hi = item
                                c0, c1 = lo * 128, (hi + 1) * 128
                                st = STB[nxt("st", 4)]
                                km = 2 * br + g
                                mm(ps[st][:, c0:c1], KT[hb:hb + 64, km, kt * 128:(kt + 1) * 128], QT[hb:hb + 64, j, c * 512 + c0:c * 512 + c1],
                                   True, br == 2, reads=[R_KT[km][kt // 4], R_QT[j][c]], writes=[R_ps[st]])
                                if br == 1:
                                    mm(ps[st][:, c0:c1], esel[0:32, kt * 128:(kt + 1) * 128], selbT[g][0:32, c0:c1], False, True,
                                       reads=[R_const, R_selbT[g]], writes=[R_ps[st]])
                                pt = nxt("pt", 4)
                                act(PT[pt][:, c0:c1], ps[st][:, c0:c1], AF.Exp, scale=0.125, reads=[R_ps[st]], writes=[R_PT[pt]])
                                if dq >= 0:
                                    dsl = slice(dq * 128, (dq + 1) * 128)
                                    tt(PT[pt][:, dsl], PT[pt][:, dsl], causal01, ALU.mult, reads=[R_PT[pt], R_const], writes=[R_PT[pt]])
                                if br == 2 and dq + 4 <= 3:
                                    dsl = slice((dq + 4) * 128, (dq + 5) * 128)
                                    tt(PT[pt][:, dsl], PT[pt][:, dsl], anti01, ALU.mult, reads=[R_PT[pt], R_const], writes=[R_PT[pt]])
                                yield
                                if pend is not None:
                                    emit_pv(*pend)
                                pend = (item, pt)
                            yield
                            emit_pv(*pend)
                            yield
                            evac_branch(ob, 65, c, hh, br, False)
                            yield

                        def lane(items, ob):
                            for (h, br) in items:
                                yield from branch_stream(c, g, h, br, ob)
                        interleave([lane([(0, 1), (1, 2), (2, 1), (3, 2)], 3), lane([(0, 2), (1, 1), (2, 2), (3, 1)], 4)])
                    for tq in range(4):
                        act(junk2[:], onsa[:, tq, :], AF.Square, accum_out=fst[:, tq:tq + 1], reads=[R_onsa], writes=[R_fst])
                    rstd_from_ss(fst[:, 4:8], fst[:, 0:4], 512, [R_fst])
                    tt(onbf[:], onsa[:], fst[:, 4:8].unsqueeze(2).to_broadcast([128, 4, 512]), ALU.mult, reads=[R_onsa, R_fst], writes=[R_onbf])
                    if "onsa" in dbg_d and seq == 0:
                        S.dma("sp", dbg_d["onsa"][:, c * 2048:(c + 1) * 2048], onsa[:].rearrange("p a b -> p (a b)"), reads=[R_onsa])
                    p7 = ps[7][:].bitcast(BF16)
                    for tq in range(4):
                        i = 4 * c + tq
                        for k in range(4):
                            transpose(p7[:, k * 128:(k + 1) * 128], onbf[:, tq, k * 128:(k + 1) * 128], ident, reads=[R_onbf], writes=[R_ps[7]])
                        tt(mixT[:, 0:4, i * 128:(i + 1) * 128], p7[:, 0:512].rearrange("p (k t) -> p k t", k=4),
                           nsag[:].unsqueeze(2).to_broadcast([128, 4, 128]), ALU.mult, reads=[R_ps[7], R_const], writes=[R_mix[0][i]])

            ph1.close()
            if dbg_d and seq == 0:
                rt = [psb("dbgrt", [128, 512], F32)]
                R_rt = [Res()]

                def dump(name, src3, nchunk, rlist):
                    for k in range(nchunk):
                        for q4 in range(4):
                            cp(rt[0][:], src3[:, k, q4 * 512:(q4 + 1) * 512], reads=rlist + [R_rt[0]], writes=[R_rt[0]])
                            S.dma("sp", dbg_d[name][:, k * T + q4 * 512:k * T + (q4 + 1) * 512], rt[0][:], reads=[R_rt[0]], writes=[R_rt[0]])
                if "QT" in dbg_d:
                    dump("QT", QT, 4, [x for y in R_QT for x in y])
                if "KT" in dbg_d:
                    dump("KT", KT, 6, [x for y in R_KT for x in y])
                if "mixN" in dbg_d:
                    dump("mixN", mixT, 4, R_mix[0])
        S.barrier()


        if stage >= 2 and mixers:
          with contextlib.ExitStack() as ph:
            def psb(name, shape, dt):
                return ph.enter_context(nc.sbuf_tensor(f"s_g{name}_{seq}", list(shape), dt))
            phg1 = contextlib.ExitStack()

            def psbg1(name, shape, dt):
                return phg1.enter_context(nc.sbuf_tensor(f"s_g{name}_{seq}", list(shape), dt))
            bctr2 = [0]

            def cut(n):
                if gdn_cut == n:
                    S.dead = True

            def bank():
                b_ = bctr2[0] % 8
                bctr2[0] += 1
                return b_

            gq = psb("gq", [128, 8, T], BF16)
            R_gq = [[Res() for _ in range(4)] for _ in range(8)]
            vtok = psb("vtok", [128, NT, 4, 128], BF16)
            R_vtok = [Res() for _ in range(NT)]
            zT = psb("zT", [128, 4, T], BF16)
            R_zT = [[Res() for _ in range(4)] for _ in range(4)]
            wb = [psbg1(f"wb{i}", [128, 1024], BF16) for i in range(4)]
            R_wb = [Res() for _ in range(4)]
            xbuf = [psbg1(f"xbuf{i}", [128, 515], F32) for i in range(2)]
            R_xb = [Res(), Res()]
            acc = [psbg1(f"acc{i}", [128, 512], F32) for i in range(2)]
            R_acc = [Res(), Res()]
            vtmp = psbg1("vtmp", [128, 512], BF16)
            R_vtmp = Res()
            wctr = [0]

            def load_w(chunk):
                r = wctr[0] % 4
                wctr[0] += 1
                S.dma("pool", wb[r][:], wfm_d[chunk], writes=[R_wb[r]])
                return r

            gl_ = {0: load_w(FM_GQKV + 0), 1: load_w(FM_GQKV + 1)}
            for cg in range(16):
                r = gl_.pop(cg)
                if cg + 2 < 16:
                    gl_[cg + 2] = load_w(FM_GQKV + cg + 2)
                for tb in range(4):
                    bk = bank()
                    for kc in range(8):
                        mm(ps[bk][:], wb[r][:, kc * 128:(kc + 1) * 128], hT[:, kc, tb * 512:(tb + 1) * 512], kc == 0, kc == 7,
                           reads=[R_wb[r]] + R_hT[tb * 4:tb * 4 + 4], writes=[R_ps[bk]])
                    if cg >= 12:
                        act(zT[:, cg - 12, tb * 512:(tb + 1) * 512], ps[bk][:], AF.Silu, reads=[R_ps[bk]], writes=[R_zT[cg - 12][tb]])
                        continue
                    b = tb % 2
                    if tb == 0:
                        memset(xbuf[b][:, 0:3], 0.0, writes=[R_xb[b]])
                    else:
                        cp(xbuf[b][:, 0:3], xbuf[1 - b][:, 512:515], reads=[R_xb[1 - b]], writes=[R_xb[b]], eng="pool")
                    act(xbuf[b][:, 3:515], ps[bk][:], AF.Copy, reads=[R_ps[bk]], writes=[R_xb[b]])
                    ts(acc[b][:], xbuf[b][:, 0:512], convw[:, cg, 0:1], None, ALU.mult, reads=[R_xb[b], R_const], writes=[R_acc[b]])
                    for tap in (1, 2, 3):
                        stt(acc[b][:], xbuf[b][:, tap:tap + 512], convw[:, cg, tap:tap + 1], acc[b][:], ALU.mult, ALU.add,
                            reads=[R_xb[b], R_acc[b], R_const], writes=[R_acc[b]])
                    if cg < 8:
                        act(gq[:, cg, tb * 512:(tb + 1) * 512], acc[b][:], AF.Silu, reads=[R_acc[b]], writes=[R_gq[cg][tb]])
                    else:
                        act(vtmp[:], acc[b][:], AF.Silu, reads=[R_acc[b]], writes=[R_vtmp])
                        bk2 = bank()
                        pbf = ps[bk2][:].bitcast(BF16)
                        for i4 in range(4):
                            transpose(pbf[:, i4 * 128:(i4 + 1) * 128], vtmp[:, i4 * 128:(i4 + 1) * 128], ident, reads=[R_vtmp], writes=[R_ps[bk2]])
                        cp(vtok[:, tb * 4:tb * 4 + 4, cg - 8, :], pbf[:, 0:512].rearrange("p (a b) -> p a b", a=4), reads=[R_ps[bk2]],
                           writes=R_vtok[tb * 4:tb * 4 + 4])
            cut(1)
            sqb = [psbg1(f"sqb{i}", [128, 512], BF16) for i in range(2)]
            R_sq = [Res(), Res()]
            lnb = [psbg1(f"lnb{i}", [128, 512], F32) for i in range(2)]
            R_ln = [Res(), Res()]
            it = 0
            for cg in range(8):
                for tb in range(4):
                    b = it % 2
                    it += 1
                    src = gq[:, cg, tb * 512:(tb + 1) * 512]
                    tt(sqb[b][:], src, src, ALU.mult, reads=[R_gq[cg][tb]], writes=[R_sq[b]], eng="pool")
                    bk = bank()
                    mm(ps[bk][:], ones_bf, sqb[b][:], True, True, reads=[R_sq[b], R_const], writes=[R_ps[bk]])
                    act(lnb[b][:], ps[bk][:], AF.Ln, bias=eps_ap, reads=[R_ps[bk]], writes=[R_ln[b]])
                    act(lnb[b][:], lnb[b][:], AF.Exp, scale=-0.5, reads=[R_ln[b]], writes=[R_ln[b]])
                    tt(src, src, lnb[b][:], ALU.mult, reads=[R_gq[cg][tb], R_ln[b]], writes=[R_gq[cg][tb]])

            cut(2)
            phg1.close()
            S.barrier()
            def t4(name, dt=BF16):
                return psb(name, [128, 4, 128], dt)
            Sst = t4("Sst", F32)
            Stmp = t4("Stmp", F32)
            Sbf = [t4("Sbf0"), t4("Sbf1")]
            R_S, R_Stmp, R_Sbf = Res(), Res(), [Res(), Res()]
            memset(Sst[:], 0.0, writes=[R_S])
            memset(Sbf[0][:], 0.0, writes=[R_Sbf[0]])
            sbi = [0]
            NSET = 3
            B = []
            for k_ in range(NSET):
                d_ = {}
                for nm, dt_ in (("EmA", BF16), ("EmQ", BF16), ("X0", BF16), ("X1", BF16), ("Y0", BF16), ("Y1", BF16), ("M0", BF16), ("M1", BF16),
                                ("qkT", BF16), ("kbg", BF16), ("kdd", BF16), ("vb", BF16), ("wTn", BF16)):
                    d_[nm] = t4(f"{nm}_{k_}", dt_)
                    d_["R_" + nm] = Res()
                d_["gs"] = psb(f"gs_{k_}", [128, 48], F32)
                d_["R_gs"] = Res()
                B.append(d_)
            shared = {}
            for nm, dt_ in (("Lg", F32),):
                shared[nm] = t4(nm, dt_)
                shared["R_" + nm] = Res()
            for d_ in B:
                d_.update(shared)
            vnew = t4("vnew")
            osb = t4("osb", F32)
            ofin = t4("ofin", F32)
            onb = t4("onb")
            R_vnew, R_osb, R_ofin, R_onb = [Res() for _ in range(4)]
            id4 = ident4.rearrange("p (a b) -> p a b", a=4)
            Mfin = {}

            def b4(ap):
                return ap.unsqueeze(2).to_broadcast([128, 4, 128])

            def v4(bk):
                return ps[bk][:].rearrange("p (a b) -> p a b", a=4)

            def pre_tile(i):
                D_ = B[i % NSET]
                gs, R_gs = D_["gs"], D_["R_gs"]
                Lg, EmA, EmQ, qkT, kbg, kdd, vb, wTn = (D_[k] for k in ("Lg", "EmA", "EmQ", "qkT", "kbg", "kdd", "vb", "wTn"))
                R_Lg, R_EmA, R_EmQ, R_qkT, R_kbg, R_kdd, R_vb, R_wTn = (D_["R_" + k] for k in ("Lg", "EmA", "EmQ", "qkT", "kbg", "kdd", "vb", "wTn"))
                Xb, Yb, Mb = [D_["X0"], D_["X1"]], [D_["Y0"], D_["Y1"]], [D_["M0"], D_["M1"]]
                R_X, R_Y, R_M = [D_["R_X0"], D_["R_X1"]], [D_["R_Y0"], D_["R_Y1"]], [D_["R_M0"], D_["R_M1"]]
                tsl = slice(i * 128, (i + 1) * 128)
                g_i = g_all[:, i, :]
                be_i = beta_all[:, i, :]
                Rk = [R_gq[4 + h][i // 4] for h in range(4)]
                Rq = [R_gq[h][i // 4] for h in range(4)]
                bk = bank()
                mm(ps[bk][:, 0:4], ltriT, g_i, True, True, reads=[R_const, R_bg[i]], writes=[R_ps[bk]])
                mm(ps[bk][:, 4:8], bones, g_i, True, True, reads=[R_const, R_bg[i]], writes=[R_ps[bk]])
                cp(gs[:, 0:4], ps[bk][:, 0:4], reads=[R_ps[bk]], writes=[R_gs])
                ts(gs[:, 4:8], be_i, -1.0, None, ALU.mult, reads=[R_bg[i]], writes=[R_gs])
                act(gs[:, 8:12], gs[:, 0:4], AF.Exp, reads=[R_gs], writes=[R_gs])
                tt(gs[:, 12:16], gs[:, 8:12], be_i, ALU.mult, reads=[R_gs, R_bg[i]], writes=[R_gs])
                tt(gs[:, 16:20], ps[bk][:, 4:8], gs[:, 0:4], ALU.subtract, reads=[R_ps[bk], R_gs], writes=[R_gs])
                act(gs[:, 16:20], gs[:, 16:20], AF.Exp, reads=[R_gs], writes=[R_gs])
                ts(gs[:, 20:24], gs[:, 0:4], -1.0, None, ALU.mult, reads=[R_gs], writes=[R_gs])
                tt(gs[:, 24:32].rearrange("p (h j) -> p h j", h=4), g_i.unsqueeze(2).to_broadcast([128, 4, 2]),
                   cind.unsqueeze(1).to_broadcast([128, 4, 2]), ALU.mult, reads=[R_bg[i], R_const], writes=[R_gs])
                tt(Lg[:], ltriT.unsqueeze(1).to_broadcast([128, 4, 128]), b4(g_i), ALU.mult, reads=[R_const, R_bg[i]], writes=[R_Lg], eng="pool")
                bk = bank()
                mm(ps[bk][:, 0:8], ones_f, gs[:, 24:32], True, True, reads=[R_const, R_gs], writes=[R_ps[bk]])
                act(gs[:, 32:40], ps[bk][:, 0:8], AF.Exp, reads=[R_ps[bk]], writes=[R_gs])
                bA = bank()
                mm(ps[bA][:], ones_f, Lg[:].rearrange("p a b -> p (a b)"), True, False, reads=[R_const, R_Lg], writes=[R_ps[bA]])
                mm(ps[bA][:], ident, maskA4, False, True, reads=[R_const], writes=[R_ps[bA]])
                for h in range(4):
                    act(EmA[:, h, :], ps[bA][:, h * 128:(h + 1) * 128], AF.Exp, bias=gs[:, h:h + 1], scale=-1.0, reads=[R_ps[bA], R_gs], writes=[R_EmA])
                bQ = bank()
                mm(ps[bQ][:], ones_f, Lg[:].rearrange("p a b -> p (a b)"), True, False, reads=[R_const, R_Lg], writes=[R_ps[bQ]])
                mm(ps[bQ][:], ident, maskQ4, False, True, reads=[R_const], writes=[R_ps[bQ]])
                for h in range(4):
                    act(EmQ[:, h, :], ps[bQ][:, h * 128:(h + 1) * 128], AF.Exp, bias=gs[:, 20 + h:21 + h], scale=1.0, reads=[R_ps[bQ], R_gs], writes=[R_EmQ])
                yield
                bK = bank()
                for h in range(4):
                    kT = gq[:, 4 + h, tsl]
                    mm(ps[bK][:, h * 128:(h + 1) * 128], kT, kT, True, True, reads=[Rk[h]], writes=[R_ps[bK]])
                for h in range(4):
                    stt(Xb[0][:, h, :], ps[bK][:, h * 128:(h + 1) * 128], gs[:, 4 + h:5 + h], EmA[:, h, :], ALU.mult, ALU.mult,
                        reads=[R_ps[bK], R_gs, R_EmA], writes=[R_X[0]])
                yield
                bKQ = bank()
                for h in range(4):
                    mm(ps[bKQ][:, h * 128:(h + 1) * 128], gq[:, 4 + h, tsl], gq[:, h, tsl], True, True, reads=[Rk[h], Rq[h]], writes=[R_ps[bKQ]])
                tt(qkT[:], v4(bKQ), EmQ[:], ALU.mult, reads=[R_ps[bKQ], R_EmQ], writes=[R_qkT])
                bT = bank()
                pT = ps[bT][:].bitcast(BF16)
                for h in range(4):
                    transpose(pT[:, h * 128:(h + 1) * 128], gq[:, 4 + h, tsl], ident, reads=[Rk[h]], writes=[R_ps[bT]])
                pT4 = pT[:, 0:512].rearrange("p (a b) -> p a b", a=4)
                tt(kbg[:], pT4, b4(gs[:, 12:16]), ALU.mult, reads=[R_ps[bT], R_gs], writes=[R_kbg])
                tt(kdd[:], pT4, b4(gs[:, 16:20]), ALU.mult, reads=[R_ps[bT], R_gs], writes=[R_kdd])
                tt(vb[:], vtok[:, i, :, :], b4(be_i), ALU.mult, reads=[R_vtok[i], R_bg[i]], writes=[R_vb], eng="pool")
                yield
                bT = bank()
                pT = ps[bT][:].bitcast(BF16)
                for h in range(4):
                    transpose(pT[:, h * 128:(h + 1) * 128], Xb[0][:, h, :], ident, reads=[R_X[0]], writes=[R_ps[bT]])
                pT4 = pT[:, 0:512].rearrange("p (a b) -> p a b", a=4)
                act(Yb[0][:], pT4, AF.Copy, reads=[R_ps[bT]], writes=[R_Y[0]])
                tt(Mb[0][:], Yb[0][:], id4, ALU.add, reads=[R_Y[0], R_const], writes=[R_M[0]])
                yield
                xc, yc, mc = 0, 0, 0
                for lvl in range(5):
                    xn, yn, mn = 1 - xc, 1 - yc, 1 - mc
                    bX = bank()
                    for h in range(4):
                        mm(ps[bX][:, h * 128:(h + 1) * 128], Yb[yc][:, h, :], Xb[xc][:, h, :], True, True, reads=[R_Y[yc], R_X[xc]], writes=[R_ps[bX]])
                    act(Xb[xn][:], v4(bX), AF.Copy, reads=[R_ps[bX]], writes=[R_X[xn]])
                    if lvl < 4:
                        bY = bank()
                        for h in range(4):
                            mm(ps[bY][:, h * 128:(h + 1) * 128], Xb[xc][:, h, :], Yb[yc][:, h, :], True, True, reads=[R_Y[yc], R_X[xc]], writes=[R_ps[bY]])
                        act(Yb[yn][:], v4(bY), AF.Copy, reads=[R_ps[bY]], writes=[R_Y[yn]])
                    yield
                    bM = bank()
                    for h in range(4):
                        mm(ps[bM][:, h * 128:(h + 1) * 128], Xb[xn][:, h, :], Mb[mc][:, h, :], True, True, reads=[R_X[xn], R_M[mc]], writes=[R_ps[bM]])
                    tt(Mb[mn][:], v4(bM), Mb[mc][:], ALU.add, reads=[R_ps[bM], R_M[mc]], writes=[R_M[mn]])
                    xc, yc, mc = xn, (yn if lvl < 4 else yc), mn
                    yield
                M, R_Mf = Mb[mc], R_M[mc]
                Mfin[i] = (M, R_Mf)
                bW = bank()
                for h in range(4):
                    mm(ps[bW][:, h * 128:(h + 1) * 128], kbg[:, h, :], M[:, h, :], True, True, reads=[R_kbg, R_Mf], writes=[R_ps[bW]])
                act(wTn[:], v4(bW), AF.Copy, scale=-1.0, reads=[R_ps[bW]], writes=[R_wTn])
                yield

            def scan_tile(i):
                D_ = B[i % NSET]
                gs, R_gs = D_["gs"], D_["R_gs"]
                qkT, kdd, vb, wTn = (D_[k] for k in ("qkT", "kdd", "vb", "wTn"))
                R_qkT, R_kdd, R_vb, R_wTn = (D_["R_" + k] for k in ("qkT", "kdd", "vb", "wTn"))
                M, R_Mf = Mfin[i]
                tsl = slice(i * 128, (i + 1) * 128)
                Rq = [R_gq[h][i // 4] for h in range(4)]
                decS = gs[:, 32:40].rearrange("p (h j) -> p h j", h=4)
                for jc in range(2):
                    rows = slice(jc * 64, (jc + 1) * 64)
                    so, sn = sbi[0], 1 - sbi[0]
                    bV = bank()
                    for h in range(4):
                        mm(ps[bV][:, h * 128:(h + 1) * 128], M[:, h, :], vb[:, h, :], True, False, reads=[R_Mf, R_vb], writes=[R_ps[bV]])
                        mm(ps[bV][:, h * 128:(h + 1) * 128], wTn[:, h, :], Sbf[so][:, h, :], False, True, reads=[R_wTn, R_Sbf[so]], writes=[R_ps[bV]])
                    act(vnew[rows, :, :], ps[bV][rows, :].rearrange("p (a b) -> p a b", a=4), AF.Copy, reads=[R_ps[bV]], writes=[R_vnew])
                    tt(Stmp[:], Sst[:], decS[:, :, jc].unsqueeze(2).to_broadcast([128, 4, 128]), ALU.mult, reads=[R_S, R_gs], writes=[R_Stmp])
                    yield
                    bO = bank()
                    for h in range(4):
                        mm(ps[bO][:, h * 128:(h + 1) * 128], gq[:, h, tsl], Sbf[so][:, h, :], True, True, reads=[Rq[h], R_Sbf[so]], writes=[R_ps[bO]])
                    tt(osb[rows, :, :], ps[bO][rows, :].rearrange("p (a b) -> p a b", a=4), gs[rows, 8:12].unsqueeze(2).to_broadcast([64, 4, 128]),
                       ALU.mult, reads=[R_ps[bO], R_gs], writes=[R_osb])
                    yield
                    bD = bank()
                    for h in range(4):
                        mm(ps[bD][:, h * 128:(h + 1) * 128], kdd[rows, h, :], vnew[rows, h, :], True, True, reads=[R_kdd, R_vnew], writes=[R_ps[bD]])
                    tt(Sbf[sn][:], v4(bD), Stmp[:], ALU.add, reads=[R_ps[bD], R_Stmp], writes=[R_Sbf[sn]])
                    tt(Sst[:], v4(bD), Stmp[:], ALU.add, reads=[R_ps[bD], R_Stmp], writes=[R_S])
                    sbi[0] = sn
                    yield
                bO2 = bank()
                for h in range(4):
                    mm(ps[bO2][:, h * 128:(h + 1) * 128], qkT[:, h, :], vnew[:, h, :], True, True, reads=[R_qkT, R_vnew], writes=[R_ps[bO2]])
                tt(ofin[:], v4(bO2), osb[:], ALU.add, reads=[R_ps[bO2], R_osb], writes=[R_ofin])
                for h in range(4):
                    act(junk2[:, 0:128], ofin[:, h, :], AF.Square, accum_out=gs[:, 40 + h:41 + h], reads=[R_ofin], writes=[R_gs])
                act(gs[:, 44:48], gs[:, 40:44], AF.Ln, bias=eps128_ap, scale=1.0 / 128, reads=[R_gs], writes=[R_gs])
                act(gs[:, 44:48], gs[:, 44:48], AF.Exp, scale=-0.5, reads=[R_gs], writes=[R_gs])
                tt(onb[:], ofin[:], b4(gs[:, 44:48]), ALU.mult, reads=[R_ofin, R_gs], writes=[R_onb])
                if "ogdn" in dbg_d and seq == 0:
                    S.dma("sp", dbg_d["ogdn"][:, i * 512:(i + 1) * 512], ofin[:].rearrange("p a b -> p (a b)"), reads=[R_ofin])
                yield
                bT = bank()
                pT = ps[bT][:].bitcast(BF16)
                for h in range(4):
                    transpose(pT[:, h * 128:(h + 1) * 128], onb[:, h, :], ident, reads=[R_onb], writes=[R_ps[bT]])
                stt(mixT[:, 4:8, tsl], pT[:, 0:512].rearrange("p (a b) -> p a b", a=4), gng[:, 0:1], zT[:, :, tsl], ALU.mult, ALU.mult,
                    reads=[R_ps[bT], R_const] + [R_zT[h][i // 4] for h in range(4)], writes=[R_mix[1][i]])
                yield

            pre_done = set()
            scan_done = -1
            active_pre = {}
            cur_scan, cur_i, next_pre = None, 0, 0
            while cur_i < NT:
                while next_pre < NT and len(active_pre) < NSET - 1 and next_pre - NSET <= scan_done:
                    active_pre[next_pre] = pre_tile(next_pre)
                    next_pre += 1
                if cur_scan is None and cur_i in pre_done:
                    cur_scan = scan_tile(cur_i)
                if cur_scan is not None:
                    try:
                        next(cur_scan)
                    except StopIteration:
                        scan_done = cur_i
                        cur_i += 1
                        cur_scan = None
                for k_ in list(active_pre):
                    try:
                        next(active_pre[k_])
                    except StopIteration:
                        pre_done.add(k_)
                        del active_pre[k_]
            if "mixG" in dbg_d and seq == 0:
                for k in range(4):
                    for q4 in range(4):
                        cp(ofin[:].rearrange("p a b -> p (a b)"), mixT[:, 4 + k, q4 * 512:(q4 + 1) * 512], reads=R_mix[1] + [R_ofin], writes=[R_ofin])
                        S.dma("sp", dbg_d["mixG"][:, k * T + q4 * 512:k * T + (q4 + 1) * 512], ofin[:].rearrange("p a b -> p (a b)"), reads=[R_ofin], writes=[R_ofin])
          S.dead = False
          S.barrier()


        if stage >= 3:
          with contextlib.ExitStack() as ph:
            def psb(name, shape, dt):
                return ph.enter_context(nc.sbuf_tensor(f"s_o{name}_{seq}", list(shape), dt))
            wo = psb("wo", [128, 8, D], BF16)
            R_wo = [Res() for _ in range(4)]
            for q4 in range(4):
                S.dma("pool", wo[:, 2 * q4:2 * q4 + 2, :].rearrange("p a b -> p (a b)"), wout_d[:, q4 * 2048:(q4 + 1) * 2048], writes=[R_wo[q4]])
            xt = [psb(f"xt{i}", [128, D], F32) for i in range(2)]
            x2t = [psb(f"x2t{i}", [128, D], F32) for i in range(2)]
            R_xt, R_x2t = [Res(), Res()], [Res(), Res()]
            hn = psb("hn", [128, D], BF16)
            R_hn = Res()
            junk = psb("junk", [128, D], BF16)
            stat = psb("stat", [128, 8], F32)
            R_stat = Res()
            pend_fin = []
            for i in range(NT):
                b = i % 2
                tsl = slice(i * 128, (i + 1) * 128)
                S.dma("sp", xt[b][:], x_d[seq, tsl, :], writes=[R_xt[b]])
                for half in range(2):
                    bk = (2 * i + half) % 6
                    for kc in range(8):
                        mm(ps[bk][:], mixT[:, kc, tsl], wo[:, kc, half * 512:(half + 1) * 512], kc == 0, kc == 7,
                           reads=[R_mix[kc // 4][i], R_wo[kc // 2]], writes=[R_ps[bk]])
                    tt(x2t[b][:, half * 512:(half + 1) * 512], ps[bk][:], xt[b][:, half * 512:(half + 1) * 512], ALU.add,
                       reads=[R_ps[bk], R_xt[b]], writes=[R_x2t[b]])
                if pend_fin:
                    pend_fin.pop(0)()
                S.dma("sp", x2_d[seq, tsl, :], x2t[b][:], reads=[R_x2t[b]], writes=[R_x2s[i]])
                act(junk[:], x2t[b][:], AF.Square, accum_out=stat[:, 0:1], reads=[R_x2t[b]], writes=[R_stat])
                rstd_from_ss(stat[:, 1:2], stat[:, 0:1], D, [R_stat])
                ts(hn[:], x2t[b][:], stat[:, 1:2], None, ALU.mult, reads=[R_x2t[b], R_stat], writes=[R_hn])
                def fin_(tsl=tsl, i=i):
                    pb = ps[7][:].bitcast(BF16)
                    for k in range(8):
                        transpose(pb[:, k * 128:(k + 1) * 128], hn[:, k * 128:(k + 1) * 128], ident, reads=[R_hn], writes=[R_ps[7]])
                    tt(hT[:, :, tsl], pb.rearrange("p (k t) -> p k t", k=8), g2T[:].unsqueeze(2).to_broadcast([128, 8, 128]), ALU.mult,
                       reads=[R_ps[7], R_const], writes=[R_hT[i]])
                pend_fin.append(fin_)
            pend_fin.pop(0)()
          S.barrier()

        if stage >= 4:
          with contextlib.ExitStack() as ph:
            def psb(name, shape, dt):
                return ph.enter_context(nc.sbuf_tensor(f"s_f{name}_{seq}", list(shape), dt))
            actT = psb("actT", [128, NFC, 1024], BF16)
            R_act = [[Res() for _ in range(2)] for _ in range(NFC)]
            wd = psb("wd", [128, NFC, D], BF16)
            R_wd = [Res() for _ in range(NFC)]
            wgu = [psb(f"wgu{i}", [128, 1024], BF16) for i in range(6)]
            R_wgu = [Res() for _ in range(6)]
            sgt = [psb(f"sgt{i}", [128, 512], BF16) for i in range(2)]
            R_sgt = [Res(), Res()]
            x2t = [psb(f"x2t{i}", [128, D], F32) for i in range(2)]
            x3t = [psb(f"x3t{i}", [128, D], F32) for i in range(2)]
            R_x2t, R_x3t = [Res(), Res()], [Res(), Res()]
            junk = psb("junk", [128, D], BF16)
            stat = psb("stat", [128, 8], F32)
            R_stat = Res()
            wi = 0
            si = 0
            for th in range(2):
                for fc in range(NFC):
                    rg = wi % 6
                    ru = (wi + 1) % 6
                    wi += 2
                    S.dma("pool", wgu[rg][:], wg_d[fc], writes=[R_wgu[rg]])
                    S.dma("pool", wgu[ru][:], wu_d[fc], writes=[R_wgu[ru]])
                    if th == 0:
                        S.dma("pool", wd[:, fc, :], wd_d[:, fc * D:(fc + 1) * D], writes=[R_wd[fc]])
                    for tb in range(2):
                        t0 = th * 1024 + tb * 512
                        bg_, bu_ = (4 * fc + 2 * tb) % 6, (4 * fc + 2 * tb + 1) % 6
                        for kc in range(8):
                            mm(ps[bg_][:], wgu[rg][:, kc * 128:(kc + 1) * 128], hT[:, kc, t0:t0 + 512], kc == 0, kc == 7,
                               reads=[R_wgu[rg]] + R_hT[t0 // 128:t0 // 128 + 4], writes=[R_ps[bg_]])
                        for kc in range(8):
                            mm(ps[bu_][:], wgu[ru][:, kc * 128:(kc + 1) * 128], hT[:, kc, t0:t0 + 512], kc == 0, kc == 7,
                               reads=[R_wgu[ru]] + R_hT[t0 // 128:t0 // 128 + 4], writes=[R_ps[bu_]])
                        sb_ = si % 2
                        si += 1
                        act(sgt[sb_][:], ps[bg_][:], AF.Silu, reads=[R_ps[bg_]], writes=[R_sgt[sb_]])
                        tt(actT[:, fc, tb * 512:(tb + 1) * 512], ps[bu_][:], sgt[sb_][:], ALU.mult, reads=[R_ps[bu_], R_sgt[sb_]], writes=[R_act[fc][tb]])
                for tl in range(8):
                    i = th * 8 + tl
                    b = i % 2
                    tsl = slice(i * 128, (i + 1) * 128)
                    S.dma("sp", x2t[b][:], x2_d[seq, tsl, :], reads=[R_x2s[i]], writes=[R_x2t[b]])
                    for half in range(2):
                        bk = 6 + half
                        for fc in range(NFC):
                            mm(ps[bk][:], actT[:, fc, tl * 128:(tl + 1) * 128], wd[:, fc, half * 512:(half + 1) * 512], fc == 0, fc == NFC - 1,
                               reads=[R_act[fc][tl // 4], R_wd[fc]], writes=[R_ps[bk]])
                        tt(x3t[b][:, half * 512:(half + 1) * 512], ps[bk][:], x2t[b][:, half * 512:(half + 1) * 512], ALU.add,
                           reads=[R_ps[bk], R_x2t[b]], writes=[R_x3t[b]])
                    act(junk[:], x3t[b][:], AF.Square, accum_out=stat[:, 0:1], reads=[R_x3t[b]], writes=[R_stat])
                    rstd_from_ss(stat[:, 1:2], stat[:, 0:1], D, [R_stat])
                    stt(x3t[b][:], x3t[b][:], stat[:, 1:2], fgb[:], ALU.mult, ALU.mult, reads=[R_x3t[b], R_stat, R_const], writes=[R_x3t[b]])
                    S.dma("sp", out_d[seq, tsl, :], x3t[b][:], reads=[R_x3t[b]], writes=[R_x3t[b]])
          S.barrier()

    S.barrier()
    S.run()
    es.close()
    return nc


def _bf(a):
    return np.asarray(a, dtype=np.float32).astype(ml_dtypes.bfloat16)


def _tile_w(w):
    return np.ascontiguousarray(w.reshape(8, 128, -1).transpose(1, 0, 2))


def _consts():
    c = {}
    ident = np.eye(128, dtype=np.float32)
    ones = np.ones((128, 128), np.float32)
    k = np.arange(128)[:, None]
    t = np.arange(128)[None, :]
    causal = np.where(k <= t, 0.0, NEG).astype(np.float32)
    anti = np.where(k > t, 0.0, NEG).astype(np.float32)
    same = (k // 64) == (t // 64)
    maskA = np.where(same & (t < k), 0.0, -NEG).astype(np.float32)
    maskQ = np.where(same & (t >= k), 0.0, NEG).astype(np.float32)
    n = np.arange(128)[:, None]
    s = np.arange(32)[None, :]
    ovl = ((16 * n < 64 * s + 64) & (16 * n + 32 > 64 * s) & (n < 127)).astype(np.float32)
    esel = np.zeros((128, 2048), np.float32)
    esel[:32] = (np.arange(2048)[None, :] // 64 == np.arange(32)[:, None]).astype(np.float32)
    tt = np.arange(2048)[None, :]
    cmpmask = np.where((16 * n + 31 <= tt) & (n < 127), 0.0, NEG).astype(np.float32)
    c["cbf"] = _bf(np.concatenate([ident, ones, causal, anti, np.tile(maskA, (1, 4)), np.tile(maskQ, (1, 4)), ovl, esel, cmpmask, np.tile(ident, (1, 4)), (k <= t).astype(np.float32), (k > t).astype(np.float32)], axis=1))
    assert c["cbf"].shape[1] == NCBF
    ltriT = (same & (k <= t)).astype(np.float32)
    bones = same.astype(np.float32)
    cind = (np.arange(128)[:, None] // 64 == np.arange(2)[None, :]).astype(np.float32)
    c["cf32"] = np.ascontiguousarray(np.concatenate([ltriT, bones, ones, cind], axis=1))
    tq = np.arange(2048)[:, None]
    cur = tq // 64
    j = np.arange(32)[None, :]
    forced = (j == 0) | (j == cur) | (j == cur - 1)
    caus = j <= cur
    selb = np.where(forced, 1e9, np.where(caus, 0.0, -1e30)).astype(np.float32)
    c["selc"] = np.ascontiguousarray(selb.reshape(16, 128, 32).transpose(1, 0, 2).reshape(128, 16 * 32))
    inv = (500000.0 ** (-np.arange(8, dtype=np.float64) * (2.0 / 16))) / (2 * np.pi)
    rc = np.zeros((128, 4), np.float32)
    for base in (0, 64):
        rc[base:base + 8, 0] = -inv
        rc[base + 8:base + 16, 0] = inv
        rc[base:base + 8, 1] = inv
        rc[base + 8:base + 16, 1] = inv
    rc[:, 2] = 0.25
    c["ropec"] = rc
    return c


def _swap_cols(wq, nheads):
    out = np.zeros_like(wq)
    for h in range(nheads):
        b = h * 64
        out[:, b:b + 8] = wq[:, b + 8:b + 16]
        out[:, b + 8:b + 16] = wq[:, b:b + 8]
    return out


def prep_shared(inp):
    w_in = np.asarray(inp["w_in"], np.float32)[0]
    sh = dict(_consts())
    cols = []
    wq = w_in[:, 0:512]
    cols.append(wq)
    cols.append(_swap_cols(wq, 8))
    kd, ksd = [], []
    for kind in (0, 2, 4):
        for g in range(2):
            o = OFF_KV + kind * 128 + g * 64
            wk = w_in[:, o:o + 64]
            kd.append(np.concatenate([wk, wk], axis=1))
            ws = _swap_cols(wk, 1)
            ksd.append(np.concatenate([ws, ws], axis=1))
    cols += kd + ksd
    cols.append(w_in[:, OFF_KV + 128:OFF_KV + 256])
    cols.append(w_in[:, OFF_GQKV:OFF_GQKV + 1536])
    cols.append(w_in[:, OFF_GZ:OFF_GZ + 512])
    wfm = np.concatenate(cols, axis=1)
    assert wfm.shape[1] == NFM * 128
    wt = _tile_w(wfm)
    sh["wfm"] = np.ascontiguousarray(wt.reshape(128, 8, NFM, 128).transpose(2, 0, 1, 3).reshape(NFM, 128, 1024))
    wtm = np.concatenate([w_in[:, OFF_KV + 3 * 128:OFF_KV + 4 * 128], w_in[:, OFF_KV + 5 * 128:OFF_KV + 6 * 128],
                          w_in[:, OFF_GATE:OFF_GATE + 24], w_in[:, OFF_GB:OFF_GB + 4], w_in[:, OFF_GA:OFF_GA + 4]], axis=1)
    assert wtm.shape[1] == NTM
    sh["wtm"] = np.ascontiguousarray(_tile_w(wtm).reshape(128, 8 * NTM))
    sh["g1"] = np.ascontiguousarray(np.asarray(inp["norm1_g"], np.float32)[0].reshape(8, 128).T)
    w1 = np.asarray(inp["cmp_w1"], np.float32)[0]
    w1t = w1.reshape(2, 32, 64, 128).transpose(2, 0, 1, 3).reshape(64, 2 * 32 * 128)
    sh["w1"] = np.ascontiguousarray(np.concatenate([w1t, w1t], axis=0))
    w2 = np.asarray(inp["cmp_w2"], np.float32)[0]
    sh["w2"] = np.ascontiguousarray(np.concatenate([w2[0], w2[0], w2[1]], axis=1))
    cp = np.asarray(inp["cmp_pos"], np.float32)[0]
    sh["posT"] = np.ascontiguousarray(cp.transpose(2, 0, 1).reshape(64, 64))
    sh["gdnc"] = np.ascontiguousarray(np.concatenate([np.asarray(inp["gdn_a_log"], np.float32)[0], np.asarray(inp["gdn_dt_bias"], np.float32)[0]]).reshape(1, 8))
    cw = np.asarray(inp["gdn_conv_w"], np.float32)[0]
    sh["convw"] = np.ascontiguousarray(cw.reshape(4, 12, 128).transpose(2, 1, 0).reshape(128, 48))
    sh["gng"] = np.ascontiguousarray(np.asarray(inp["gdn_norm_g"], np.float32)[0].reshape(128, 1))
    sh["wout"] = np.ascontiguousarray(_tile_w(np.asarray(inp["w_out"], np.float32)[0]).reshape(128, 8 * D))
    sh["g2"] = np.ascontiguousarray(np.asarray(inp["norm2_g"], np.float32)[0].reshape(8, 128).T)
    sh["fg"] = np.ascontiguousarray(np.asarray(inp["final_g"], np.float32).reshape(1, D))
    for nm, key in (("wg", "w_gate"), ("wu", "w_up")):
        wt_ = _tile_w(np.asarray(inp[key], np.float32)[0])
        sh[nm] = np.ascontiguousarray(wt_.reshape(128, 8, NFC, 128).transpose(2, 0, 1, 3).reshape(NFC, 128, 1024))
    wdn = np.asarray(inp["w_down"], np.float32)[0]
    sh["wd"] = np.ascontiguousarray(wdn.reshape(NFC, 128, D).transpose(1, 0, 2).reshape(128, NFC * D))
    sh["nsag"] = np.ascontiguousarray(np.asarray(inp["nsa_norm_g"], np.float32)[0].reshape(4, 128).T)
    return sh


def kernel(**inputs):
    x = np.asarray(inputs["x"], np.float32)
    pos = np.asarray(inputs["positions"], np.int32)
    sh = prep_shared(inputs)
    nc = build_program()
    in_maps = []
    for c in range(NCORES):
        m = dict(sh)
        m["x"] = np.ascontiguousarray(x[c * NSEQ:(c + 1) * NSEQ])
        m["pos"] = np.ascontiguousarray(pos[c * NSEQ:(c + 1) * NSEQ])
        in_maps.append(m)
    res = run_bass_kernel_spmd(nc, in_maps, core_ids=list(range(NCORES)))
    return np.concatenate([r["out"] for r in res.results], axis=0)
```
